# Optimizing a Trainium2 kernel written in Bass

```python
import jax
import jax.numpy as jnp
from jax import lax
import numpy as np

D_MODEL = 2048
BATCH = 4
SEQ = 4096
DEPTH = 2

C_MIX = D_MODEL // 2
HEAD_DIM = 64
H_RWKV = C_MIX // HEAD_DIM
D_DECAY_LORA = 64
D_AAA_LORA = 64
D_MV_LORA = 32
D_GATE_LORA = 160
RWKV_GN_EPS = 64e-5
H_LRU = C_MIX // HEAD_DIM
LRU_CONV = 4
LRU_C = 8.0
CONF_CONV = 31
N_EXPERTS = 32
TOP_K = 4
D_FF_EXPERT = D_MODEL // 2
SWIGLU_ALPHA = 1.702
SWIGLU_LIMIT = 7.0
MOE_BLOCK = 512
LN_EPS = 1e-5
DEEPNORM_ALPHA = (2 * DEPTH) ** 0.25
DEEPNORM_BETA = (8 * DEPTH) ** -0.25

RWKV_SPLITS = [C_MIX, C_MIX, C_MIX, D_DECAY_LORA, D_AAA_LORA, D_GATE_LORA]
N_RWKV = sum(RWKV_SPLITS)
IN_SPLITS = [N_RWKV, C_MIX, C_MIX, 2 * C_MIX, 3 * D_MODEL]
N_IN = sum(IN_SPLITS)

kernel_name = 'hybrid_rwkv7_rglru_conformer_moe_deepnorm'


def _split(t, sizes):
    return jnp.split(t, np.cumsum(sizes)[:-1].tolist(), axis=-1)


def layer_norm(x, g, b, eps=LN_EPS):
    xf = x.astype(jnp.float32)
    mu = jnp.mean(xf, -1, keepdims=True)
    var = jnp.mean(jnp.square(xf - mu), -1, keepdims=True)
    return ((xf - mu) * lax.rsqrt(var + eps) * g + b).astype(x.dtype)


def token_shift(p):
    return jnp.pad(p, ((0, 0), (1, 0), (0, 0)))[:, :-1]


def causal_depthwise_conv(x, w, b):
    width, ch = w.shape
    y = lax.conv_general_dilated(x, w[:, None, :], (1,), [(width - 1, 0)],
                                 dimension_numbers=('NWC', 'WIO', 'NWC'),
                                 feature_group_count=ch)
    return y + b


def _rwkv7_step(state, inp):
    r, w, k, v, a, b = inp
    sa = jnp.einsum('bhij,bhj->bhi', state, a)
    state = state * w[:, :, None, :] + sa[..., None] * b[:, :, None, :] + v[..., None] * k[:, :, None, :]
    return state, jnp.einsum('bhij,bhj->bhi', state, r)


def rwkv7_mixer(p, v_first, p_vres, mu, mu_vres, w0, w2, a0, a2, v0, v2, g2, k_k, k_a, r_k, gn_g, gn_b):
    dt = p.dtype
    B, S, _ = p.shape
    f32 = jnp.float32
    p = p + (token_shift(p) - p) * mu
    r, k, v, wl, al, gl = _split(p, RWKV_SPLITS)
    w_log = -jax.nn.softplus(-(w0 + jnp.tanh(wl) @ w2)) - 0.5
    a = jax.nn.sigmoid(a0 + al @ a2)
    g = jax.nn.sigmoid(gl) @ g2
    if v_first is None:
        v_first = v
    else:
        p_vres = p_vres + (token_shift(p_vres) - p_vres) * mu_vres
        v = v + (v_first - v) * jax.nn.sigmoid(v0 + p_vres @ v2)
    heads = lambda t: t.astype(f32).reshape(B, S, H_RWKV, HEAD_DIM)
    kk = heads(k * k_k)
    kk = kk / jnp.maximum(jnp.linalg.norm(kk, axis=-1, keepdims=True), 1e-12)
    k = heads(k * (1 + (a - 1) * k_a))
    r, v, a = heads(r), heads(v), heads(a)
    decay = jnp.exp(-jnp.exp(heads(w_log)))
    to_seq = lambda t: jnp.moveaxis(t, 1, 0)
    state0 = jnp.zeros((B, H_RWKV, HEAD_DIM, HEAD_DIM), f32)
    _, y = lax.scan(_rwkv7_step, state0, tuple(to_seq(t) for t in (r, decay, k, v, -kk, kk * a)))
    y = jnp.moveaxis(y, 0, 1)
    mu_y = jnp.mean(y, -1, keepdims=True)
    var_y = jnp.mean(jnp.square(y - mu_y), -1, keepdims=True)
    y = (y - mu_y) * lax.rsqrt(var_y + RWKV_GN_EPS) * gn_g.reshape(H_RWKV, HEAD_DIM) + gn_b.reshape(H_RWKV, HEAD_DIM)
    y = y + jnp.sum(r * k * r_k, -1, keepdims=True) * v
    return y.reshape(B, S, C_MIX).astype(dt) * g, v_first


def _linear_combine(e1, e2):
    a1, b1 = e1
    a2, b2 = e2
    return a1 * a2, a2 * b1 + b2


def rglru_mixer(gate_in, x_in, conv_w, conv_b, wa, ba, wx, bx, lam):
    B, S, _ = x_in.shape
    xc = causal_depthwise_conv(x_in, conv_w, conv_b)
    xh = xc.reshape(B, S, H_LRU, HEAD_DIM)
    r = jax.nn.sigmoid(jnp.einsum('bshi,hij->bshj', xh, wa).reshape(B, S, C_MIX) + ba)
    i = jax.nn.sigmoid(jnp.einsum('bshi,hij->bshj', xh, wx).reshape(B, S, C_MIX) + bx)
    log_a = (-LRU_C * r * jax.nn.softplus(-lam)).astype(jnp.float32)
    a = jnp.exp(log_a)
    b = jnp.sqrt(-jnp.expm1(2.0 * log_a)) * (i * xc).astype(jnp.float32)
    _, h = lax.associative_scan(_linear_combine, (a, b), axis=1)
    return h.astype(x_in.dtype) * jax.nn.gelu(gate_in)


def conformer_conv_module(u, conv_w, conv_b, ln_g, ln_b):
    val, gate = jnp.split(u, 2, axis=-1)
    c = causal_depthwise_conv(val * jax.nn.sigmoid(gate), conv_w, conv_b)
    return jax.nn.silu(layer_norm(c, ln_g, ln_b))


def moe_ffn(x, router_w, router_b, w_gu, b_gu, w_down, b_down):
    B, S, D = x.shape
    T = B * S
    TK = T * TOP_K
    n_blocks = -(-TK // MOE_BLOCK) + N_EXPERTS
    xf = x.reshape(T, D)
    logits = (xf @ router_w + router_b).astype(jnp.float32)
    top_val, top_idx = lax.top_k(logits, TOP_K)
    gates = jax.nn.softmax(top_val, axis=-1).astype(x.dtype)
    flat_e = top_idx.reshape(TK)
    flat_t = jnp.repeat(jnp.arange(T, dtype=jnp.int32), TOP_K)
    order = jnp.argsort(flat_e)
    sorted_e = flat_e[order]
    counts = jnp.bincount(flat_e, length=N_EXPERTS)
    padded = (counts + MOE_BLOCK - 1) // MOE_BLOCK * MOE_BLOCK
    pad_end = jnp.cumsum(padded)
    pad_start = pad_end - padded
    start = jnp.cumsum(counts) - counts
    dest = pad_start[sorted_e] + jnp.arange(TK) - start[sorted_e]
    row_tok = jnp.zeros((n_blocks * MOE_BLOCK,), jnp.int32).at[dest].set(flat_t[order])
    row_w = jnp.zeros((n_blocks * MOE_BLOCK,), x.dtype).at[dest].set(gates.reshape(TK)[order])
    block_e = jnp.minimum(jnp.searchsorted(pad_end, jnp.arange(n_blocks) * MOE_BLOCK, side='right'), N_EXPERTS - 1)

    def expert_block(args):
        rows, e = args
        gu = xf[rows] @ w_gu[e] + b_gu[e]
        gate, up = jnp.split(gu, 2, axis=-1)
        gate = jnp.minimum(gate, SWIGLU_LIMIT)
        up = jnp.clip(up, -SWIGLU_LIMIT, SWIGLU_LIMIT)
        h = (up + 1.0) * gate * jax.nn.sigmoid(SWIGLU_ALPHA * gate)
        return h @ w_down[e] + b_down[e]

    out = lax.map(expert_block, (row_tok.reshape(n_blocks, MOE_BLOCK), block_e))
    out = out.reshape(-1, D) * row_w[:, None]
    return jnp.zeros_like(xf).at[row_tok].add(out).reshape(B, S, D)


def setup_inputs(seed: int = 0) -> dict:
    key = jax.random.key(seed)
    ks = iter(jax.random.split(key, 48))
    f32 = jnp.float32
    L, Lr, D, C, N = DEPTH, DEPTH - 1, D_MODEL, C_MIX, HEAD_DIM

    def nrm(shape, scale):
        return jax.random.normal(next(ks), shape, f32) * scale

    def unif(shape, lo, hi):
        return jax.random.uniform(next(ks), shape, f32, lo, hi)

    u = unif((L, C), 0.9, 0.999)
    s = u ** (1.0 / LRU_C)
    lam = jnp.log(s) - jnp.log1p(-s)
    return {
        'x': nrm((BATCH, SEQ, D), 1.0),
        'w_in': nrm((L, D, N_IN), D ** -0.5),
        'w_in_vres': nrm((Lr, D, D_MV_LORA), D ** -0.5),
        'shift_mu': unif((L, N_RWKV), 0.0, 1.0),
        'shift_mu_vres': unif((Lr, D_MV_LORA), 0.0, 1.0),
        'rwkv_w0': unif((L, C), -6.5, -1.5),
        'rwkv_w2': nrm((L, D_DECAY_LORA, C), 0.5 * D_DECAY_LORA ** -0.5),
        'rwkv_a0': nrm((L, C), 0.5),
        'rwkv_a2': nrm((L, D_AAA_LORA, C), 0.5 * D_AAA_LORA ** -0.5),
        'rwkv_v0': 1.0 + nrm((Lr, C), 0.1),
        'rwkv_v2': nrm((Lr, D_MV_LORA, C), 0.5 * D_MV_LORA ** -0.5),
        'rwkv_g2': nrm((L, D_GATE_LORA, C), D_GATE_LORA ** -0.5),
        'rwkv_k_k': 0.85 + nrm((L, C), 0.05),
        'rwkv_k_a': 1.0 + nrm((L, C), 0.05),
        'rwkv_r_k': nrm((L, H_RWKV, N), 0.1),
        'rwkv_gn_g': 1.0 + nrm((L, C), 0.02),
        'rwkv_gn_b': nrm((L, C), 0.02),
        'lru_conv_w': nrm((L, LRU_CONV, C), LRU_CONV ** -0.5),
        'lru_conv_b': nrm((L, C), 0.01),
        'lru_wa': nrm((L, H_LRU, N, N), N ** -0.5),
        'lru_ba': nrm((L, C), 0.01),
        'lru_wx': nrm((L, H_LRU, N, N), N ** -0.5),
        'lru_bx': nrm((L, C), 0.01),
        'lru_lambda': lam,
        'conf_conv_w': nrm((L, CONF_CONV, C), CONF_CONV ** -0.5),
        'conf_conv_b': nrm((L, C), 0.01),
        'conf_ln_g': 1.0 + nrm((L, C), 0.02),
        'conf_ln_b': nrm((L, C), 0.02),
        'w_branch': nrm((L, 3, C, D), C ** -0.5),
        'w_out': nrm((L, D, D), DEEPNORM_BETA * D ** -0.5),
        'ln1_g': 1.0 + nrm((L, D), 0.02),
        'ln1_b': nrm((L, D), 0.02),
        'router_w': nrm((L, D, N_EXPERTS), D ** -0.5),
        'router_b': nrm((L, N_EXPERTS), 0.01),
        'exp_w_gu': nrm((L, N_EXPERTS, D, 2 * D_FF_EXPERT), D ** -0.5),
        'exp_b_gu': nrm((L, N_EXPERTS, 2 * D_FF_EXPERT), 0.01),
        'exp_w_down': nrm((L, N_EXPERTS, D_FF_EXPERT, D), DEEPNORM_BETA * D_FF_EXPERT ** -0.5),
        'exp_b_down': nrm((L, N_EXPERTS, D), 0.01),
        'ln2_g': 1.0 + nrm((L, D), 0.02),
        'ln2_b': nrm((L, D), 0.02),
    }


def reference(x, w_in, w_in_vres, shift_mu, shift_mu_vres, rwkv_w0, rwkv_w2, rwkv_a0, rwkv_a2,
              rwkv_v0, rwkv_v2, rwkv_g2, rwkv_k_k, rwkv_k_a, rwkv_r_k, rwkv_gn_g, rwkv_gn_b,
              lru_conv_w, lru_conv_b, lru_wa, lru_ba, lru_wx, lru_bx, lru_lambda,
              conf_conv_w, conf_conv_b, conf_ln_g, conf_ln_b, w_branch, w_out, ln1_g, ln1_b,
              router_w, router_b, exp_w_gu, exp_b_gu, exp_w_down, exp_b_down, ln2_g, ln2_b):
    v_first = None
    for l in range(DEPTH):
        if l == 0:
            proj = x @ w_in[l]
            p_vres, mu_vres, v0, v2 = None, None, None, None
        else:
            proj_all = x @ jnp.concatenate([w_in[l], w_in_vres[l - 1]], axis=1)
            proj, p_vres = proj_all[..., :N_IN], proj_all[..., N_IN:]
            mu_vres, v0, v2 = shift_mu_vres[l - 1], rwkv_v0[l - 1], rwkv_v2[l - 1]
        p_rwkv, lru_gate, lru_x, conf_u, merge = _split(proj, IN_SPLITS)
        y_a, v_first = rwkv7_mixer(p_rwkv, v_first, p_vres, shift_mu[l], mu_vres,
                                   rwkv_w0[l], rwkv_w2[l], rwkv_a0[l], rwkv_a2[l], v0, v2,
                                   rwkv_g2[l], rwkv_k_k[l], rwkv_k_a[l], rwkv_r_k[l],
                                   rwkv_gn_g[l], rwkv_gn_b[l])
        y_b = rglru_mixer(lru_gate, lru_x, lru_conv_w[l], lru_conv_b[l], lru_wa[l], lru_ba[l],
                          lru_wx[l], lru_bx[l], lru_lambda[l])
        y_c = conformer_conv_module(conf_u, conf_conv_w[l], conf_conv_b[l], conf_ln_g[l], conf_ln_b[l])
        g_a, g_b, g_c = jnp.split(jax.nn.sigmoid(merge), 3, axis=-1)
        mixed = (g_a * (y_a @ w_branch[l, 0]) + g_b * (y_b @ w_branch[l, 1])
                 + g_c * (y_c @ w_branch[l, 2]))
        x = layer_norm(DEEPNORM_ALPHA * x + mixed @ w_out[l], ln1_g[l], ln1_b[l])
        ffn = moe_ffn(x, router_w[l], router_b[l], exp_w_gu[l], exp_b_gu[l], exp_w_down[l], exp_b_down[l])
        x = layer_norm(DEEPNORM_ALPHA * x + ffn, ln2_g[l], ln2_b[l])
    return x
```

```python
import contextlib
import math
import numpy as np
import concourse.bass as bass
import concourse.mybir as mybir
from concourse.bass_utils import run_bass_kernel_spmd

F32 = mybir.dt.float32
BF16 = mybir.dt.bfloat16
AF = mybir.ActivationFunctionType
ALU = mybir.AluOpType
AX = mybir.AxisListType
ENG = ("pe", "act", "dve", "pool", "sp")

D = 2048
C = 1024
NIN = 13600
NE = 32
DFF = 1024
KAPPA = math.exp(-0.5)
ALPHA = 4.0 ** 0.25
O_R, O_K, O_V, O_WL, O_AL, O_GL, O_LG, O_LX, O_CU, O_MG, O_VR = 0, 1024, 2048, 3072, 3136, 3200, 3360, 4384, 5408, 7456, 13600


class Buf:
    __slots__ = ("name", "we", "wd", "re", "rd", "sem", "cnt", "last")

    def __init__(self, name):
        self.name = name
        self.reset()

    def reset(self):
        self.we = {}
        self.wd = {}
        self.re = {}
        self.rd = {}
        self.sem = None
        self.cnt = 0
        self.last = None


class T:
    def __init__(self, t, name, nsub=0):
        self.t = t
        self.b = Buf(name)
        self.sub = [Buf(f"{name}.{i}") for i in range(nsub)]

    def __getitem__(self, k):
        return self.t[k]


class Ring:
    def __init__(self, items):
        self.items = items
        self.i = 0

    def next(self):
        r = self.items[self.i % len(self.items)]
        self.i += 1
        return r


class Stage:
    def __init__(self, kb, name):
        self.kb = kb
        self.nc = kb.nc
        self.name = name
        self.es = contextlib.ExitStack()
        self.streams = {e: [] for e in ENG}
        self.count = {e: 0 for e in ENG}
        self.waited = {e: {} for e in ENG}
        self.sems = []
        self.touched = []
        self.esem = {e: self._newsem(e) for e in ENG if e != "sp"}
        self.alt = 0

    def _newsem(self, nm):
        s = self.es.enter_context(self.nc.semaphore(f"{self.name}_{nm}_{len(self.sems)}"))
        self.sems.append(s)
        return len(self.sems) - 1

    def sb(self, name, shape, dt=F32, nsub=0):
        t = self.es.enter_context(self.nc.sbuf_tensor(f"{self.name}_{name}", list(shape), dt))
        return T(t, name, nsub)

    def ps(self, name, shape, dt=F32):
        t = self.es.enter_context(self.nc.psum_tensor(f"{self.name}_{name}", list(shape), dt))
        return T(t, name)

    def ring(self, name, n, shape, dt=F32, psum=False):
        mk = self.ps if psum else self.sb
        return Ring([mk(f"{name}{i}", shape, dt) for i in range(n)])

    def _touch(self, b):
        self.touched.append(b)

    def _deps(self, eng, reads, writes, disjoint=False):
        need = {}

        def add_e(d):
            for e2, n in d.items():
                if e2 == eng and eng == "pe":
                    continue
                s = self.esem[e2]
                if need.get(s, 0) < n:
                    need[s] = n

        def add_d(d):
            for s, v in d.items():
                if need.get(s, 0) < v:
                    need[s] = v

        for b in reads:
            add_e(b.we)
            add_d(b.wd)
        for b in writes:
            if not disjoint:
                add_e(b.we)
                add_d(b.wd)
            add_e(b.re)
            add_d(b.rd)
        out = []
        wt = self.waited[eng]
        for s, v in need.items():
            if wt.get(s, 0) >= v:
                continue
            wt[s] = v
            out.append((s, v))
        return out

    def op(self, eng, fn, r=(), w=()):
        waits = self._deps(eng, r, w)
        self.count[eng] += 1
        n = self.count[eng]
        si = self.esem[eng]
        sems = self.sems

        def run(e, fn=fn, waits=waits, si=si):
            for s, v in waits:
                e.wait_ge(sems[s], v)
            fn(e).then_inc(sems[si], 1)

        self.streams[eng].append(run)
        for b in r:
            b.re[eng] = n
            self._touch(b)
        for b in w:
            b.we = {eng: n}
            b.wd = {}
            b.re = {}
            b.rd = {}
            self._touch(b)

    def dma(self, q, out, in_, owner, r=(), w=(), disjoint=False, kw=None, group=False):
        disjoint = disjoint or group
        waits = self._deps(q, r, w, disjoint=disjoint)
        if owner.sem is None:
            owner.sem = self._newsem("d")
            owner.cnt = 0
            self._touch(owner)
        if owner.last is not None and not group:
            s, v = owner.last
            if self.waited[q].get(s, 0) < v:
                self.waited[q][s] = v
                waits.append((s, v))
        owner.cnt += 16
        ev = (owner.sem, owner.cnt)
        owner.last = ev
        sems = self.sems
        kw = kw or {}

        def run(e, waits=waits, ev=ev, out=out, in_=in_):
            for s, v in waits:
                e.wait_ge(sems[s], v)
            e.dma_start(out=out, in_=in_, **kw).then_inc(sems[ev[0]], 16)

        self.streams[q].append(run)
        for b in r:
            b.rd[ev[0]] = ev[1]
            self._touch(b)
        for b in w:
            if disjoint:
                b.wd[ev[0]] = ev[1]
            else:
                b.we = {}
                b.wd = {ev[0]: ev[1]}
                b.re = {}
                b.rd = {}
            self._touch(b)

    def aeng(self, choices=("dve", "pool")):
        self.alt += 1
        return choices[self.alt % len(choices)]

    def finish(self):
        nc = self.nc
        sems = self.sems
        finals = []
        seen = set()
        for b in self.touched:
            if b.sem is not None and b.sem not in seen:
                seen.add(b.sem)
                finals.append((b.sem, b.cnt))
        for e in ENG:
            if e != "sp" and self.count[e] > 0:
                finals.append((self.esem[e], self.count[e]))

        def fin(e):
            for s, v in finals:
                e.wait_ge(sems[s], v)

        self.streams["sp"].append(fin)
        streams = self.streams
        with nc.Block(self.name) as block:
            @block.sync
            def _(e):
                for f in streams["sp"]:
                    f(e)

            @block.tensor
            def _(e):
                for f in streams["pe"]:
                    f(e)

            @block.scalar
            def _(e):
                for f in streams["act"]:
                    f(e)

            @block.vector
            def _(e):
                for f in streams["dve"]:
                    f(e)

            @block.gpsimd
            def _(e):
                for f in streams["pool"]:
                    f(e)
        with nc.Block(self.name + "_clr") as blk:
            @blk.gpsimd
            def _(e):
                for s_ in sems:
                    e.sem_clear(s_)
        for b in self.touched:
            b.reset()
        self.es.close()


def tt(st, eng, out, in0, in1, op, r, w):
    st.op(eng, lambda e: e.tensor_tensor(out=out, in0=in0, in1=in1, op=op), r=r, w=w)


def ts(st, eng, out, in0, s1, op0, r, w, s2=None, op1=None):
    if op1 is None:
        st.op(eng, lambda e: e.tensor_scalar(out=out, in0=in0, scalar1=s1, scalar2=None, op0=op0), r=r, w=w)
    else:
        st.op(eng, lambda e: e.tensor_scalar(out=out, in0=in0, scalar1=s1, scalar2=s2, op0=op0, op1=op1), r=r, w=w)


def stt(st, eng, out, in0, scalar, in1, op0, op1, r, w):
    st.op(eng, lambda e: e.scalar_tensor_tensor(out=out, in0=in0, scalar=scalar, in1=in1, op0=op0, op1=op1), r=r, w=w)


def act(st, out, in_, func, r, w, bias=None, scale=None):
    kw = {}
    if bias is not None:
        kw["bias"] = bias
    if scale is not None:
        kw["scale"] = scale
    st.op("act", lambda e: e.activation(out=out, in_=in_, func=func, **kw), r=r, w=w)


def cp(st, eng, out, in_, r, w):
    if eng == "act":
        st.op("act", lambda e: e.copy(out=out, in_=in_), r=r, w=w)
    else:
        st.op(eng, lambda e: e.tensor_copy(out=out, in_=in_), r=r, w=w)


def mm(st, out, lhsT, rhs, start, stop, r, w):
    st.op("pe", lambda e: e.matmul(out, lhsT, rhs, start=start, stop=stop), r=r, w=w)


def tr(st, out, in_, ident, r, w):
    st.op("pe", lambda e: e.transpose(out, in_, ident), r=r, w=w)


def col_table():
    tab = {}
    off = 0
    for nm, n in [("mu_r", 8), ("mu_k", 8), ("mu_v", 8), ("w0", 8), ("a0", 8), ("v0", 8), ("k_k", 8), ("k_a", 8),
                  ("r_k", 8), ("gn_g", 8), ("gn_b", 8), ("lconv_b", 8), ("l_ba", 8), ("l_bx", 8), ("l_lam", 8),
                  ("lconv_w", 32), ("cconv_w", 248), ("cconv_b", 8), ("cln_g", 8), ("cln_b", 8),
                  ("ln1_g", 16), ("ln1_b", 16), ("mu_wl", 1), ("mu_al", 1), ("mu_gl", 2), ("mu_vr", 1)]:
        tab[nm] = (off, n)
        off += n
    return tab, off


COLT, NCOLS = col_table()


def chunkcols(v):
    v = np.asarray(v, np.float32).reshape(-1)
    n = v.shape[0]
    m = (n + 127) // 128
    buf = np.zeros((m * 128,), np.float32)
    buf[:n] = v
    return buf.reshape(m, 128).T


def host_layout(inp, l):
    cols = np.zeros((128, NCOLS), np.float32)

    def put(nm, arr):
        o, n = COLT[nm]
        cols[:, o:o + n] = arr

    mu = inp["shift_mu"][l]
    put("mu_r", chunkcols(mu[0:1024]))
    put("mu_k", chunkcols(mu[1024:2048]))
    put("mu_v", chunkcols(mu[2048:3072]))
    put("mu_wl", chunkcols(mu[3072:3136]))
    put("mu_al", chunkcols(mu[3136:3200]))
    put("mu_gl", chunkcols(mu[3200:3360]))
    put("w0", chunkcols(inp["rwkv_w0"][l]))
    put("a0", chunkcols(inp["rwkv_a0"][l]))
    if l > 0:
        put("v0", chunkcols(inp["rwkv_v0"][l - 1]))
        put("mu_vr", chunkcols(inp["shift_mu_vres"][l - 1]))
    put("k_k", chunkcols(inp["rwkv_k_k"][l]))
    put("k_a", chunkcols(inp["rwkv_k_a"][l]))
    put("r_k", chunkcols(inp["rwkv_r_k"][l].reshape(-1)))
    put("gn_g", chunkcols(inp["rwkv_gn_g"][l]))
    put("gn_b", chunkcols(inp["rwkv_gn_b"][l]))
    put("lconv_b", chunkcols(inp["lru_conv_b"][l]))
    put("l_ba", chunkcols(inp["lru_ba"][l]))
    put("l_bx", chunkcols(inp["lru_bx"][l]))
    put("l_lam", chunkcols(inp["lru_lambda"][l]))
    lw = inp["lru_conv_w"][l]
    put("lconv_w", lw.reshape(4, 8, 128).transpose(2, 1, 0).reshape(128, 32))
    cw = inp["conf_conv_w"][l]
    put("cconv_w", cw.reshape(31, 8, 128).transpose(2, 1, 0).reshape(128, 248))
    put("cconv_b", chunkcols(inp["conf_conv_b"][l]))
    put("cln_g", chunkcols(inp["conf_ln_g"][l]))
    put("cln_b", chunkcols(inp["conf_ln_b"][l]))
    put("ln1_g", chunkcols(inp["ln1_g"][l]))
    put("ln1_b", chunkcols(inp["ln1_b"][l]))
    bd = np.zeros((8, 128, 256), np.float32)
    for c in range(8):
        for hh in range(2):
            bd[c, hh * 64:(hh + 1) * 64, hh * 64:(hh + 1) * 64] = inp["lru_wa"][l][2 * c + hh]
            bd[c, hh * 64:(hh + 1) * 64, 128 + hh * 64:128 + (hh + 1) * 64] = inp["lru_wx"][l][2 * c + hh]
    bgu = np.ascontiguousarray(inp["exp_b_gu"][l].reshape(NE, 16, 128).transpose(2, 0, 1)).reshape(128, NE * 16)
    ln2 = np.ascontiguousarray(np.broadcast_to(
        np.concatenate([inp["ln2_g"][l], inp["ln2_b"][l]])[None, :], (128, 2 * D))).astype(np.float32)
    rb = np.ascontiguousarray(np.broadcast_to(inp["router_b"][l][None, :], (128, NE))).astype(np.float32)
    return {"cols": cols, "lrubd": bd, "bgu": bgu, "ln2bc": ln2, "rbbc": rb}


def host_consts():
    ident = np.eye(128, dtype=np.float32)
    ones = np.ones((128, 128), np.float32)
    bones = np.zeros((128, 128), np.float32)
    bones[:64, :64] = 1
    bones[64:, 64:] = 1
    s = np.arange(128)[:, None]
    t = np.arange(128)[None, :]
    m_su = (s < t).astype(np.float32)
    m_ui = (s <= t).astype(np.float32)
    m_sl = (s > t).astype(np.float32)
    cmask = np.ones((128, 512), np.float32)
    cmask[:, ::128] = 0
    return np.concatenate([ident, ones, bones, m_su, m_ui, m_sl, cmask], axis=1)


K_ID, K_ONES, K_BONES, K_SU, K_UI, K_SL, K_CM = 0, 128, 256, 384, 512, 640, 768
NCONST = 768 + 512


class KB:
    def __init__(self, S, L, debug=False):
        self.S = S
        self.L = L
        self.debug = debug
        self.nc = bass.Bass("TRN2", target_bir_lowering=False)
        self.d = {}
        self.bufs = {}

    def din(self, name, shape, dt=F32):
        self.d[name] = self.nc.dram_tensor("d_" + name, list(shape), dt, kind="ExternalInput").ap()
        self.bufs[name] = Buf(name)

    def dout(self, name, shape, dt=F32):
        self.d[name] = self.nc.dram_tensor("d_" + name, list(shape), dt, kind="ExternalOutput").ap()
        self.bufs[name] = Buf(name)

    def dscr(self, name, shape, dt=F32):
        kind = "ExternalOutput" if (self.debug and name in self.debug) else "Internal"
        self.d[name] = self.nc.dram_tensor("d_" + name, list(shape), dt, kind=kind).ap()
        self.bufs[name] = Buf(name)

    def stage(self, name):
        return Stage(self, name)


def load_consts(st, kb):
    k = st.sb("konst", [128, NCONST], F32)
    st.dma("sp", k[:, :], kb.d["konst"][:, :], k.b, r=[kb.bufs["konst"]], w=[k.b])
    return k


def load_cols(st, kb, l):
    c = st.sb("cols", [128, NCOLS], F32)
    st.dma("sp", c[:, :], kb.d[f"cols{l}"][:, :], c.b, r=[kb.bufs[f"cols{l}"]], w=[c.b])
    return c


def colap(cols, nm, j=0, rows=128):
    o, n = COLT[nm]
    return cols[0:rows, o + j:o + j + 1]


def stage_inproj(kb, l, xsrc):
    S = kb.S
    st = kb.stage(f"A{l}")
    TS = min(2048, S)
    nsb = S // TS
    ntb = TS // 512
    xbf = st.sb("xbf", [128, 16, TS], BF16, nsub=16)
    xst = st.ring("xst", 2, [128, TS])
    wst = st.ring("wst", 2, [128, 16, 256])
    wbf = st.ring("wbf", 2, [128, 16, 256], BF16)
    ost = st.ring("ost", 4, [128, 512])
    pss = st.ring("ps", 4, [128, 512], psum=True)
    P = kb.d["P"]
    Pb = kb.bufs["P"]
    groups = [(kb.d["w_in"][l], kb.bufs["w_in"], c0, min(256, NIN - c0), c0) for c0 in range(0, NIN, 256)]
    if l > 0:
        groups.append((kb.d["w_in_vres"][l - 1], kb.bufs["w_in_vres"], 0, 32, O_VR))
    xs, xsb = kb.d[xsrc], kb.bufs[xsrc]

    def load_w(g):
        W, Wb, c0, gc, prow = g
        ws = wst.next()
        st.dma("sp", ws[:, :, 0:gc], W[:, c0:c0 + gc].rearrange("(kc p) c -> p kc c", p=128), ws.b, r=[Wb], w=[ws.b])
        wb = wbf.next()
        cp(st, "pool", wb[:, :, 0:gc], ws[:, :, 0:gc], r=[ws.b], w=[wb.b])
        return wb

    for sbi in range(nsb):
        t0 = sbi * TS
        for kc in range(16):
            xt = xst.next()
            st.dma("sp", xt[:, :], xs[kc * 128:(kc + 1) * 128, t0:t0 + TS], xt.b, r=[xsb], w=[xt.b])
            cp(st, "act" if kc % 2 else "dve", xbf[:, kc, :], xt[:, :], r=[xt.b], w=[xbf.sub[kc]])
        nxt = load_w(groups[0])
        for gi, g in enumerate(groups):
            wb = nxt
            if gi + 1 < len(groups):
                nxt = load_w(groups[gi + 1])
            _, _, c0, gc, prow = g
            for cc in range(0, gc, 128):
                ncol = min(128, gc - cc)
                for tb in range(ntb):
                    ps = pss.next()
                    for kc in range(16):
                        mm(st, ps[0:ncol, :], wb[:, kc, cc:cc + ncol], xbf[:, kc, tb * 512:(tb + 1) * 512],
                           kc == 0, kc == 15, r=[wb.b, xbf.sub[kc]], w=[ps.b])
                    o = ost.next()
                    cp(st, "dve", o[0:ncol, :], ps[0:ncol, :], r=[ps.b], w=[o.b])
                    st.dma("act", P[prow + cc:prow + cc + ncol, t0 + tb * 512:t0 + (tb + 1) * 512], o[0:ncol, :], o.b,
                           r=[o.b], w=[Pb], disjoint=True)
    st.finish()


def load_halo(st, dst, src, rows, t0, n, h, srcbuf, q="sp"):
    if t0 == 0:
        st.op("pool", lambda e: e.memset(dst[0:rows, 0:h], 0.0), r=[], w=[dst.b])
        st.dma(q, dst[0:rows, h:h + n], src[:, 0:n], dst.b, r=[srcbuf], w=[dst.b])
    else:
        st.dma(q, dst[0:rows, 0:h + n], src[:, t0 - h:t0 + n], dst.b, r=[srcbuf], w=[dst.b])


def stage_rwkv_prep(kb, l):
    S = kb.S
    st = kb.stage(f"B{l}")
    k = load_consts(st, kb)
    cols = load_cols(st, kb, l)
    P, Pb = kb.d["P"], kb.bufs["P"]
    ntb = S // 512
    bones = k[:, K_BONES:K_BONES + 128]
    cmask = k[:, K_CM:K_CM + 512]
    w2 = st.sb("w2", [64, C])
    a2 = st.sb("a2", [64, C])
    g2a = st.sb("g2a", [128, C])
    g2b = st.sb("g2b", [32, C])
    st.dma("sp", w2[:, :], kb.d["rwkv_w2"][l], w2.b, r=[kb.bufs["rwkv_w2"]], w=[w2.b])
    st.dma("sp", a2[:, :], kb.d["rwkv_a2"][l], a2.b, r=[kb.bufs["rwkv_a2"]], w=[a2.b])
    st.dma("sp", g2a[:, :], kb.d["rwkv_g2"][l][0:128, :], g2a.b, r=[kb.bufs["rwkv_g2"]], w=[g2a.b])
    st.dma("sp", g2b[:, :], kb.d["rwkv_g2"][l][128:160, :], g2b.b, r=[kb.bufs["rwkv_g2"]], w=[g2b.b])
    if l > 0:
        v2 = st.sb("v2", [32, C])
        st.dma("sp", v2[:, :], kb.d["rwkv_v2"][l - 1], v2.b, r=[kb.bufs["rwkv_v2"]], w=[v2.b])

    def R(name, n=2, shape=(128, 512)):
        return st.ring(name, n, list(shape))

    lraw = R("lraw", 2, (128, 513))
    ld = R("ld", 1)
    LW, LA, LG1, LG2, LV = R("LW", 2), R("LA", 2), R("LG1", 2), R("LG2", 2), R("LV", 2)
    raw = {n: R("raw" + n, 2, (128, 513)) for n in "rkv"}
    names = ["dm", "rp", "kp", "vp", "sg", "ag", "kk", "kk2", "nrm", "kkn", "u", "kmod", "rkp", "bvec", "cs", "d3",
             "d4", "E1", "E2", "E3", "E4", "o_r", "o_a", "o_b", "o_k", "o_bh", "o_kh", "o_g", "o_rkv", "vf", "sv"]
    tl = {n: R(n, 2 if n.startswith("o_") else 1) for n in names}
    psr = st.ring("ps", 6, [128, 512], psum=True)

    def mix(dst, src, mucol, rows):
        d = ld.next()
        tt(st, "pool", d[0:rows, :], src[0:rows, 0:512], src[0:rows, 1:513], ALU.subtract, r=[src.b], w=[d.b])
        stt(st, "dve", dst[0:rows, :], d[0:rows, :], mucol, src[0:rows, 1:513], ALU.mult, ALU.add,
            r=[d.b, src.b, cols.b], w=[dst.b])

    def store(dname, row0, t0, tile_, rows=128, n=512):
        st.dma("act", kb.d[dname][row0:row0 + rows, t0:t0 + n], tile_[0:rows, 0:n], tile_.b, r=[tile_.b],
               w=[kb.bufs[dname]], disjoint=True)

    for tb in range(ntb):
        t0 = tb * 512
        x = lraw.next()
        load_halo(st, x, P[O_WL:O_WL + 64, :], 64, t0, 512, 1, Pb)
        lw = LW.next()
        mix(lw, x, colap(cols, "mu_wl", 0, 64), 64)
        act(st, lw[0:64, :], lw[0:64, :], AF.Tanh, r=[lw.b], w=[lw.b])
        x = lraw.next()
        load_halo(st, x, P[O_AL:O_AL + 64, :], 64, t0, 512, 1, Pb)
        la = LA.next()
        mix(la, x, colap(cols, "mu_al", 0, 64), 64)
        x = lraw.next()
        load_halo(st, x, P[O_GL:O_GL + 128, :], 128, t0, 512, 1, Pb)
        lg1 = LG1.next()
        mix(lg1, x, colap(cols, "mu_gl", 0, 128), 128)
        act(st, lg1[:, :], lg1[:, :], AF.Sigmoid, r=[lg1.b], w=[lg1.b])
        x = lraw.next()
        load_halo(st, x, P[O_GL + 128:O_GL + 160, :], 32, t0, 512, 1, Pb)
        lg2 = LG2.next()
        mix(lg2, x, colap(cols, "mu_gl", 1, 32), 32)
        act(st, lg2[0:32, :], lg2[0:32, :], AF.Sigmoid, r=[lg2.b], w=[lg2.b])
        if l > 0:
            x = lraw.next()
            load_halo(st, x, P[O_VR:O_VR + 32, :], 32, t0, 512, 1, Pb)
            lv = LV.next()
            mix(lv, x, colap(cols, "mu_vr", 0, 32), 32)
        for c in range(8):
            cs_ = slice(c * 128, (c + 1) * 128)
            rw = {}
            for n_, off in (("r", O_R), ("k", O_K), ("v", O_V)):
                rw[n_] = raw[n_].next()
                load_halo(st, rw[n_], P[off + c * 128:off + (c + 1) * 128, :], 128, t0, 512, 1, Pb)
            rp, kp, vp = tl["rp"].next(), tl["kp"].next(), tl["vp"].next()
            mix(rp, rw["r"], colap(cols, "mu_r", c), 128)
            mix(kp, rw["k"], colap(cols, "mu_k", c), 128)
            mix(vp, rw["v"], colap(cols, "mu_v", c), 128)
            ps = psr.next()
            mm(st, ps[:, :], w2[0:64, cs_], lw[0:64, :], True, True, r=[w2.b, lw.b], w=[ps.b])
            sg = tl["sg"].next()
            act(st, sg[:, :], ps[:, :], AF.Sigmoid, r=[ps.b, cols.b], w=[sg.b], bias=colap(cols, "w0", c))
            ps = psr.next()
            mm(st, ps[:, :], a2[0:64, cs_], la[0:64, :], True, True, r=[a2.b, la.b], w=[ps.b])
            ag = tl["ag"].next()
            act(st, ag[:, :], ps[:, :], AF.Sigmoid, r=[ps.b, cols.b], w=[ag.b], bias=colap(cols, "a0", c))
            ps = psr.next()
            mm(st, ps[:, :], g2a[:, cs_], lg1[:, :], True, False, r=[g2a.b, lg1.b], w=[ps.b])
            mm(st, ps[:, :], g2b[0:32, cs_], lg2[0:32, :], False, True, r=[g2b.b, lg2.b], w=[ps.b])
            og = tl["o_g"].next()
            cp(st, "act", og[:, :], ps[:, :], r=[ps.b], w=[og.b])
            store("G", c * 128, t0, og)
            if l > 0:
                ps = psr.next()
                mm(st, ps[:, :], v2[0:32, cs_], lv[0:32, :], True, True, r=[v2.b, lv.b], w=[ps.b])
                sv = tl["sv"].next()
                act(st, sv[:, :], ps[:, :], AF.Sigmoid, r=[ps.b, cols.b], w=[sv.b], bias=colap(cols, "v0", c))
                vf = tl["vf"].next()
                st.dma("sp", vf[:, :], kb.d["VF"][cs_, t0:t0 + 512], vf.b, r=[kb.bufs["VF"]], w=[vf.b])
                tt(st, "pool", vf[:, :], vf[:, :], vp[:, :], ALU.subtract, r=[vf.b, vp.b], w=[vf.b])
                tt(st, "dve", vf[:, :], vf[:, :], sv[:, :], ALU.mult, r=[vf.b, sv.b], w=[vf.b])
                tt(st, "pool", vp[:, :], vp[:, :], vf[:, :], ALU.add, r=[vp.b, vf.b], w=[vp.b])
            else:
                store("VF", c * 128, t0, vp)
            store("SV", c * 128, t0, vp)
            kk, kk2 = tl["kk"].next(), tl["kk2"].next()
            ts(st, "dve", kk[:, :], kp[:, :], colap(cols, "k_k", c), ALU.mult, r=[kp.b, cols.b], w=[kk.b])
            tt(st, "pool", kk2[:, :], kk[:, :], kk[:, :], ALU.mult, r=[kk.b], w=[kk2.b])
            ps = psr.next()
            mm(st, ps[:, :], bones, kk2[:, :], True, True, r=[k.b, kk2.b], w=[ps.b])
            nrm = tl["nrm"].next()
            act(st, nrm[:, :], ps[:, :], AF.Sqrt, r=[ps.b], w=[nrm.b])
            ts(st, "dve", nrm[:, :], nrm[:, :], 1e-12, ALU.max, r=[nrm.b], w=[nrm.b])
            st.op("dve", lambda e, o=nrm: e.reciprocal(out=o[:, :], in_=o[:, :]), r=[nrm.b], w=[nrm.b])
            kkn = tl["kkn"].next()
            tt(st, "pool", kkn[:, :], kk[:, :], nrm[:, :], ALU.mult, r=[kk.b, nrm.b], w=[kkn.b])
            u, kmod = tl["u"].next(), tl["kmod"].next()
            ts(st, "dve", u[:, :], ag[:, :], -1.0, ALU.add, r=[ag.b, cols.b], w=[u.b], s2=colap(cols, "k_a", c),
               op1=ALU.mult)
            stt(st, "dve", kmod[:, :], u[:, :], 1.0, kp[:, :], ALU.add, ALU.mult, r=[u.b, kp.b], w=[kmod.b])
            rkp = tl["rkp"].next()
            stt(st, "dve", rkp[:, :], rp[:, :], colap(cols, "r_k", c), kmod[:, :], ALU.mult, ALU.mult,
                r=[rp.b, kmod.b, cols.b], w=[rkp.b])
            ps = psr.next()
            mm(st, ps[:, :], bones, rkp[:, :], True, True, r=[k.b, rkp.b], w=[ps.b])
            orkv = tl["o_rkv"].next()
            tt(st, "dve", orkv[:, :], ps[:, :], vp[:, :], ALU.mult, r=[ps.b, vp.b], w=[orkv.b])
            store("RKV", c * 128, t0, orkv)
            bvec = tl["bvec"].next()
            tt(st, "pool", bvec[:, :], kkn[:, :], ag[:, :], ALU.mult, r=[kkn.b, ag.b], w=[bvec.b])
            cs = tl["cs"].next()
            st.op("dve", lambda e, o=cs, s=sg: e.tensor_tensor_scan(out=o[:, :], data0=cmask, data1=s[:, :],
                                                                    initial=0.0, op0=ALU.mult, op1=ALU.add),
                  r=[k.b, sg.b], w=[cs.b])
            E1, E2, E3, E4 = tl["E1"].next(), tl["E2"].next(), tl["E3"].next(), tl["E4"].next()
            act(st, E1[:, :], cs[:, :], AF.Exp, r=[cs.b], w=[E1.b], scale=-KAPPA)
            act(st, E2[:, :], cs[:, :], AF.Exp, r=[cs.b], w=[E2.b], scale=KAPPA)
            d3, d4 = tl["d3"].next(), tl["d4"].next()
            tt(st, "pool", d3[:, :], cs[:, :], sg[:, :], ALU.subtract, r=[cs.b, sg.b], w=[d3.b])
            act(st, E3[:, :], d3[:, :], AF.Exp, r=[d3.b], w=[E3.b], scale=-KAPPA)
            for ci in range(4):
                ts(st, "dve", d4[:, ci * 128:(ci + 1) * 128], cs[:, ci * 128:(ci + 1) * 128],
                   cs[:, ci * 128 + 127:ci * 128 + 128], ALU.subtract, r=[cs.b], w=[d4.b])
            act(st, E4[:, :], d4[:, :], AF.Exp, r=[d4.b], w=[E4.b], scale=KAPPA)
            o = tl["o_r"].next()
            tt(st, "pool", o[:, :], rp[:, :], E1[:, :], ALU.mult, r=[rp.b, E1.b], w=[o.b])
            store("SR", c * 128, t0, o)
            o = tl["o_a"].next()
            stt(st, "dve", o[:, :], kkn[:, :], -1.0, E3[:, :], ALU.mult, ALU.mult, r=[kkn.b, E3.b], w=[o.b])
            store("SA", c * 128, t0, o)
            o = tl["o_b"].next()
            tt(st, "pool", o[:, :], bvec[:, :], E2[:, :], ALU.mult, r=[bvec.b, E2.b], w=[o.b])
            store("SB", c * 128, t0, o)
            o = tl["o_k"].next()
            tt(st, "dve", o[:, :], kmod[:, :], E2[:, :], ALU.mult, r=[kmod.b, E2.b], w=[o.b])
            store("SK", c * 128, t0, o)
            o = tl["o_bh"].next()
            tt(st, "pool", o[:, :], bvec[:, :], E4[:, :], ALU.mult, r=[bvec.b, E4.b], w=[o.b])
            store("SBH", c * 128, t0, o)
            o = tl["o_kh"].next()
            tt(st, "dve", o[:, :], kmod[:, :], E4[:, :], ALU.mult, r=[kmod.b, E4.b], w=[o.b])
            store("SKH", c * 128, t0, o)
            st.dma("act", kb.d["GL"][cs_, tb * 4:tb * 4 + 4], E1[:, 127:512:128], E1.b, r=[E1.b], w=[kb.bufs["GL"]],
                   disjoint=True, kw={"allow_slow_non_contiguous": True})
    st.finish()


def stage_rwkv_scan(kb, l, G=2, upto=99):
    S = kb.S
    st = kb.stage(f"C{l}")
    k = load_consts(st, kb)
    ident64 = k[0:64, K_ID:K_ID + 64]
    NCH = S // 128
    NB = S // 256
    slots = []
    for g in range(G):
        d = {}
        d["AR"] = st.ring(f"AR{g}", 2, [64, 2, 256])
        d["BK"] = st.ring(f"BK{g}", 2, [64, 2, 256])
        d["HV"] = st.ring(f"HV{g}", 2, [64, 3, 256])
        d["YS"] = st.ring(f"YS{g}", 2, [64, 256])
        d["GL"] = st.sb(f"GL{g}", [64, NCH])
        d["N0RB"] = st.sb(f"N0RB{g}", [128, 256])
        d["AKRK"] = st.sb(f"AKRK{g}", [128, 256])
        d["N"] = [None] + [st.sb(f"N{g}_{i}", [128, 128]) for i in range(1, 7)]
        d["M"] = [st.sb(f"M{g}_{i}", [128, 128]) for i in range(2)]
        d["VBK"] = st.sb(f"VBK{g}", [128, 192])
        d["U"] = [st.sb(f"U{g}_{i}", [128, 64]) for i in range(2)]
        d["H"] = [st.sb(f"H{g}_{i}", [64, 64]) for i in range(2)]
        pa = st.ps(f"pa{g}", [128, 512])
        pb = st.ps(f"pb{g}", [128, 512])
        pc = st.ps(f"pc{g}", [128, 512])
        pd = st.ps(f"pd{g}", [128, 512])
        d["p_s1"] = (pa, slice(0, 256), pa.b)
        d["p_s2"] = (pa, slice(256, 512), pa.b)
        d["p_m"] = (pb, slice(0, 128), pb.b)
        d["p_t"] = (pb, slice(128, 320), pb.b)
        d["p_w"] = (pb, slice(320, 384), pb.b)
        d["p_n"] = (pc, slice(0, 128), pc.b)
        d["p_mm"] = (pc, slice(128, 256), pc.b)
        d["p_u"] = (pd, slice(0, 64), pd.b)
        d["p_h"] = (pd, slice(64, 128), pd.b)
        d["p_y"] = (pd, slice(128, 256), pd.b)
        slots.append(d)
    m_a = k[:, K_SU:K_SU + 256]
    m_sl = k[:, K_SL:K_SL + 128]

    def P_(d, nm, rows=128):
        t_, sl, b = d[nm]
        return t_[0:rows, sl], b

    for h0 in range(0, 16, G):
        heads = list(range(h0, min(16, h0 + G)))
        hs = {}
        for gi, h in enumerate(heads):
            d = slots[gi]
            r0 = h * 64
            st.dma("sp", d["GL"][:, :], kb.d["GL"][r0:r0 + 64, :], d["GL"].b, r=[kb.bufs["GL"]], w=[d["GL"].b])
            st.op("pool", lambda e, d=d: e.memset(d["H"][0][:, :], 0.0), r=[], w=[d["H"][0].b])
            hs[h] = {"hi": 0}
        for bi in range(NB):
            t0 = bi * 256
            cur = {}
            for gi, h in enumerate(heads):
                d = slots[gi]
                r0 = h * 64
                AR, BK, HV, YS = d["AR"].next(), d["BK"].next(), d["HV"].next(), d["YS"].next()

                def ld(dst, nm):
                    st.dma("sp", dst, kb.d[nm][r0:r0 + 64, t0:t0 + 256], dstb, r=[kb.bufs[nm]], w=[dstb])

                dstb = AR.b
                st.dma("sp", AR[:, :, 0:128], kb.d["SA"][r0:r0 + 64, t0:t0 + 256].rearrange("p (c t) -> p c t", t=128),
                       AR.b, r=[kb.bufs["SA"]], w=[AR.b])
                st.dma("sp", AR[:, :, 128:256], kb.d["SR"][r0:r0 + 64, t0:t0 + 256].rearrange("p (c t) -> p c t", t=128),
                       AR.b, r=[kb.bufs["SR"]], w=[AR.b], group=True)
                st.dma("sp", BK[:, 0, :], kb.d["SB"][r0:r0 + 64, t0:t0 + 256], BK.b, r=[kb.bufs["SB"]], w=[BK.b])
                st.dma("sp", BK[:, 1, :], kb.d["SK"][r0:r0 + 64, t0:t0 + 256], BK.b, r=[kb.bufs["SK"]], w=[BK.b], group=True)
                st.dma("sp", HV[:, 0, :], kb.d["SBH"][r0:r0 + 64, t0:t0 + 256], HV.b, r=[kb.bufs["SBH"]], w=[HV.b])
                st.dma("sp", HV[:, 1, :], kb.d["SKH"][r0:r0 + 64, t0:t0 + 256], HV.b, r=[kb.bufs["SKH"]], w=[HV.b], group=True)
                st.dma("sp", HV[:, 2, :], kb.d["SV"][r0:r0 + 64, t0:t0 + 256], HV.b, r=[kb.bufs["SV"]], w=[HV.b], group=True)
                cur[h] = (AR, BK, HV, YS)
            for ci in range(2):
                ch = bi * 2 + ci
                cs_ = slice(ci * 128, (ci + 1) * 128)
                for gi, h in enumerate(heads):
                    d = slots[gi]
                    AR, BK, HV, YS = cur[h]
                    o, b = P_(d, "p_s1")
                    mm(st, o, BK[:, 0, cs_], AR[:, ci, :], True, True, r=[BK.b, AR.b], w=[b])
                    tt(st, "dve", d["N0RB"][:, :], o, m_a, ALU.mult, r=[b, k.b], w=[d["N0RB"].b])
                    o, b = P_(d, "p_s2")
                    mm(st, o, BK[:, 1, cs_], AR[:, ci, :], True, True, r=[BK.b, AR.b], w=[b])
                    tt(st, "dve", d["AKRK"][:, :], o, m_a, ALU.mult, r=[b, k.b], w=[d["AKRK"].b])
                    o, b = P_(d, "p_m")
                    mm(st, o, AR[:, ci, 0:128], BK[:, 0, cs_], True, True, r=[BK.b, AR.b], w=[b])
                    tt(st, "dve", d["M"][0][:, :], o, m_sl, ALU.mult, r=[b, k.b], w=[d["M"][0].b])
                    t_, sl, b = d["p_t"]
                    base = sl.start
                    tr(st, t_[:, base:base + 64], HV[:, 2, cs_], ident64, r=[HV.b, k.b], w=[b])
                    tr(st, t_[:, base + 64:base + 128], HV[:, 0, cs_], ident64, r=[HV.b, k.b], w=[b])
                    tr(st, t_[:, base + 128:base + 192], HV[:, 1, cs_], ident64, r=[HV.b, k.b], w=[b])
                    cp(st, "act", d["VBK"][:, :], t_[:, sl], r=[b], w=[d["VBK"].b])
                if upto < 2:
                    continue
                for kk_ in range(6):
                    for gi, h in enumerate(heads):
                        d = slots[gi]
                        Nk = d["N0RB"][:, 0:128] if kk_ == 0 else d["N"][kk_][:, :]
                        Nkb = d["N0RB"].b if kk_ == 0 else d["N"][kk_].b
                        Mk = d["M"][kk_ % 2]
                        o, b = P_(d, "p_n")
                        mm(st, o, Mk[:, :], Nk, True, True, r=[Mk.b, Nkb], w=[b])
                        cp(st, "act", d["N"][kk_ + 1][:, :], o, r=[b], w=[d["N"][kk_ + 1].b])
                        if kk_ < 5:
                            o, b = P_(d, "p_mm")
                            mm(st, o, Nk, Mk[:, :], True, True, r=[Mk.b, Nkb], w=[b])
                            Mn = d["M"][(kk_ + 1) % 2]
                            cp(st, "dve", Mn[:, :], o, r=[b], w=[Mn.b])
                if upto < 3:
                    continue
                for gi, h in enumerate(heads):
                    d = slots[gi]
                    AR, BK, HV, YS = cur[h]
                    H = d["H"][hs[h]["hi"]]
                    o, b = P_(d, "p_w")
                    mm(st, o, AR[:, ci, 0:128], H[:, :], True, False, r=[AR.b, H.b], w=[b])
                    mm(st, o, d["AKRK"][:, 0:128], d["VBK"][:, 0:64], False, True, r=[d["AKRK"].b, d["VBK"].b], w=[b])
                    cp(st, "act", d["U"][0][:, :], o, r=[b], w=[d["U"][0].b])
                for kk_ in range(7):
                    for gi, h in enumerate(heads):
                        d = slots[gi]
                        Nk = d["N0RB"][:, 0:128] if kk_ == 0 else d["N"][kk_][:, :]
                        Nkb = d["N0RB"].b if kk_ == 0 else d["N"][kk_].b
                        Uk, Un = d["U"][kk_ % 2], d["U"][(kk_ + 1) % 2]
                        o, b = P_(d, "p_u")
                        mm(st, o, Nk, Uk[:, :], True, True, r=[Nkb, Uk.b], w=[b])
                        tt(st, "dve", Un[:, :], o, Uk[:, :], ALU.add, r=[b, Uk.b], w=[Un.b])
                if upto < 4:
                    continue
                for gi, h in enumerate(heads):
                    d = slots[gi]
                    AR, BK, HV, YS = cur[h]
                    H = d["H"][hs[h]["hi"]]
                    Hn = d["H"][1 - hs[h]["hi"]]
                    Uf = d["U"][1]
                    o, b = P_(d, "p_y", 64)
                    mm(st, o, H[:, :], AR[:, ci, 128:256], True, False, r=[H.b, AR.b], w=[b])
                    mm(st, o, Uf[:, :], d["N0RB"][:, 128:256], False, False, r=[Uf.b, d["N0RB"].b], w=[b])
                    mm(st, o, d["VBK"][:, 0:64], d["AKRK"][:, 128:256], False, True, r=[d["VBK"].b, d["AKRK"].b], w=[b])
                    cp(st, "act", YS[:, cs_], o, r=[b], w=[YS.b])
                    o, b = P_(d, "p_h", 64)
                    mm(st, o, d["VBK"][:, 64:128], Uf[:, :], True, False, r=[d["VBK"].b, Uf.b], w=[b])
                    mm(st, o, d["VBK"][:, 128:192], d["VBK"][:, 0:64], False, True, r=[d["VBK"].b], w=[b])
                    stt(st, "dve", Hn[:, :], H[:, :], d["GL"][:, ch:ch + 1], o, ALU.mult, ALU.add,
                        r=[H.b, d["GL"].b, b], w=[Hn.b])
                    hs[h]["hi"] = 1 - hs[h]["hi"]
            for gi, h in enumerate(heads):
                AR, BK, HV, YS = cur[h]
                st.dma("act", kb.d["YS"][h * 64:h * 64 + 64, t0:t0 + 256], YS[:, :], YS.b, r=[YS.b], w=[kb.bufs["YS"]],
                       disjoint=True)
    st.finish()


def stage_rwkv_post(kb, l):
    S = kb.S
    st = kb.stage(f"D{l}")
    k = load_consts(st, kb)
    cols = load_cols(st, kb, l)
    bones = k[:, K_BONES:K_BONES + 128]
    yr, rr, gr = st.ring("y", 2, [128, 512]), st.ring("rkv", 2, [128, 512]), st.ring("g", 2, [128, 512])
    y2r, mr, vr, ycr = (st.ring(n, 1, [128, 512]) for n in ("y2", "mean", "var", "yc"))
    outr = st.ring("out", 2, [128, 512], BF16)
    psr = st.ring("ps", 4, [128, 512], psum=True)
    for tb in range(S // 512):
        t0 = tb * 512
        for c in range(8):
            rs = slice(c * 128, (c + 1) * 128)
            y, rk, g = yr.next(), rr.next(), gr.next()
            st.dma("sp", y[:, :], kb.d["YS"][rs, t0:t0 + 512], y.b, r=[kb.bufs["YS"]], w=[y.b])
            st.dma("sp", rk[:, :], kb.d["RKV"][rs, t0:t0 + 512], rk.b, r=[kb.bufs["RKV"]], w=[rk.b])
            st.dma("sp", g[:, :], kb.d["G"][rs, t0:t0 + 512], g.b, r=[kb.bufs["G"]], w=[g.b])
            y2, mean, var, yc = y2r.next(), mr.next(), vr.next(), ycr.next()
            tt(st, "pool", y2[:, :], y[:, :], y[:, :], ALU.mult, r=[y.b], w=[y2.b])
            p1, p2 = psr.next(), psr.next()
            mm(st, p1[:, :], bones, y[:, :], True, True, r=[k.b, y.b], w=[p1.b])
            mm(st, p2[:, :], bones, y2[:, :], True, True, r=[k.b, y2.b], w=[p2.b])
            st.op("act", lambda e, o=mean, i=p1: e.mul(out=o[:, :], in_=i[:, :], mul=1.0 / 64), r=[p1.b], w=[mean.b])
            tt(st, "pool", y2[:, :], mean[:, :], mean[:, :], ALU.mult, r=[mean.b], w=[y2.b])
            stt(st, "dve", var[:, :], p2[:, :], 1.0 / 64, y2[:, :], ALU.mult, ALU.subtract, r=[p2.b, y2.b], w=[var.b])
            ts(st, "dve", var[:, :], var[:, :], 64e-5, ALU.add, r=[var.b], w=[var.b])
            act(st, var[:, :], var[:, :], AF.Sqrt, r=[var.b], w=[var.b])
            st.op("dve", lambda e, o=var: e.reciprocal(out=o[:, :], in_=o[:, :]), r=[var.b], w=[var.b])
            tt(st, "pool", yc[:, :], y[:, :], mean[:, :], ALU.subtract, r=[y.b, mean.b], w=[yc.b])
            tt(st, "dve", yc[:, :], yc[:, :], var[:, :], ALU.mult, r=[yc.b, var.b], w=[yc.b])
            ts(st, "dve", yc[:, :], yc[:, :], colap(cols, "gn_g", c), ALU.mult, r=[yc.b, cols.b], w=[yc.b],
               s2=colap(cols, "gn_b", c), op1=ALU.add)
            tt(st, "pool", yc[:, :], yc[:, :], rk[:, :], ALU.add, r=[yc.b, rk.b], w=[yc.b])
            o = outr.next()
            tt(st, "dve", o[:, :], yc[:, :], g[:, :], ALU.mult, r=[yc.b, g.b], w=[o.b])
            st.dma("act", kb.d["YA"][rs, t0:t0 + 512], o[:, :], o.b, r=[o.b], w=[kb.bufs["YA"]], disjoint=True)
    st.finish()


def declare(kb, moe=True):
    S, L = kb.S, kb.L
    kb.din("xT", [D, S])
    kb.din("konst", [128, NCONST])
    kb.din("w_in", [L, D, NIN])
    kb.din("w_in_vres", [max(L - 1, 1), D, 32])
    kb.din("rwkv_w2", [L, 64, C])
    kb.din("rwkv_a2", [L, 64, C])
    kb.din("rwkv_v2", [max(L - 1, 1), 32, C])
    kb.din("rwkv_g2", [L, 160, C])
    kb.din("w_branch", [L, 3, C, D])
    kb.din("w_out", [L, D, D])
    kb.din("router_w", [L, D, NE])
    if moe:
        kb.din("exp_w_gu", [L, NE, D, 2 * DFF])
        kb.din("exp_w_down", [L, NE, DFF, D])
    kb.din("exp_b_down", [L, NE, D])
    for l in range(L):
        kb.din(f"cols{l}", [128, NCOLS])
        kb.din(f"lrubd{l}", [8, 128, 256])
        kb.din(f"bgu{l}", [128, NE * 16])
        kb.din(f"ln2bc{l}", [128, 2 * D])
        kb.din(f"rbbc{l}", [128, NE])
    kb.dscr("P", [NIN + 32, S])
    for nm in ("G", "VF", "SV", "RKV", "SR", "SA", "SB", "SK", "SBH", "SKH", "YS", "CC"):
        kb.dscr(nm, [C, S])
    kb.dscr("GL", [C, S // 128])
    for nm in ("YA", "YB", "YC"):
        kb.dscr(nm, [C, S], BF16)
    kb.dscr("X1T", [D, S])
    kb.dscr("X2T", [D, S])
    kb.dscr("GT", [S, NE])
    kb.dout("out", [S, D])


def make_inputs(inp, L, moe=True):
    m = {"konst": host_consts()}
    for nm in ("w_in", "rwkv_w2", "rwkv_a2", "rwkv_g2", "w_branch", "w_out", "router_w", "exp_b_down") + (
            ("exp_w_gu", "exp_w_down") if moe else ()):
        m[nm] = np.ascontiguousarray(inp[nm][:L], dtype=np.float32)
    for nm in ("w_in_vres", "rwkv_v2"):
        m[nm] = np.ascontiguousarray(inp[nm][:max(L - 1, 1)], dtype=np.float32)
    for l in range(L):
        for k_, v in host_layout(inp, l).items():
            m[f"{k_}{l}"] = np.ascontiguousarray(v, dtype=np.float32)
    return m


def stage_lru(kb, l):
    S = kb.S
    st = kb.stage(f"E{l}")
    cols = load_cols(st, kb, l)
    TB = min(1024, S)
    P, Pb = kb.d["P"], kb.bufs["P"]
    bd = st.sb("bd", [128, 8, 256])
    st.dma("sp", bd[:, :, :], kb.d[f"lrubd{l}"].rearrange("c p n -> p c n"), bd.b, r=[kb.bufs[f"lrubd{l}"]], w=[bd.b])
    csp = st.sb("csp", [128, 8])
    o, n = COLT["l_lam"]
    act(st, csp[:, :], cols[:, o:o + 8], AF.Exp, r=[cols.b], w=[csp.b], scale=-1.0)
    act(st, csp[:, :], csp[:, :], AF.Ln, r=[csp.b], w=[csp.b], bias=1.0)
    ts(st, "dve", csp[:, :], csp[:, :], -8.0, ALU.mult, r=[csp.b], w=[csp.b])
    lxr = st.ring("lx", 2, [128, TB + 3])
    lgr = st.ring("lg", 2, [128, TB])
    names = ["xc", "rg", "ig", "a", "t1", "b", "h", "ge"]
    tl = {n_: st.ring(n_, 1, [128, TB]) for n_ in names}
    outr = st.ring("out", 2, [128, TB], BF16)
    hc = st.sb("hcarry", [128, 8])
    st.op("pool", lambda e: e.memset(hc[:, :], 0.0), r=[], w=[hc.b])
    psr = st.ring("ps", 4, [128, 512], psum=True)
    for tb in range(S // TB):
        t0 = tb * TB
        for c in range(8):
            rs = slice(c * 128, (c + 1) * 128)
            lx, lg = lxr.next(), lgr.next()
            load_halo(st, lx, P[O_LX + c * 128:O_LX + (c + 1) * 128, :], 128, t0, TB, 3, Pb)
            st.dma("sp", lg[:, :], P[O_LG + c * 128:O_LG + (c + 1) * 128, t0:t0 + TB], lg.b, r=[Pb], w=[lg.b])
            xc, rg, ig, a, t1, b, h, ge = (tl[n_].next() for n_ in names)
            wo = COLT["lconv_w"][0] + c * 4
            ts(st, "dve", xc[:, :], lx[:, 3:3 + TB], cols[:, wo + 3:wo + 4], ALU.mult, r=[lx.b, cols.b], w=[xc.b],
               s2=colap(cols, "lconv_b", c), op1=ALU.add)
            for j in range(1, 4):
                stt(st, "dve", xc[:, :], lx[:, 3 - j:3 - j + TB], cols[:, wo + 3 - j:wo + 4 - j], xc[:, :], ALU.mult,
                    ALU.add, r=[lx.b, cols.b, xc.b], w=[xc.b])
            for hb in range(TB // 512):
                hs_ = slice(hb * 512, (hb + 1) * 512)
                p1, p2 = psr.next(), psr.next()
                mm(st, p1[:, :], bd[:, c, 0:128], xc[:, hs_], True, True, r=[bd.b, xc.b], w=[p1.b])
                mm(st, p2[:, :], bd[:, c, 128:256], xc[:, hs_], True, True, r=[bd.b, xc.b], w=[p2.b])
                act(st, rg[:, hs_], p1[:, :], AF.Sigmoid, r=[p1.b, cols.b], w=[rg.b], bias=colap(cols, "l_ba", c))
                act(st, ig[:, hs_], p2[:, :], AF.Sigmoid, r=[p2.b, cols.b], w=[ig.b], bias=colap(cols, "l_bx", c))
            act(st, a[:, :], rg[:, :], AF.Exp, r=[rg.b, csp.b], w=[a.b], scale=csp[:, c:c + 1])
            tt(st, "pool", t1[:, :], a[:, :], a[:, :], ALU.mult, r=[a.b], w=[t1.b])
            ts(st, "dve", t1[:, :], t1[:, :], -1.0, ALU.mult, r=[t1.b], w=[t1.b], s2=1.0, op1=ALU.add)
            ts(st, "dve", t1[:, :], t1[:, :], 0.0, ALU.max, r=[t1.b], w=[t1.b])
            act(st, t1[:, :], t1[:, :], AF.Sqrt, r=[t1.b], w=[t1.b])
            tt(st, "pool", b[:, :], ig[:, :], xc[:, :], ALU.mult, r=[ig.b, xc.b], w=[b.b])
            tt(st, "pool", b[:, :], b[:, :], t1[:, :], ALU.mult, r=[b.b, t1.b], w=[b.b])
            st.op("dve", lambda e, h=h, a=a, b=b, c=c: e.tensor_tensor_scan(
                out=h[:, :], data0=a[:, :], data1=b[:, :], initial=hc[:, c:c + 1], op0=ALU.mult, op1=ALU.add),
                r=[a.b, b.b, hc.b], w=[h.b])
            cp(st, "dve", hc[:, c:c + 1], h[:, TB - 1:TB], r=[h.b], w=[hc.b])
            tt(st, "pool", ge[:, :], lg[:, :], lg[:, :], ALU.mult, r=[lg.b], w=[ge.b])
            ts(st, "dve", ge[:, :], ge[:, :], 0.044715, ALU.mult, r=[ge.b], w=[ge.b], s2=1.0, op1=ALU.add)
            tt(st, "pool", ge[:, :], ge[:, :], lg[:, :], ALU.mult, r=[ge.b, lg.b], w=[ge.b])
            act(st, ge[:, :], ge[:, :], AF.Sigmoid, r=[ge.b], w=[ge.b], scale=1.5957691216057308)
            tt(st, "pool", ge[:, :], ge[:, :], lg[:, :], ALU.mult, r=[ge.b, lg.b], w=[ge.b])
            o_ = outr.next()
            tt(st, "dve", o_[:, :], ge[:, :], h[:, :], ALU.mult, r=[ge.b, h.b], w=[o_.b])
            st.dma("act", kb.d["YB"][rs, t0:t0 + TB], o_[:, :], o_.b, r=[o_.b], w=[kb.bufs["YB"]], disjoint=True)
    st.finish()


def stage_conf(kb, l):
    S = kb.S
    st = kb.stage(f"F{l}")
    k = load_consts(st, kb)
    cols = load_cols(st, kb, l)
    ones = k[:, K_ONES:K_ONES + 128]
    P, Pb = kb.d["P"], kb.bufs["P"]
    TB = min(1024, S)
    HL = 30
    vr = st.ring("val", 2, [128, TB + HL])
    gr = st.ring("gate", 2, [128, TB + HL])
    accr = st.ring("acc", 2, [128, TB])
    acc2r = st.ring("acc2", 2, [128, TB])
    tmpr = st.ring("tmp", 3, [128, TB])
    for tb in range(S // TB):
        t0 = tb * TB
        for c in range(8):
            rs = slice(c * 128, (c + 1) * 128)
            v, g = vr.next(), gr.next()
            load_halo(st, v, P[O_CU + c * 128:O_CU + (c + 1) * 128, :], 128, t0, TB, HL, Pb)
            load_halo(st, g, P[O_CU + C + c * 128:O_CU + C + (c + 1) * 128, :], 128, t0, TB, HL, Pb)
            act(st, g[:, :], g[:, :], AF.Sigmoid, r=[g.b], w=[g.b])
            tt(st, "pool", v[:, :], v[:, :], g[:, :], ALU.mult, r=[v.b, g.b], w=[v.b])
            acc, acc2 = accr.next(), acc2r.next()
            wo = COLT["cconv_w"][0] + c * 31
            ts(st, "dve", acc[:, :], v[:, HL:HL + TB], cols[:, wo + 30:wo + 31], ALU.mult, r=[v.b, cols.b], w=[acc.b],
               s2=colap(cols, "cconv_b", c), op1=ALU.add)
            for kk_ in range(29, 14, -1):
                sh = 30 - kk_
                stt(st, "dve", acc[:, :], v[:, HL - sh:HL - sh + TB], cols[:, wo + kk_:wo + kk_ + 1], acc[:, :],
                    ALU.mult, ALU.add, r=[v.b, cols.b, acc.b], w=[acc.b])
            act(st, acc2[:, :], v[:, 0:TB], AF.Copy, r=[v.b, cols.b], w=[acc2.b], scale=cols[:, wo:wo + 1])
            for kk_ in range(1, 15):
                sh = 30 - kk_
                tmp = tmpr.next()
                act(st, tmp[:, :], v[:, HL - sh:HL - sh + TB], AF.Copy, r=[v.b, cols.b], w=[tmp.b],
                    scale=cols[:, wo + kk_:wo + kk_ + 1])
                tt(st, "pool", acc2[:, :], acc2[:, :], tmp[:, :], ALU.add, r=[acc2.b, tmp.b], w=[acc2.b])
            tt(st, "dve", acc[:, :], acc[:, :], acc2[:, :], ALU.add, r=[acc.b, acc2.b], w=[acc.b])
            st.dma("act", kb.d["CC"][rs, t0:t0 + TB], acc[:, :], acc.b, r=[acc.b], w=[kb.bufs["CC"]], disjoint=True)
    st.finish()
    st = kb.stage(f"F2{l}")
    k = load_consts(st, kb)
    cols = load_cols(st, kb, l)
    ones = k[:, K_ONES:K_ONES + 128]
    ccr = st.ring("cc", 2, [128, 8, 512])
    sqr = st.ring("sq", 2, [128, 512])
    mean, var, tmp = st.sb("mean", [128, 512]), st.sb("var", [128, 512]), st.sb("tmp", [128, 512])
    ycr = st.ring("yc", 2, [128, 512])
    sgr = st.ring("sg", 2, [128, 512])
    outr = st.ring("out", 2, [128, 8, 512], BF16)
    ps1, ps2 = st.ring("ps1", 2, [128, 512], psum=True), st.ring("ps2", 2, [128, 512], psum=True)
    for tb in range(S // 512):
        t0 = tb * 512
        cc = ccr.next()
        st.dma("sp", cc[:, :, :], kb.d["CC"][:, t0:t0 + 512].rearrange("(c p) t -> p c t", p=128), cc.b,
               r=[kb.bufs["CC"]], w=[cc.b])
        p1, p2 = ps1.next(), ps2.next()
        for c in range(8):
            sq = sqr.next()
            tt(st, "pool", sq[:, :], cc[:, c, :], cc[:, c, :], ALU.mult, r=[cc.b], w=[sq.b])
            mm(st, p1[:, :], ones, cc[:, c, :], c == 0, c == 7, r=[k.b, cc.b], w=[p1.b])
            mm(st, p2[:, :], ones, sq[:, :], c == 0, c == 7, r=[k.b, sq.b], w=[p2.b])
        st.op("act", lambda e, i=p1: e.mul(out=mean[:, :], in_=i[:, :], mul=1.0 / C), r=[p1.b], w=[mean.b])
        tt(st, "pool", tmp[:, :], mean[:, :], mean[:, :], ALU.mult, r=[mean.b], w=[tmp.b])
        stt(st, "dve", var[:, :], p2[:, :], 1.0 / C, tmp[:, :], ALU.mult, ALU.subtract, r=[p2.b, tmp.b], w=[var.b])
        ts(st, "dve", var[:, :], var[:, :], 1e-5, ALU.add, r=[var.b], w=[var.b])
        act(st, var[:, :], var[:, :], AF.Sqrt, r=[var.b], w=[var.b])
        st.op("dve", lambda e: e.reciprocal(out=var[:, :], in_=var[:, :]), r=[var.b], w=[var.b])
        o_ = outr.next()
        for c in range(8):
            yc, sg = ycr.next(), sgr.next()
            tt(st, "pool", yc[:, :], cc[:, c, :], mean[:, :], ALU.subtract, r=[cc.b, mean.b], w=[yc.b])
            tt(st, "dve", yc[:, :], yc[:, :], var[:, :], ALU.mult, r=[yc.b, var.b], w=[yc.b])
            ts(st, "dve", yc[:, :], yc[:, :], colap(cols, "cln_g", c), ALU.mult, r=[yc.b, cols.b], w=[yc.b],
               s2=colap(cols, "cln_b", c), op1=ALU.add)
            act(st, sg[:, :], yc[:, :], AF.Sigmoid, r=[yc.b], w=[sg.b])
            tt(st, "pool", o_[:, c, :], yc[:, :], sg[:, :], ALU.mult, r=[yc.b, sg.b], w=[o_.b])
        st.dma("act", kb.d["YC"][:, t0:t0 + 512].rearrange("(c p) t -> p c t", p=128), o_[:, :, :], o_.b, r=[o_.b],
               w=[kb.bufs["YC"]], disjoint=True)
    st.finish()


def stage_merge(kb, l, xsrc):
    S = kb.S
    st = kb.stage(f"G{l}")
    k = load_consts(st, kb)
    cols = load_cols(st, kb, l)
    ones = k[:, K_ONES:K_ONES + 128]
    P, Pb = kb.d["P"], kb.bufs["P"]
    ybf = [st.ring(f"y{b}", 1, [128, 8, 512], BF16) for b in range(3)]
    wstB = st.ring("wstB", 2, [128, 8, 256])
    wbfB = st.ring("wbfB", 4, [128, 8, 256], BF16)
    wstO = st.ring("wstO", 2, [128, 16, 128])
    wbfO = st.ring("wbfO", 2, [128, 16, 128], BF16)
    mixed = st.sb("mixed", [128, 16, 512], BF16, nsub=16)
    res = st.sb("res", [128, 16, 512], F32, nsub=16)
    gts = st.ring("gt", 3, [128, 512])
    mts = st.ring("mt", 3, [128, 512])
    xts = st.ring("xt", 2, [128, 512])
    sqr = st.ring("sq", 2, [128, 512])
    mean, var, tmp = st.sb("mean", [128, 512]), st.sb("var", [128, 512]), st.sb("tmp", [128, 512])
    outr = st.ring("out", 3, [128, 512])
    psr = st.ring("ps", 6, [128, 512], psum=True)
    ps1, ps2 = st.ps("pstatA", [128, 512]), st.ps("pstatB", [128, 512])
    ynames = ("YA", "YB", "YC")
    xs, xsb = kb.d[xsrc], kb.bufs[xsrc]
    for tb in range(S // 512):
        t0 = tb * 512
        ys = []
        for b in range(3):
            y = ybf[b].next()
            st.dma("sp", y[:, :, :], kb.d[ynames[b]][:, t0:t0 + 512].rearrange("(c p) t -> p c t", p=128), y.b,
                   r=[kb.bufs[ynames[b]]], w=[y.b])
            ys.append(y)
        for g in range(8):
            wbs = []
            for b in range(3):
                ws = wstB.next()
                st.dma("sp", ws[:, :, :], kb.d["w_branch"][l, b][:, g * 256:(g + 1) * 256].rearrange(
                    "(kc p) c -> p kc c", p=128), ws.b, r=[kb.bufs["w_branch"]], w=[ws.b])
                wb = wbfB.next()
                cp(st, "pool", wb[:, :, :], ws[:, :, :], r=[ws.b], w=[wb.b])
                wbs.append(wb)
            for dc in range(2):
                dch = g * 2 + dc
                ms = []
                for b in range(3):
                    gt = gts.next()
                    r0 = O_MG + b * D + dch * 128
                    st.dma("sp", gt[:, :], P[r0:r0 + 128, t0:t0 + 512], gt.b, r=[Pb], w=[gt.b])
                    act(st, gt[:, :], gt[:, :], AF.Sigmoid, r=[gt.b], w=[gt.b])
                    ps = psr.next()
                    for kc in range(8):
                        mm(st, ps[:, :], wbs[b][:, kc, dc * 128:(dc + 1) * 128], ys[b][:, kc, :], kc == 0, kc == 7,
                           r=[wbs[b].b, ys[b].b], w=[ps.b])
                    mt = mts.next()
                    tt(st, "dve", mt[:, :], ps[:, :], gt[:, :], ALU.mult, r=[ps.b, gt.b], w=[mt.b])
                    ms.append(mt)
                tt(st, "pool", ms[0][:, :], ms[0][:, :], ms[1][:, :], ALU.add, r=[ms[0].b, ms[1].b], w=[ms[0].b])
                tt(st, "pool", mixed[:, dch, :], ms[0][:, :], ms[2][:, :], ALU.add, r=[ms[0].b, ms[2].b],
                   w=[mixed.sub[dch]])
        for dch in range(16):
            ws = wstO.next()
            st.dma("sp", ws[:, :, :], kb.d["w_out"][l][:, dch * 128:(dch + 1) * 128].rearrange("(kc p) c -> p kc c", p=128),
                   ws.b, r=[kb.bufs["w_out"]], w=[ws.b])
            wb = wbfO.next()
            cp(st, "pool", wb[:, :, :], ws[:, :, :], r=[ws.b], w=[wb.b])
            xt = xts.next()
            st.dma("sp", xt[:, :], xs[dch * 128:(dch + 1) * 128, t0:t0 + 512], xt.b, r=[xsb], w=[xt.b])
            ps = psr.next()
            for kc in range(16):
                mm(st, ps[:, :], wb[:, kc, :], mixed[:, kc, :], kc == 0, kc == 15, r=[wb.b, mixed.sub[kc]], w=[ps.b])
            stt(st, "dve", res[:, dch, :], xt[:, :], ALPHA, ps[:, :], ALU.mult, ALU.add, r=[xt.b, ps.b],
                w=[res.sub[dch]])
        for dch in range(16):
            sq = sqr.next()
            tt(st, "pool", sq[:, :], res[:, dch, :], res[:, dch, :], ALU.mult, r=[res.sub[dch]], w=[sq.b])
            mm(st, ps1[:, :], ones, res[:, dch, :], dch == 0, dch == 15, r=[k.b, res.sub[dch]], w=[ps1.b])
            mm(st, ps2[:, :], ones, sq[:, :], dch == 0, dch == 15, r=[k.b, sq.b], w=[ps2.b])
        st.op("act", lambda e: e.mul(out=mean[:, :], in_=ps1[:, :], mul=1.0 / D), r=[ps1.b], w=[mean.b])
        tt(st, "pool", tmp[:, :], mean[:, :], mean[:, :], ALU.mult, r=[mean.b], w=[tmp.b])
        stt(st, "dve", var[:, :], ps2[:, :], 1.0 / D, tmp[:, :], ALU.mult, ALU.subtract, r=[ps2.b, tmp.b], w=[var.b])
        ts(st, "dve", var[:, :], var[:, :], 1e-5, ALU.add, r=[var.b], w=[var.b])
        act(st, var[:, :], var[:, :], AF.Sqrt, r=[var.b], w=[var.b])
        st.op("dve", lambda e: e.reciprocal(out=var[:, :], in_=var[:, :]), r=[var.b], w=[var.b])
        for dch in range(16):
            o_ = outr.next()
            tt(st, "pool", o_[:, :], res[:, dch, :], mean[:, :], ALU.subtract, r=[res.sub[dch], mean.b], w=[o_.b])
            tt(st, "dve", o_[:, :], o_[:, :], var[:, :], ALU.mult, r=[o_.b, var.b], w=[o_.b])
            ts(st, "dve", o_[:, :], o_[:, :], colap(cols, "ln1_g", dch), ALU.mult, r=[o_.b, cols.b], w=[o_.b],
               s2=colap(cols, "ln1_b", dch), op1=ALU.add)
            st.dma("act", kb.d["X1T"][dch * 128:(dch + 1) * 128, t0:t0 + 512], o_[:, :], o_.b, r=[o_.b],
                   w=[kb.bufs["X1T"]], disjoint=True)
    st.finish()


def stage_router(kb, l):
    S = kb.S
    st = kb.stage(f"H1{l}")
    rw = st.sb("rw", [128, 16, NE])
    st.dma("sp", rw[:, :, :], kb.d["router_w"][l].rearrange("(kc p) e -> p kc e", p=128), rw.b,
           r=[kb.bufs["router_w"]], w=[rw.b])
    rb = st.sb("rb", [128, NE])
    st.dma("sp", rb[:, :], kb.d[f"rbbc{l}"][:, :], rb.b, r=[kb.bufs[f"rbbc{l}"]], w=[rb.b])
    xr = st.ring("x", 3, [128, 16, 128])
    lgr, er, mkr, gr_ = (st.ring(n_, 2, [128, NE]) for n_ in ("lg", "e", "mk", "g"))
    m8r = st.ring("m8", 2, [128, 8])
    smr = st.ring("sm", 2, [128, 2])
    psr = st.ring("ps", 4, [128, 512], psum=True)
    for ti in range(S // 128):
        t0 = ti * 128
        x = xr.next()
        st.dma("sp", x[:, :, :], kb.d["X1T"][:, t0:t0 + 128].rearrange("(c p) t -> p c t", p=128), x.b,
               r=[kb.bufs["X1T"]], w=[x.b])
        ps = psr.next()
        for kc in range(16):
            mm(st, ps[:, 0:NE], x[:, kc, :], rw[:, kc, :], kc == 0, kc == 15, r=[x.b, rw.b], w=[ps.b])
        lg, e_, mk, g, m8, sm = lgr.next(), er.next(), mkr.next(), gr_.next(), m8r.next(), smr.next()
        tt(st, "dve", lg[:, :], ps[:, 0:NE], rb[:, :], ALU.add, r=[ps.b, rb.b], w=[lg.b])
        st.op("dve", lambda e, m8=m8, lg=lg: e.max(out=m8[:, :], in_=lg[:, :]), r=[lg.b], w=[m8.b])
        ts(st, "dve", sm[:, 0:1], m8[:, 0:1], -1.0, ALU.mult, r=[m8.b], w=[sm.b])
        act(st, e_[:, :], lg[:, :], AF.Exp, r=[lg.b, sm.b], w=[e_.b], bias=sm[:, 0:1])
        ts(st, "dve", mk[:, :], lg[:, :], m8[:, 3:4], ALU.is_ge, r=[lg.b, m8.b], w=[mk.b])
        tt(st, "dve", e_[:, :], e_[:, :], mk[:, :], ALU.mult, r=[e_.b, mk.b], w=[e_.b])
        st.op("dve", lambda e, sm=sm, e_=e_: e.reduce_sum(out=sm[:, 1:2], in_=e_[:, :], axis=AX.X), r=[e_.b], w=[sm.b])
        st.op("dve", lambda e, sm=sm: e.reciprocal(out=sm[:, 1:2], in_=sm[:, 1:2]), r=[sm.b], w=[sm.b])
        ts(st, "dve", g[:, :], e_[:, :], sm[:, 1:2], ALU.mult, r=[e_.b, sm.b], w=[g.b])
        st.dma("act", kb.d["GT"][t0:t0 + 128, :], g[:, :], g.b, r=[g.b], w=[kb.bufs["GT"]], disjoint=True)
    st.finish()


def stage_moe(kb, l, last):
    S = kb.S
    st = kb.stage(f"H2{l}")
    k = load_consts(st, kb)
    ident = k[:, K_ID:K_ID + 128]
    TS = min(1024, S)
    NTL = TS // 128
    NTB = TS // 512
    x1b = st.sb("x1b", [128, 16, TS], BF16, nsub=16)
    acc = st.sb("acc", [128, NTL, D], F32, nsub=NTL)
    hbf = st.sb("h", [128, 8, TS], BF16, nsub=8)
    wst = st.ring("wst", 4, [128, 2048])
    wbf = st.ring("wbf", 2, [128, 8192], BF16)
    gates = st.sb("gates", [128, NTL, NE])
    gT = st.sb("gT", [32, TS])
    bgu = st.sb("bgu", [128, NE * 16])
    st.dma("sp", bgu[:, :], kb.d[f"bgu{l}"][:, :], bgu.b, r=[kb.bufs[f"bgu{l}"]], w=[bgu.b])
    gpr, sgr, upr = (st.ring(n_, 1, [128, 512]) for n_ in ("gp", "sg", "up"))
    stat = st.sb("stat", [128, 4, 6])
    mv = st.sb("mv", [128, 2])
    stg = None if last else st.ring("stg", 1, [128, 16, 128])
    psg = st.ring("psg", 2, [128, 512], psum=True)
    psu = st.ring("psu", 2, [128, 512], psum=True)
    psd = st.ring("psd", 2, [128, 512], psum=True)
    pst = st.ring("pst", 2, [128, 512], psum=True)
    Wgu, Wgub = kb.d["exp_w_gu"], kb.bufs["exp_w_gu"]
    Wd, Wdb = kb.d["exp_w_down"], kb.bufs["exp_w_down"]
    for sbi in range(S // TS):
        t0 = sbi * TS
        for kc4 in range(4):
            xts = []
            for q in range(4):
                kc = kc4 * 4 + q
                xt = wst.next()
                st.dma("sp", xt[:, 0:TS], kb.d["X1T"][kc * 128:(kc + 1) * 128, t0:t0 + TS], xt.b, r=[kb.bufs["X1T"]],
                       w=[xt.b])
                cp(st, "act" if kc % 2 else "dve", x1b[:, kc, :], xt[:, 0:TS], r=[xt.b], w=[x1b.sub[kc]])
                for tl_ in range(NTL):
                    pt = pst.next()
                    tr(st, pt[:, 0:128], xt[:, tl_ * 128:(tl_ + 1) * 128], ident, r=[xt.b, k.b], w=[pt.b])
                    st.op("act", lambda e, pt=pt, tl_=tl_, kc=kc: e.mul(out=acc[:, tl_, kc * 128:(kc + 1) * 128],
                                                                       in_=pt[:, 0:128], mul=ALPHA),
                          r=[pt.b], w=[acc.sub[tl_]])
        st.dma("sp", gates[:, :, :], kb.d["GT"][t0:t0 + TS, :].rearrange("(a p) e -> p a e", p=128), gates.b,
               r=[kb.bufs["GT"]], w=[gates.b])
        for tl_ in range(NTL):
            pt = pst.next()
            tr(st, pt[0:32, 0:128], gates[:, tl_, :], ident, r=[gates.b, k.b], w=[pt.b])
            cp(st, "act", gT[:, tl_ * 128:(tl_ + 1) * 128], pt[0:32, 0:128], r=[pt.b], w=[gT.b])
        bd_ = wst.next()
        st.dma("sp", bd_[0:32, :], kb.d["exp_b_down"][l], bd_.b, r=[kb.bufs["exp_b_down"]], w=[bd_.b])
        for tl_ in range(NTL):
            for blk in range(4):
                ps = psd.next()
                mm(st, ps[:, :], gT[:, tl_ * 128:(tl_ + 1) * 128], bd_[0:32, blk * 512:(blk + 1) * 512], True, True,
                   r=[gT.b, bd_.b], w=[ps.b])
                tt(st, "dve", acc[:, tl_, blk * 512:(blk + 1) * 512], ps[:, :], acc[:, tl_, blk * 512:(blk + 1) * 512],
                   ALU.add, r=[ps.b, acc.sub[tl_]], w=[acc.sub[tl_]])
        for e_i in range(NE):
            bcol = e_i * 16
            for g4 in range(4):
                wb = wbf.next()
                wbv = wb[:, :].rearrange("p (a b) -> p a b", b=512)
                for q in range(4):
                    ws = wst.next()
                    wsv = ws[:, :].rearrange("p (a b) -> p a b", b=512)
                    rows = slice(q * 512, (q + 1) * 512)
                    st.dma("sp", wsv[:, :, 0:256],
                           Wgu[l, e_i][rows, g4 * 256:(g4 + 1) * 256].rearrange("(kc p) c -> p kc c", p=128), ws.b,
                           r=[Wgub], w=[ws.b])
                    st.dma("sp", wsv[:, :, 256:512],
                           Wgu[l, e_i][rows, DFF + g4 * 256:DFF + (g4 + 1) * 256].rearrange("(kc p) c -> p kc c", p=128),
                           ws.b, r=[Wgub], w=[ws.b], group=True)
                    cp(st, "pool", wb[:, q * 2048:(q + 1) * 2048], ws[:, :], r=[ws.b], w=[wb.b])
                for jj in range(2):
                    j = g4 * 2 + jj
                    for tb in range(NTB):
                        tsl = slice(tb * 512, (tb + 1) * 512)
                        pg, pu = psg.next(), psu.next()
                        for kc in range(16):
                            mm(st, pg[:, :], wbv[:, kc, jj * 128:(jj + 1) * 128], x1b[:, kc, tsl], kc == 0, kc == 15,
                               r=[wb.b, x1b.sub[kc]], w=[pg.b])
                        for kc in range(16):
                            mm(st, pu[:, :], wbv[:, kc, 256 + jj * 128:256 + (jj + 1) * 128], x1b[:, kc, tsl], kc == 0,
                               kc == 15, r=[wb.b, x1b.sub[kc]], w=[pu.b])
                        gp, sg, up = gpr.next(), sgr.next(), upr.next()
                        ts(st, "dve", gp[:, :], pg[:, :], bgu[:, bcol + j:bcol + j + 1], ALU.add, r=[pg.b, bgu.b],
                           w=[gp.b], s2=7.0, op1=ALU.min)
                        act(st, sg[:, :], gp[:, :], AF.Sigmoid, r=[gp.b], w=[sg.b], scale=1.702)
                        ts(st, "dve", up[:, :], pu[:, :], bgu[:, bcol + 8 + j:bcol + 9 + j], ALU.add, r=[pu.b, bgu.b],
                           w=[up.b], s2=7.0, op1=ALU.min)
                        ts(st, "pool", up[:, :], up[:, :], -7.0, ALU.max, r=[up.b], w=[up.b], s2=1.0, op1=ALU.add)
                        tt(st, "pool", gp[:, :], gp[:, :], sg[:, :], ALU.mult, r=[gp.b, sg.b], w=[gp.b])
                        tt(st, "pool", hbf[:, j, tsl], up[:, :], gp[:, :], ALU.mult, r=[up.b, gp.b], w=[hbf.sub[j]])
            for dg in range(2):
                wb = wbf.next()
                wbv = wb[:, :].rearrange("p (a b) -> p a b", b=1024)
                for q in range(4):
                    ws = wst.next()
                    st.dma("sp", ws[:, :].rearrange("p (a b) -> p a b", b=1024),
                           Wd[l, e_i][q * 256:(q + 1) * 256, dg * 1024:(dg + 1) * 1024].rearrange(
                               "(kc p) c -> p kc c", p=128), ws.b, r=[Wdb], w=[ws.b])
                    cp(st, "pool", wb[:, q * 2048:(q + 1) * 2048], ws[:, :], r=[ws.b], w=[wb.b])
                for tl_ in range(NTL):
                    for hh in range(2):
                        c0 = dg * 1024 + hh * 512
                        ps = psd.next()
                        for kc in range(8):
                            mm(st, ps[:, :], hbf[:, kc, tl_ * 128:(tl_ + 1) * 128], wbv[:, kc, hh * 512:(hh + 1) * 512],
                               kc == 0, kc == 7, r=[hbf.sub[kc], wb.b], w=[ps.b])
                        stt(st, "dve", acc[:, tl_, c0:c0 + 512], ps[:, :], gates[:, tl_, e_i:e_i + 1],
                            acc[:, tl_, c0:c0 + 512], ALU.mult, ALU.add, r=[ps.b, gates.b, acc.sub[tl_]],
                            w=[acc.sub[tl_]])
        lg_ = wst.next()
        lb_ = wst.next()
        st.dma("sp", lg_[:, :], kb.d[f"ln2bc{l}"][:, 0:D], lg_.b, r=[kb.bufs[f"ln2bc{l}"]], w=[lg_.b])
        st.dma("sp", lb_[:, :], kb.d[f"ln2bc{l}"][:, D:2 * D], lb_.b, r=[kb.bufs[f"ln2bc{l}"]], w=[lb_.b])
        for tl_ in range(NTL):
            a_ = acc[:, tl_, :]
            ab = acc.sub[tl_]
            for i in range(4):
                st.op("dve", lambda e, i=i, tl_=tl_: e.bn_stats(out=stat[:, i, :], in_=acc[:, tl_, i * 512:(i + 1) * 512]),
                      r=[ab], w=[stat.b])
            st.op("dve", lambda e: e.bn_aggr(out=mv[:, :], in_=stat[:, :, :].rearrange("p a b -> p (a b)")),
                  r=[stat.b], w=[mv.b])
            ts(st, "dve", mv[:, 1:2], mv[:, 1:2], 1e-5, ALU.add, r=[mv.b], w=[mv.b])
            act(st, mv[:, 1:2], mv[:, 1:2], AF.Sqrt, r=[mv.b], w=[mv.b])
            st.op("dve", lambda e: e.reciprocal(out=mv[:, 1:2], in_=mv[:, 1:2]), r=[mv.b], w=[mv.b])
            ts(st, "dve", a_, a_, mv[:, 0:1], ALU.subtract, r=[ab, mv.b], w=[ab], s2=mv[:, 1:2], op1=ALU.mult)
            tt(st, "pool", a_, a_, lg_[:, :], ALU.mult, r=[ab, lg_.b], w=[ab])
            tt(st, "pool", a_, a_, lb_[:, :], ALU.add, r=[ab, lb_.b], w=[ab])
            if last:
                st.dma("act", kb.d["out"][t0 + tl_ * 128:t0 + (tl_ + 1) * 128, :], a_, ab, r=[ab],
                       w=[kb.bufs["out"]], disjoint=True)
            else:
                sg_ = stg.next()
                for kc4 in range(4):
                    pt = pst.next()
                    for q in range(4):
                        kc = kc4 * 4 + q
                        tr(st, pt[:, q * 128:(q + 1) * 128], acc[:, tl_, kc * 128:(kc + 1) * 128], ident, r=[ab, k.b],
                           w=[pt.b])
                    cp(st, "act", sg_[:, kc4 * 4:(kc4 + 1) * 4, :], pt[:, :].rearrange("p (a b) -> p a b", b=128),
                       r=[pt.b], w=[sg_.b])
                tsl = slice(t0 + tl_ * 128, t0 + (tl_ + 1) * 128)
                st.dma("act", kb.d["X2T"][:, tsl].rearrange("(c p) t -> p c t", p=128), sg_[:, :, :], sg_.b, r=[sg_.b],
                       w=[kb.bufs["X2T"]], disjoint=True)
    st.finish()


def build_program(S, L, debug=False, moe=True):
    kb = KB(S, L, debug=debug)
    declare(kb, moe=moe)
    src = "xT"
    for l in range(L):
        stage_inproj(kb, l, src)
        stage_rwkv_prep(kb, l)
        stage_rwkv_scan(kb, l)
        stage_rwkv_post(kb, l)
        stage_lru(kb, l)
        stage_conf(kb, l)
        stage_merge(kb, l, src)
        stage_router(kb, l)
        if moe:
            stage_moe(kb, l, last=(l == L - 1))
        src = "X2T"
    return kb


def kernel(**inputs):
    x = np.asarray(inputs["x"], np.float32)
    B, S, _ = x.shape
    L = inputs["w_in"].shape[0]
    kb = build_program(S, L)
    shared = {"d_" + k_: v for k_, v in make_inputs(inputs, L).items()}
    in_maps = []
    for b in range(B):
        m = dict(shared)
        m["d_xT"] = np.ascontiguousarray(x[b].T)
        in_maps.append(m)
    res = run_bass_kernel_spmd(kb.nc, in_maps, core_ids=list(range(B)))
    return np.stack([np.asarray(r["d_out"], np.float32) for r in res.results], axis=0)
```

```python
import contextlib
import math
import numpy as np
import concourse.bass as bass
import concourse.mybir as mybir
from concourse.bass_utils import run_bass_kernel_spmd

F32 = mybir.dt.float32
BF16 = mybir.dt.bfloat16
AF = mybir.ActivationFunctionType
ALU = mybir.AluOpType
AX = mybir.AxisListType
ENG = ("pe", "act", "dve", "pool", "sp")

D = 2048
C = 1024
NIN = 13600
NE = 32
DFF = 1024
KAPPA = math.exp(-0.5)
ALPHA = 4.0 ** 0.25
O_R, O_K, O_V, O_WL, O_AL, O_GL, O_LG, O_LX, O_CU, O_MG, O_VR = 0, 1024, 2048, 3072, 3136, 3200, 3360, 4384, 5408, 7456, 13600


class Buf:
    __slots__ = ("name", "we", "wd", "re", "rd", "sem", "cnt", "last")

    def __init__(self, name):
        self.name = name
        self.reset()

    def reset(self):
        self.we = {}
        self.wd = {}
        self.re = {}
        self.rd = {}
        self.sem = None
        self.cnt = 0
        self.last = None


class T:
    def __init__(self, t, name, nsub=0):
        self.t = t
        self.b = Buf(name)
        self.sub = [Buf(f"{name}.{i}") for i in range(nsub)]

    def __getitem__(self, k):
        return self.t[k]


class Ring:
    def __init__(self, items):
        self.items = items
        self.i = 0

    def next(self):
        r = self.items[self.i % len(self.items)]
        self.i += 1
        return r


class Stage:
    def __init__(self, kb, name):
        self.kb = kb
        self.nc = kb.nc
        self.name = name
        self.es = contextlib.ExitStack()
        self.streams = {e: [] for e in ENG}
        self.count = {e: 0 for e in ENG}
        self.waited = {e: {} for e in ENG}
        self.sems = []
        self.touched = []
        self.esem = {e: self._newsem(e) for e in ENG if e != "sp"}
        self.alt = 0

    def _newsem(self, nm):
        s = self.es.enter_context(self.nc.semaphore(f"{self.name}_{nm}_{len(self.sems)}"))
        self.sems.append(s)
        return len(self.sems) - 1

    def sb(self, name, shape, dt=F32, nsub=0):
        t = self.es.enter_context(self.nc.sbuf_tensor(f"{self.name}_{name}", list(shape), dt))
        return T(t, name, nsub)

    def ps(self, name, shape, dt=F32):
        t = self.es.enter_context(self.nc.psum_tensor(f"{self.name}_{name}", list(shape), dt))
        return T(t, name)

    def ring(self, name, n, shape, dt=F32, psum=False):
        mk = self.ps if psum else self.sb
        return Ring([mk(f"{name}{i}", shape, dt) for i in range(n)])

    def _touch(self, b):
        self.touched.append(b)

    def _deps(self, eng, reads, writes, disjoint=False):
        need = {}

        def add_e(d):
            for e2, n in d.items():
                if e2 == eng and eng == "pe":
                    continue
                s = self.esem[e2]
                if need.get(s, 0) < n:
                    need[s] = n

        def add_d(d):
            for s, v in d.items():
                if need.get(s, 0) < v:
                    need[s] = v

        for b in reads:
            add_e(b.we)
            add_d(b.wd)
        for b in writes:
            if not disjoint:
                add_e(b.we)
                add_d(b.wd)
            add_e(b.re)
            add_d(b.rd)
        out = []
        wt = self.waited[eng]
        for s, v in need.items():
            if wt.get(s, 0) >= v:
                continue
            wt[s] = v
            out.append((s, v))
        return out

    def op(self, eng, fn, r=(), w=()):
        waits = self._deps(eng, r, w)
        self.count[eng] += 1
        n = self.count[eng]
        si = self.esem[eng]
        sems = self.sems

        def run(e, fn=fn, waits=waits, si=si):
            for s, v in waits:
                e.wait_ge(sems[s], v)
            fn(e).then_inc(sems[si], 1)

        self.streams[eng].append(run)
        for b in r:
            b.re[eng] = n
            self._touch(b)
        for b in w:
            b.we = {eng: n}
            b.wd = {}
            b.re = {}
            b.rd = {}
            self._touch(b)

    def dma(self, q, out, in_, owner, r=(), w=(), disjoint=False, kw=None, group=False):
        disjoint = disjoint or group
        waits = self._deps(q, r, w, disjoint=disjoint)
        if owner.sem is None:
            owner.sem = self._newsem("d")
            owner.cnt = 0
            self._touch(owner)
        if owner.last is not None and not group:
            s, v = owner.last
            if self.waited[q].get(s, 0) < v:
                self.waited[q][s] = v
                waits.append((s, v))
        owner.cnt += 16
        ev = (owner.sem, owner.cnt)
        owner.last = ev
        sems = self.sems
        kw = kw or {}

        def run(e, waits=waits, ev=ev, out=out, in_=in_):
            for s, v in waits:
                e.wait_ge(sems[s], v)
            e.dma_start(out=out, in_=in_, **kw).then_inc(sems[ev[0]], 16)

        self.streams[q].append(run)
        for b in r:
            b.rd[ev[0]] = ev[1]
            self._touch(b)
        for b in w:
            if disjoint:
                b.wd[ev[0]] = ev[1]
            else:
                b.we = {}
                b.wd = {ev[0]: ev[1]}
                b.re = {}
                b.rd = {}
            self._touch(b)

    def aeng(self, choices=("dve", "pool")):
        self.alt += 1
        return choices[self.alt % len(choices)]

    def finish(self):
        nc = self.nc
        sems = self.sems
        finals = []
        seen = set()
        for b in self.touched:
            if b.sem is not None and b.sem not in seen:
                seen.add(b.sem)
                finals.append((b.sem, b.cnt))
        for e in ENG:
            if e != "sp" and self.count[e] > 0:
                finals.append((self.esem[e], self.count[e]))

        def fin(e):
            for s, v in finals:
                e.wait_ge(sems[s], v)

        self.streams["sp"].append(fin)
        streams = self.streams
        with nc.Block(self.name) as block:
            @block.sync
            def _(e):
                for f in streams["sp"]:
                    f(e)

            @block.tensor
            def _(e):
                for f in streams["pe"]:
                    f(e)

            @block.scalar
            def _(e):
                for f in streams["act"]:
                    f(e)

            @block.vector
            def _(e):
                for f in streams["dve"]:
                    f(e)

            @block.gpsimd
            def _(e):
                for f in streams["pool"]:
                    f(e)
        with nc.Block(self.name + "_clr") as blk:
            @blk.gpsimd
            def _(e):
                for s_ in sems:
                    e.sem_clear(s_)
        for b in self.touched:
            b.reset()
        self.es.close()


def tt(st, eng, out, in0, in1, op, r, w):
    st.op(eng, lambda e: e.tensor_tensor(out=out, in0=in0, in1=in1, op=op), r=r, w=w)


def ts(st, eng, out, in0, s1, op0, r, w, s2=None, op1=None):
    if op1 is None:
        st.op(eng, lambda e: e.tensor_scalar(out=out, in0=in0, scalar1=s1, scalar2=None, op0=op0), r=r, w=w)
    else:
        st.op(eng, lambda e: e.tensor_scalar(out=out, in0=in0, scalar1=s1, scalar2=s2, op0=op0, op1=op1), r=r, w=w)


def stt(st, eng, out, in0, scalar, in1, op0, op1, r, w):
    st.op(eng, lambda e: e.scalar_tensor_tensor(out=out, in0=in0, scalar=scalar, in1=in1, op0=op0, op1=op1), r=r, w=w)


def act(st, out, in_, func, r, w, bias=None, scale=None):
    kw = {}
    if bias is not None:
        kw["bias"] = bias
    if scale is not None:
        kw["scale"] = scale
    st.op("act", lambda e: e.activation(out=out, in_=in_, func=func, **kw), r=r, w=w)


def cp(st, eng, out, in_, r, w):
    if eng == "act":
        st.op("act", lambda e: e.copy(out=out, in_=in_), r=r, w=w)
    else:
        st.op(eng, lambda e: e.tensor_copy(out=out, in_=in_), r=r, w=w)


def mm(st, out, lhsT, rhs, start, stop, r, w):
    st.op("pe", lambda e: e.matmul(out, lhsT, rhs, start=start, stop=stop), r=r, w=w)


F32R = mybir.dt.float32r


def mmr(st, out, lhsT, rhs, start, stop, r, w):
    st.op("pe", lambda e: e.matmul(out, lhsT.bitcast(F32R), rhs.bitcast(F32R), start=start, stop=stop), r=r, w=w)


def tr(st, out, in_, ident, r, w):
    st.op("pe", lambda e: e.transpose(out, in_, ident), r=r, w=w)


def col_table():
    tab = {}
    off = 0
    for nm, n in [("mu_r", 8), ("mu_k", 8), ("mu_v", 8), ("w0", 8), ("a0", 8), ("v0", 8), ("k_k", 8), ("k_a", 8),
                  ("r_k", 8), ("gn_g", 8), ("gn_b", 8), ("lconv_b", 8), ("l_ba", 8), ("l_bx", 8), ("l_lam", 8),
                  ("lconv_w", 32), ("cconv_w", 248), ("cconv_b", 8), ("cln_g", 8), ("cln_b", 8),
                  ("ln1_g", 16), ("ln1_b", 16), ("mu_wl", 1), ("mu_al", 1), ("mu_gl", 2), ("mu_vr", 1)]:
        tab[nm] = (off, n)
        off += n
    return tab, off


COLT, NCOLS = col_table()


def chunkcols(v):
    v = np.asarray(v, np.float32).reshape(-1)
    n = v.shape[0]
    m = (n + 127) // 128
    buf = np.zeros((m * 128,), np.float32)
    buf[:n] = v
    return buf.reshape(m, 128).T


def host_layout(inp, l):
    cols = np.zeros((128, NCOLS), np.float32)

    def put(nm, arr):
        o, n = COLT[nm]
        cols[:, o:o + n] = arr

    mu = inp["shift_mu"][l]
    put("mu_r", chunkcols(mu[0:1024]))
    put("mu_k", chunkcols(mu[1024:2048]))
    put("mu_v", chunkcols(mu[2048:3072]))
    put("mu_wl", chunkcols(mu[3072:3136]))
    put("mu_al", chunkcols(mu[3136:3200]))
    put("mu_gl", chunkcols(mu[3200:3360]))
    put("w0", chunkcols(inp["rwkv_w0"][l]))
    put("a0", chunkcols(inp["rwkv_a0"][l]))
    if l > 0:
        put("v0", chunkcols(inp["rwkv_v0"][l - 1]))
        put("mu_vr", chunkcols(inp["shift_mu_vres"][l - 1]))
    put("k_k", chunkcols(inp["rwkv_k_k"][l]))
    put("k_a", chunkcols(inp["rwkv_k_a"][l]))
    put("r_k", chunkcols(inp["rwkv_r_k"][l].reshape(-1)))
    put("gn_g", chunkcols(inp["rwkv_gn_g"][l]))
    put("gn_b", chunkcols(inp["rwkv_gn_b"][l]))
    put("lconv_b", chunkcols(inp["lru_conv_b"][l]))
    put("l_ba", chunkcols(inp["lru_ba"][l]))
    put("l_bx", chunkcols(inp["lru_bx"][l]))
    put("l_lam", chunkcols(inp["lru_lambda"][l]))
    lw = inp["lru_conv_w"][l]
    put("lconv_w", lw.reshape(4, 8, 128).transpose(2, 1, 0).reshape(128, 32))
    cw = inp["conf_conv_w"][l]
    put("cconv_w", cw.reshape(31, 8, 128).transpose(2, 1, 0).reshape(128, 248))
    put("cconv_b", chunkcols(inp["conf_conv_b"][l]))
    put("cln_g", chunkcols(inp["conf_ln_g"][l]))
    put("cln_b", chunkcols(inp["conf_ln_b"][l]))
    put("ln1_g", chunkcols(inp["ln1_g"][l]))
    put("ln1_b", chunkcols(inp["ln1_b"][l]))
    bd = np.zeros((8, 128, 256), np.float32)
    for c in range(8):
        for hh in range(2):
            bd[c, hh * 64:(hh + 1) * 64, hh * 64:(hh + 1) * 64] = inp["lru_wa"][l][2 * c + hh]
            bd[c, hh * 64:(hh + 1) * 64, 128 + hh * 64:128 + (hh + 1) * 64] = inp["lru_wx"][l][2 * c + hh]
    bgu = np.ascontiguousarray(inp["exp_b_gu"][l].reshape(NE, 16, 128).transpose(2, 0, 1)).reshape(128, NE * 16)
    ln2 = np.ascontiguousarray(np.broadcast_to(
        np.concatenate([inp["ln2_g"][l], inp["ln2_b"][l]])[None, :], (128, 2 * D))).astype(np.float32)
    rb = np.ascontiguousarray(np.broadcast_to(inp["router_b"][l][None, :], (128, NE))).astype(np.float32)
    return {"cols": cols, "lrubd": bd, "bgu": bgu, "ln2bc": ln2, "rbbc": rb}


def host_consts():
    ident = np.eye(128, dtype=np.float32)
    ones = np.ones((128, 128), np.float32)
    bones = np.zeros((128, 128), np.float32)
    bones[:64, :64] = 1
    bones[64:, 64:] = 1
    s = np.arange(128)[:, None]
    t = np.arange(128)[None, :]
    m_su = (s < t).astype(np.float32)
    m_ui = (s <= t).astype(np.float32)
    m_sl = (s > t).astype(np.float32)
    cmask = np.ones((128, 512), np.float32)
    cmask[:, ::128] = 0
    return np.concatenate([ident, ones, bones, m_su, m_ui, m_sl, cmask], axis=1)


K_ID, K_ONES, K_BONES, K_SU, K_UI, K_SL, K_CM = 0, 128, 256, 384, 512, 640, 768
NCONST = 768 + 512


class KB:
    def __init__(self, S, L, debug=False):
        self.S = S
        self.L = L
        self.debug = debug
        self.nc = bass.Bass("TRN2", target_bir_lowering=False)
        self.d = {}
        self.bufs = {}

    def din(self, name, shape, dt=F32):
        self.d[name] = self.nc.dram_tensor("d_" + name, list(shape), dt, kind="ExternalInput").ap()
        self.bufs[name] = Buf(name)

    def dout(self, name, shape, dt=F32):
        self.d[name] = self.nc.dram_tensor("d_" + name, list(shape), dt, kind="ExternalOutput").ap()
        self.bufs[name] = Buf(name)

    def dscr(self, name, shape, dt=F32):
        kind = "ExternalOutput" if (self.debug and name in self.debug) else "Internal"
        self.d[name] = self.nc.dram_tensor("d_" + name, list(shape), dt, kind=kind).ap()
        self.bufs[name] = Buf(name)

    def stage(self, name):
        return Stage(self, name)


def load_consts(st, kb):
    k = st.sb("konst", [128, NCONST], F32)
    st.dma("sp", k[:, :], kb.d["konst"][:, :], k.b, r=[kb.bufs["konst"]], w=[k.b])
    return k


def load_cols(st, kb, l):
    c = st.sb("cols", [128, NCOLS], F32)
    st.dma("sp", c[:, :], kb.d[f"cols{l}"][:, :], c.b, r=[kb.bufs[f"cols{l}"]], w=[c.b])
    return c


def colap(cols, nm, j=0, rows=128):
    o, n = COLT[nm]
    return cols[0:rows, o + j:o + j + 1]


def stage_inproj(kb, l, xsrc):
    S = kb.S
    st = kb.stage(f"A{l}")
    TS = min(2048, S)
    nsb = S // TS
    ntb = TS // 512
    xbf = st.sb("xbf", [128, 16, TS], BF16, nsub=16)
    xst = st.ring("xst", 2, [128, TS])
    wst = st.ring("wst", 2, [128, 16, 256])
    wbf = st.ring("wbf", 2, [128, 16, 256], BF16)
    ost = st.ring("ost", 4, [128, 512])
    pss = st.ring("ps", 4, [128, 512], psum=True)
    P = kb.d["P"]
    Pb = kb.bufs["P"]
    groups = [(kb.d["w_in"][l], kb.bufs["w_in"], c0, min(256, NIN - c0), c0) for c0 in range(0, NIN, 256)]
    if l > 0:
        groups.append((kb.d["w_in_vres"][l - 1], kb.bufs["w_in_vres"], 0, 32, O_VR))
    xs, xsb = kb.d[xsrc], kb.bufs[xsrc]

    def load_w(g):
        W, Wb, c0, gc, prow = g
        ws = wst.next()
        st.dma("sp", ws[:, :, 0:gc], W[:, c0:c0 + gc].rearrange("(kc p) c -> p kc c", p=128), ws.b, r=[Wb], w=[ws.b])
        wb = wbf.next()
        cp(st, "pool", wb[:, :, 0:gc], ws[:, :, 0:gc], r=[ws.b], w=[wb.b])
        return wb

    for sbi in range(nsb):
        t0 = sbi * TS
        for kc in range(16):
            xt = xst.next()
            st.dma("sp", xt[:, :], xs[kc * 128:(kc + 1) * 128, t0:t0 + TS], xt.b, r=[xsb], w=[xt.b])
            cp(st, "act" if kc % 2 else "dve", xbf[:, kc, :], xt[:, :], r=[xt.b], w=[xbf.sub[kc]])
        nxt = load_w(groups[0])
        for gi, g in enumerate(groups):
            wb = nxt
            if gi + 1 < len(groups):
                nxt = load_w(groups[gi + 1])
            _, _, c0, gc, prow = g
            for cc in range(0, gc, 128):
                ncol = min(128, gc - cc)
                for tb in range(ntb):
                    ps = pss.next()
                    for kc in range(16):
                        mm(st, ps[0:ncol, :], wb[:, kc, cc:cc + ncol], xbf[:, kc, tb * 512:(tb + 1) * 512],
                           kc == 0, kc == 15, r=[wb.b, xbf.sub[kc]], w=[ps.b])
                    o = ost.next()
                    cp(st, "dve", o[0:ncol, :], ps[0:ncol, :], r=[ps.b], w=[o.b])
                    st.dma("act", P[prow + cc:prow + cc + ncol, t0 + tb * 512:t0 + (tb + 1) * 512], o[0:ncol, :], o.b,
                           r=[o.b], w=[Pb], disjoint=True)
    st.finish()


def load_halo(st, dst, src, rows, t0, n, h, srcbuf, q="sp"):
    if t0 == 0:
        st.op("pool", lambda e: e.memset(dst[0:rows, 0:h], 0.0), r=[], w=[dst.b])
        st.dma(q, dst[0:rows, h:h + n], src[:, 0:n], dst.b, r=[srcbuf], w=[dst.b])
    else:
        st.dma(q, dst[0:rows, 0:h + n], src[:, t0 - h:t0 + n], dst.b, r=[srcbuf], w=[dst.b])


def stage_rwkv_prep(kb, l):
    S = kb.S
    st = kb.stage(f"B{l}")
    k = load_consts(st, kb)
    cols = load_cols(st, kb, l)
    P, Pb = kb.d["P"], kb.bufs["P"]
    ntb = S // 512
    bones = k[:, K_BONES:K_BONES + 128]
    cmask = k[:, K_CM:K_CM + 512]
    w2 = st.sb("w2", [64, C])
    a2 = st.sb("a2", [64, C])
    g2a = st.sb("g2a", [128, C])
    g2b = st.sb("g2b", [32, C])
    st.dma("sp", w2[:, :], kb.d["rwkv_w2"][l], w2.b, r=[kb.bufs["rwkv_w2"]], w=[w2.b])
    st.dma("sp", a2[:, :], kb.d["rwkv_a2"][l], a2.b, r=[kb.bufs["rwkv_a2"]], w=[a2.b])
    st.dma("sp", g2a[:, :], kb.d["rwkv_g2"][l][0:128, :], g2a.b, r=[kb.bufs["rwkv_g2"]], w=[g2a.b])
    st.dma("sp", g2b[:, :], kb.d["rwkv_g2"][l][128:160, :], g2b.b, r=[kb.bufs["rwkv_g2"]], w=[g2b.b])
    if l > 0:
        v2 = st.sb("v2", [32, C])
        st.dma("sp", v2[:, :], kb.d["rwkv_v2"][l - 1], v2.b, r=[kb.bufs["rwkv_v2"]], w=[v2.b])

    def R(name, n=2, shape=(128, 512)):
        return st.ring(name, n, list(shape))

    lraw = R("lraw", 2, (128, 513))
    ld = R("ld", 1)
    LW, LA, LG1, LG2, LV = R("LW", 2), R("LA", 2), R("LG1", 2), R("LG2", 2), R("LV", 2)
    raw = {n: R("raw" + n, 2, (128, 513)) for n in "rkv"}
    names = ["dm", "rp", "kp", "vp", "sg", "ag", "kk", "kk2", "nrm", "kkn", "u", "kmod", "rkp", "bvec", "cs", "d3",
             "d4", "E1", "E2", "E3", "E4", "o_r", "o_a", "o_b", "o_k", "o_bh", "o_kh", "o_g", "o_rkv", "vf", "sv"]
    tl = {n: R(n, 2 if n.startswith("o_") else 1) for n in names}
    psr = st.ring("ps", 6, [128, 512], psum=True)

    def mix(dst, src, mucol, rows):
        d = ld.next()
        tt(st, "pool", d[0:rows, :], src[0:rows, 0:512], src[0:rows, 1:513], ALU.subtract, r=[src.b], w=[d.b])
        stt(st, "dve", dst[0:rows, :], d[0:rows, :], mucol, src[0:rows, 1:513], ALU.mult, ALU.add,
            r=[d.b, src.b, cols.b], w=[dst.b])

    def store(dname, row0, t0, tile_, rows=128, n=512):
        st.dma("act", kb.d[dname][row0:row0 + rows, t0:t0 + n], tile_[0:rows, 0:n], tile_.b, r=[tile_.b],
               w=[kb.bufs[dname]], disjoint=True)

    for tb in range(ntb):
        t0 = tb * 512
        x = lraw.next()
        load_halo(st, x, P[O_WL:O_WL + 64, :], 64, t0, 512, 1, Pb)
        lw = LW.next()
        mix(lw, x, colap(cols, "mu_wl", 0, 64), 64)
        act(st, lw[0:64, :], lw[0:64, :], AF.Tanh, r=[lw.b], w=[lw.b])
        x = lraw.next()
        load_halo(st, x, P[O_AL:O_AL + 64, :], 64, t0, 512, 1, Pb)
        la = LA.next()
        mix(la, x, colap(cols, "mu_al", 0, 64), 64)
        x = lraw.next()
        load_halo(st, x, P[O_GL:O_GL + 128, :], 128, t0, 512, 1, Pb)
        lg1 = LG1.next()
        mix(lg1, x, colap(cols, "mu_gl", 0, 128), 128)
        act(st, lg1[:, :], lg1[:, :], AF.Sigmoid, r=[lg1.b], w=[lg1.b])
        x = lraw.next()
        load_halo(st, x, P[O_GL + 128:O_GL + 160, :], 32, t0, 512, 1, Pb)
        lg2 = LG2.next()
        mix(lg2, x, colap(cols, "mu_gl", 1, 32), 32)
        act(st, lg2[0:32, :], lg2[0:32, :], AF.Sigmoid, r=[lg2.b], w=[lg2.b])
        if l > 0:
            x = lraw.next()
            load_halo(st, x, P[O_VR:O_VR + 32, :], 32, t0, 512, 1, Pb)
            lv = LV.next()
            mix(lv, x, colap(cols, "mu_vr", 0, 32), 32)
        for c in range(8):
            cs_ = slice(c * 128, (c + 1) * 128)
            rw = {}
            for n_, off in (("r", O_R), ("k", O_K), ("v", O_V)):
                rw[n_] = raw[n_].next()
                load_halo(st, rw[n_], P[off + c * 128:off + (c + 1) * 128, :], 128, t0, 512, 1, Pb)
            rp, kp, vp = tl["rp"].next(), tl["kp"].next(), tl["vp"].next()
            mix(rp, rw["r"], colap(cols, "mu_r", c), 128)
            mix(kp, rw["k"], colap(cols, "mu_k", c), 128)
            mix(vp, rw["v"], colap(cols, "mu_v", c), 128)
            ps = psr.next()
            mm(st, ps[:, :], w2[0:64, cs_], lw[0:64, :], True, True, r=[w2.b, lw.b], w=[ps.b])
            sg = tl["sg"].next()
            act(st, sg[:, :], ps[:, :], AF.Sigmoid, r=[ps.b, cols.b], w=[sg.b], bias=colap(cols, "w0", c))
            ps = psr.next()
            mm(st, ps[:, :], a2[0:64, cs_], la[0:64, :], True, True, r=[a2.b, la.b], w=[ps.b])
            ag = tl["ag"].next()
            act(st, ag[:, :], ps[:, :], AF.Sigmoid, r=[ps.b, cols.b], w=[ag.b], bias=colap(cols, "a0", c))
            ps = psr.next()
            mm(st, ps[:, :], g2a[:, cs_], lg1[:, :], True, False, r=[g2a.b, lg1.b], w=[ps.b])
            mm(st, ps[:, :], g2b[0:32, cs_], lg2[0:32, :], False, True, r=[g2b.b, lg2.b], w=[ps.b])
            og = tl["o_g"].next()
            cp(st, "act", og[:, :], ps[:, :], r=[ps.b], w=[og.b])
            store("G", c * 128, t0, og)
            if l > 0:
                ps = psr.next()
                mm(st, ps[:, :], v2[0:32, cs_], lv[0:32, :], True, True, r=[v2.b, lv.b], w=[ps.b])
                sv = tl["sv"].next()
                act(st, sv[:, :], ps[:, :], AF.Sigmoid, r=[ps.b, cols.b], w=[sv.b], bias=colap(cols, "v0", c))
                vf = tl["vf"].next()
                st.dma("sp", vf[:, :], kb.d["VF"][cs_, t0:t0 + 512], vf.b, r=[kb.bufs["VF"]], w=[vf.b])
                tt(st, "pool", vf[:, :], vf[:, :], vp[:, :], ALU.subtract, r=[vf.b, vp.b], w=[vf.b])
                tt(st, "dve", vf[:, :], vf[:, :], sv[:, :], ALU.mult, r=[vf.b, sv.b], w=[vf.b])
                tt(st, "pool", vp[:, :], vp[:, :], vf[:, :], ALU.add, r=[vp.b, vf.b], w=[vp.b])
            else:
                store("VF", c * 128, t0, vp)
            store("SV", c * 128, t0, vp)
            kk, kk2 = tl["kk"].next(), tl["kk2"].next()
            ts(st, "dve", kk[:, :], kp[:, :], colap(cols, "k_k", c), ALU.mult, r=[kp.b, cols.b], w=[kk.b])
            tt(st, "pool", kk2[:, :], kk[:, :], kk[:, :], ALU.mult, r=[kk.b], w=[kk2.b])
            ps = psr.next()
            mm(st, ps[:, :], bones, kk2[:, :], True, True, r=[k.b, kk2.b], w=[ps.b])
            nrm = tl["nrm"].next()
            act(st, nrm[:, :], ps[:, :], AF.Sqrt, r=[ps.b], w=[nrm.b])
            ts(st, "dve", nrm[:, :], nrm[:, :], 1e-12, ALU.max, r=[nrm.b], w=[nrm.b])
            st.op("dve", lambda e, o=nrm: e.reciprocal(out=o[:, :], in_=o[:, :]), r=[nrm.b], w=[nrm.b])
            kkn = tl["kkn"].next()
            tt(st, "pool", kkn[:, :], kk[:, :], nrm[:, :], ALU.mult, r=[kk.b, nrm.b], w=[kkn.b])
            u, kmod = tl["u"].next(), tl["kmod"].next()
            ts(st, "dve", u[:, :], ag[:, :], -1.0, ALU.add, r=[ag.b, cols.b], w=[u.b], s2=colap(cols, "k_a", c),
               op1=ALU.mult)
            stt(st, "dve", kmod[:, :], u[:, :], 1.0, kp[:, :], ALU.add, ALU.mult, r=[u.b, kp.b], w=[kmod.b])
            rkp = tl["rkp"].next()
            stt(st, "dve", rkp[:, :], rp[:, :], colap(cols, "r_k", c), kmod[:, :], ALU.mult, ALU.mult,
                r=[rp.b, kmod.b, cols.b], w=[rkp.b])
            ps = psr.next()
            mm(st, ps[:, :], bones, rkp[:, :], True, True, r=[k.b, rkp.b], w=[ps.b])
            orkv = tl["o_rkv"].next()
            tt(st, "dve", orkv[:, :], ps[:, :], vp[:, :], ALU.mult, r=[ps.b, vp.b], w=[orkv.b])
            store("RKV", c * 128, t0, orkv)
            bvec = tl["bvec"].next()
            tt(st, "pool", bvec[:, :], kkn[:, :], ag[:, :], ALU.mult, r=[kkn.b, ag.b], w=[bvec.b])
            cs = tl["cs"].next()
            st.op("dve", lambda e, o=cs, s=sg: e.tensor_tensor_scan(out=o[:, :], data0=cmask, data1=s[:, :],
                                                                    initial=0.0, op0=ALU.mult, op1=ALU.add),
                  r=[k.b, sg.b], w=[cs.b])
            E1, E2, E3, E4 = tl["E1"].next(), tl["E2"].next(), tl["E3"].next(), tl["E4"].next()
            act(st, E1[:, :], cs[:, :], AF.Exp, r=[cs.b], w=[E1.b], scale=-KAPPA)
            act(st, E2[:, :], cs[:, :], AF.Exp, r=[cs.b], w=[E2.b], scale=KAPPA)
            d3, d4 = tl["d3"].next(), tl["d4"].next()
            tt(st, "pool", d3[:, :], cs[:, :], sg[:, :], ALU.subtract, r=[cs.b, sg.b], w=[d3.b])
            act(st, E3[:, :], d3[:, :], AF.Exp, r=[d3.b], w=[E3.b], scale=-KAPPA)
            for ci in range(4):
                ts(st, "dve", d4[:, ci * 128:(ci + 1) * 128], cs[:, ci * 128:(ci + 1) * 128],
                   cs[:, ci * 128 + 127:ci * 128 + 128], ALU.subtract, r=[cs.b], w=[d4.b])
            act(st, E4[:, :], d4[:, :], AF.Exp, r=[d4.b], w=[E4.b], scale=KAPPA)
            o = tl["o_r"].next()
            tt(st, "pool", o[:, :], rp[:, :], E1[:, :], ALU.mult, r=[rp.b, E1.b], w=[o.b])
            store("SR", c * 128, t0, o)
            o = tl["o_a"].next()
            stt(st, "dve", o[:, :], kkn[:, :], -1.0, E3[:, :], ALU.mult, ALU.mult, r=[kkn.b, E3.b], w=[o.b])
            store("SA", c * 128, t0, o)
            o = tl["o_b"].next()
            tt(st, "pool", o[:, :], bvec[:, :], E2[:, :], ALU.mult, r=[bvec.b, E2.b], w=[o.b])
            store("SB", c * 128, t0, o)
            o = tl["o_k"].next()
            tt(st, "dve", o[:, :], kmod[:, :], E2[:, :], ALU.mult, r=[kmod.b, E2.b], w=[o.b])
            store("SK", c * 128, t0, o)
            o = tl["o_bh"].next()
            tt(st, "pool", o[:, :], bvec[:, :], E4[:, :], ALU.mult, r=[bvec.b, E4.b], w=[o.b])
            store("SBH", c * 128, t0, o)
            o = tl["o_kh"].next()
            tt(st, "dve", o[:, :], kmod[:, :], E4[:, :], ALU.mult, r=[kmod.b, E4.b], w=[o.b])
            store("SKH", c * 128, t0, o)
            st.dma("act", kb.d["GL"][cs_, tb * 4:tb * 4 + 4], E1[:, 127:512:128], E1.b, r=[E1.b], w=[kb.bufs["GL"]],
                   disjoint=True, kw={"allow_slow_non_contiguous": True})
    st.finish()


def stage_rwkv_scan(kb, l, G=2, upto=99):
    S = kb.S
    st = kb.stage(f"C{l}")
    k = load_consts(st, kb)
    ident64 = k[0:64, K_ID:K_ID + 64]
    NCH = S // 128
    NB = S // 256
    slots = []
    for g in range(G):
        d = {}
        d["AR"] = st.ring(f"AR{g}", 2, [64, 2, 256])
        d["BK"] = st.ring(f"BK{g}", 2, [64, 2, 256])
        d["HV"] = st.ring(f"HV{g}", 2, [64, 3, 256])
        d["YS"] = st.ring(f"YS{g}", 2, [64, 256])
        d["GL"] = st.sb(f"GL{g}", [64, NCH])
        d["N0RB"] = st.sb(f"N0RB{g}", [128, 256])
        d["AKRK"] = st.sb(f"AKRK{g}", [128, 256])
        d["N"] = [None] + [st.sb(f"N{g}_{i}", [128, 128]) for i in range(1, 7)]
        d["M"] = [st.sb(f"M{g}_{i}", [128, 128]) for i in range(2)]
        d["VBK"] = st.sb(f"VBK{g}", [128, 192])
        d["U"] = [st.sb(f"U{g}_{i}", [128, 64]) for i in range(2)]
        d["H"] = [st.sb(f"H{g}_{i}", [64, 64]) for i in range(2)]
        pa = st.ps(f"pa{g}", [128, 512])
        pb = st.ps(f"pb{g}", [128, 512])
        pc = st.ps(f"pc{g}", [128, 512])
        pd = st.ps(f"pd{g}", [128, 512])
        d["p_s1"] = (pa, slice(0, 256), pa.b)
        d["p_s2"] = (pa, slice(256, 512), pa.b)
        d["p_m"] = (pb, slice(0, 128), pb.b)
        d["p_t"] = (pb, slice(128, 320), pb.b)
        d["p_w"] = (pb, slice(320, 384), pb.b)
        d["p_n"] = (pc, slice(0, 128), pc.b)
        d["p_mm"] = (pc, slice(128, 256), pc.b)
        d["p_u"] = (pd, slice(0, 64), pd.b)
        d["p_h"] = (pd, slice(64, 128), pd.b)
        d["p_y"] = (pd, slice(128, 256), pd.b)
        slots.append(d)
    m_a = k[:, K_SU:K_SU + 256]
    m_sl = k[:, K_SL:K_SL + 128]

    def P_(d, nm, rows=128):
        t_, sl, b = d[nm]
        return t_[0:rows, sl], b

    for h0 in range(0, 16, G):
        heads = list(range(h0, min(16, h0 + G)))
        hs = {}
        for gi, h in enumerate(heads):
            d = slots[gi]
            r0 = h * 64
            st.dma("sp", d["GL"][:, :], kb.d["GL"][r0:r0 + 64, :], d["GL"].b, r=[kb.bufs["GL"]], w=[d["GL"].b])
            st.op("pool", lambda e, d=d: e.memset(d["H"][0][:, :], 0.0), r=[], w=[d["H"][0].b])
            hs[h] = {"hi": 0}
        for bi in range(NB):
            t0 = bi * 256
            cur = {}
            for gi, h in enumerate(heads):
                d = slots[gi]
                r0 = h * 64
                AR, BK, HV, YS = d["AR"].next(), d["BK"].next(), d["HV"].next(), d["YS"].next()

                def ld(dst, nm):
                    st.dma("sp", dst, kb.d[nm][r0:r0 + 64, t0:t0 + 256], dstb, r=[kb.bufs[nm]], w=[dstb])

                dstb = AR.b
                st.dma("sp", AR[:, :, 0:128], kb.d["SA"][r0:r0 + 64, t0:t0 + 256].rearrange("p (c t) -> p c t", t=128),
                       AR.b, r=[kb.bufs["SA"]], w=[AR.b])
                st.dma("sp", AR[:, :, 128:256], kb.d["SR"][r0:r0 + 64, t0:t0 + 256].rearrange("p (c t) -> p c t", t=128),
                       AR.b, r=[kb.bufs["SR"]], w=[AR.b], group=True)
                st.dma("sp", BK[:, 0, :], kb.d["SB"][r0:r0 + 64, t0:t0 + 256], BK.b, r=[kb.bufs["SB"]], w=[BK.b])
                st.dma("sp", BK[:, 1, :], kb.d["SK"][r0:r0 + 64, t0:t0 + 256], BK.b, r=[kb.bufs["SK"]], w=[BK.b], group=True)
                st.dma("sp", HV[:, 0, :], kb.d["SBH"][r0:r0 + 64, t0:t0 + 256], HV.b, r=[kb.bufs["SBH"]], w=[HV.b])
                st.dma("sp", HV[:, 1, :], kb.d["SKH"][r0:r0 + 64, t0:t0 + 256], HV.b, r=[kb.bufs["SKH"]], w=[HV.b], group=True)
                st.dma("sp", HV[:, 2, :], kb.d["SV"][r0:r0 + 64, t0:t0 + 256], HV.b, r=[kb.bufs["SV"]], w=[HV.b], group=True)
                cur[h] = (AR, BK, HV, YS)
            for ci in range(2):
                ch = bi * 2 + ci
                cs_ = slice(ci * 128, (ci + 1) * 128)
                for gi, h in enumerate(heads):
                    d = slots[gi]
                    AR, BK, HV, YS = cur[h]
                    o, b = P_(d, "p_s1")
                    mm(st, o, BK[:, 0, cs_], AR[:, ci, :], True, True, r=[BK.b, AR.b], w=[b])
                    tt(st, "dve", d["N0RB"][:, :], o, m_a, ALU.mult, r=[b, k.b], w=[d["N0RB"].b])
                    o, b = P_(d, "p_s2")
                    mm(st, o, BK[:, 1, cs_], AR[:, ci, :], True, True, r=[BK.b, AR.b], w=[b])
                    tt(st, "dve", d["AKRK"][:, :], o, m_a, ALU.mult, r=[b, k.b], w=[d["AKRK"].b])
                    o, b = P_(d, "p_m")
                    mm(st, o, AR[:, ci, 0:128], BK[:, 0, cs_], True, True, r=[BK.b, AR.b], w=[b])
                    tt(st, "dve", d["M"][0][:, :], o, m_sl, ALU.mult, r=[b, k.b], w=[d["M"][0].b])
                    t_, sl, b = d["p_t"]
                    base = sl.start
                    tr(st, t_[:, base:base + 64], HV[:, 2, cs_], ident64, r=[HV.b, k.b], w=[b])
                    tr(st, t_[:, base + 64:base + 128], HV[:, 0, cs_], ident64, r=[HV.b, k.b], w=[b])
                    tr(st, t_[:, base + 128:base + 192], HV[:, 1, cs_], ident64, r=[HV.b, k.b], w=[b])
                    cp(st, "act", d["VBK"][:, :], t_[:, sl], r=[b], w=[d["VBK"].b])
                if upto < 2:
                    continue
                for kk_ in range(6):
                    for gi, h in enumerate(heads):
                        d = slots[gi]
                        Nk = d["N0RB"][:, 0:128] if kk_ == 0 else d["N"][kk_][:, :]
                        Nkb = d["N0RB"].b if kk_ == 0 else d["N"][kk_].b
                        Mk = d["M"][kk_ % 2]
                        o, b = P_(d, "p_n")
                        mm(st, o, Mk[:, :], Nk, True, True, r=[Mk.b, Nkb], w=[b])
                        cp(st, "act", d["N"][kk_ + 1][:, :], o, r=[b], w=[d["N"][kk_ + 1].b])
                        if kk_ < 5:
                            o, b = P_(d, "p_mm")
                            mm(st, o, Nk, Mk[:, :], True, True, r=[Mk.b, Nkb], w=[b])
                            Mn = d["M"][(kk_ + 1) % 2]
                            cp(st, "dve", Mn[:, :], o, r=[b], w=[Mn.b])
                if upto < 3:
                    continue
                for gi, h in enumerate(heads):
                    d = slots[gi]
                    AR, BK, HV, YS = cur[h]
                    H = d["H"][hs[h]["hi"]]
                    o, b = P_(d, "p_w")
                    mm(st, o, AR[:, ci, 0:128], H[:, :], True, False, r=[AR.b, H.b], w=[b])
                    mm(st, o, d["AKRK"][:, 0:128], d["VBK"][:, 0:64], False, True, r=[d["AKRK"].b, d["VBK"].b], w=[b])
                    cp(st, "act", d["U"][0][:, :], o, r=[b], w=[d["U"][0].b])
                for kk_ in range(7):
                    for gi, h in enumerate(heads):
                        d = slots[gi]
                        Nk = d["N0RB"][:, 0:128] if kk_ == 0 else d["N"][kk_][:, :]
                        Nkb = d["N0RB"].b if kk_ == 0 else d["N"][kk_].b
                        Uk, Un = d["U"][kk_ % 2], d["U"][(kk_ + 1) % 2]
                        o, b = P_(d, "p_u")
                        mm(st, o, Nk, Uk[:, :], True, True, r=[Nkb, Uk.b], w=[b])
                        tt(st, "dve", Un[:, :], o, Uk[:, :], ALU.add, r=[b, Uk.b], w=[Un.b])
                if upto < 4:
                    continue
                for gi, h in enumerate(heads):
                    d = slots[gi]
                    AR, BK, HV, YS = cur[h]
                    H = d["H"][hs[h]["hi"]]
                    Hn = d["H"][1 - hs[h]["hi"]]
                    Uf = d["U"][1]
                    o, b = P_(d, "p_y", 64)
                    mm(st, o, H[:, :], AR[:, ci, 128:256], True, False, r=[H.b, AR.b], w=[b])
                    mm(st, o, Uf[:, :], d["N0RB"][:, 128:256], False, False, r=[Uf.b, d["N0RB"].b], w=[b])
                    mm(st, o, d["VBK"][:, 0:64], d["AKRK"][:, 128:256], False, True, r=[d["VBK"].b, d["AKRK"].b], w=[b])
                    cp(st, "act", YS[:, cs_], o, r=[b], w=[YS.b])
                    o, b = P_(d, "p_h", 64)
                    mm(st, o, d["VBK"][:, 64:128], Uf[:, :], True, False, r=[d["VBK"].b, Uf.b], w=[b])
                    mm(st, o, d["VBK"][:, 128:192], d["VBK"][:, 0:64], False, True, r=[d["VBK"].b], w=[b])
                    stt(st, "dve", Hn[:, :], H[:, :], d["GL"][:, ch:ch + 1], o, ALU.mult, ALU.add,
                        r=[H.b, d["GL"].b, b], w=[Hn.b])
                    hs[h]["hi"] = 1 - hs[h]["hi"]
            for gi, h in enumerate(heads):
                AR, BK, HV, YS = cur[h]
                st.dma("act", kb.d["YS"][h * 64:h * 64 + 64, t0:t0 + 256], YS[:, :], YS.b, r=[YS.b], w=[kb.bufs["YS"]],
                       disjoint=True)
    st.finish()


def stage_rwkv_post(kb, l):
    S = kb.S
    st = kb.stage(f"D{l}")
    k = load_consts(st, kb)
    cols = load_cols(st, kb, l)
    bones = k[:, K_BONES:K_BONES + 128]
    yr, rr, gr = st.ring("y", 2, [128, 512]), st.ring("rkv", 2, [128, 512]), st.ring("g", 2, [128, 512])
    y2r, mr, vr, ycr = (st.ring(n, 1, [128, 512]) for n in ("y2", "mean", "var", "yc"))
    outr = st.ring("out", 2, [128, 512], BF16)
    psr = st.ring("ps", 4, [128, 512], psum=True)
    for tb in range(S // 512):
        t0 = tb * 512
        for c in range(8):
            rs = slice(c * 128, (c + 1) * 128)
            y, rk, g = yr.next(), rr.next(), gr.next()
            st.dma("sp", y[:, :], kb.d["YS"][rs, t0:t0 + 512], y.b, r=[kb.bufs["YS"]], w=[y.b])
            st.dma("sp", rk[:, :], kb.d["RKV"][rs, t0:t0 + 512], rk.b, r=[kb.bufs["RKV"]], w=[rk.b])
            st.dma("sp", g[:, :], kb.d["G"][rs, t0:t0 + 512], g.b, r=[kb.bufs["G"]], w=[g.b])
            y2, mean, var, yc = y2r.next(), mr.next(), vr.next(), ycr.next()
            tt(st, "pool", y2[:, :], y[:, :], y[:, :], ALU.mult, r=[y.b], w=[y2.b])
            p1, p2 = psr.next(), psr.next()
            mm(st, p1[:, :], bones, y[:, :], True, True, r=[k.b, y.b], w=[p1.b])
            mm(st, p2[:, :], bones, y2[:, :], True, True, r=[k.b, y2.b], w=[p2.b])
            st.op("act", lambda e, o=mean, i=p1: e.mul(out=o[:, :], in_=i[:, :], mul=1.0 / 64), r=[p1.b], w=[mean.b])
            tt(st, "pool", y2[:, :], mean[:, :], mean[:, :], ALU.mult, r=[mean.b], w=[y2.b])
            stt(st, "dve", var[:, :], p2[:, :], 1.0 / 64, y2[:, :], ALU.mult, ALU.subtract, r=[p2.b, y2.b], w=[var.b])
            ts(st, "dve", var[:, :], var[:, :], 64e-5, ALU.add, r=[var.b], w=[var.b])
            act(st, var[:, :], var[:, :], AF.Sqrt, r=[var.b], w=[var.b])
            st.op("dve", lambda e, o=var: e.reciprocal(out=o[:, :], in_=o[:, :]), r=[var.b], w=[var.b])
            tt(st, "pool", yc[:, :], y[:, :], mean[:, :], ALU.subtract, r=[y.b, mean.b], w=[yc.b])
            tt(st, "dve", yc[:, :], yc[:, :], var[:, :], ALU.mult, r=[yc.b, var.b], w=[yc.b])
            ts(st, "dve", yc[:, :], yc[:, :], colap(cols, "gn_g", c), ALU.mult, r=[yc.b, cols.b], w=[yc.b],
               s2=colap(cols, "gn_b", c), op1=ALU.add)
            tt(st, "pool", yc[:, :], yc[:, :], rk[:, :], ALU.add, r=[yc.b, rk.b], w=[yc.b])
            o = outr.next()
            tt(st, "dve", o[:, :], yc[:, :], g[:, :], ALU.mult, r=[yc.b, g.b], w=[o.b])
            st.dma("act", kb.d["YA"][rs, t0:t0 + 512], o[:, :], o.b, r=[o.b], w=[kb.bufs["YA"]], disjoint=True)
    st.finish()


def declare(kb, moe=True):
    S, L = kb.S, kb.L
    kb.din("xT", [D, S])
    kb.din("konst", [128, NCONST])
    kb.din("w_in", [L, D, NIN])
    kb.din("w_in_vres", [max(L - 1, 1), D, 32])
    kb.din("rwkv_w2", [L, 64, C])
    kb.din("rwkv_a2", [L, 64, C])
    kb.din("rwkv_v2", [max(L - 1, 1), 32, C])
    kb.din("rwkv_g2", [L, 160, C])
    kb.din("w_branch", [L, 3, C, D])
    kb.din("w_out", [L, D, D])
    kb.din("router_w", [L, D, NE])
    if moe:
        kb.din("exp_w_gu", [L, NE, D, 2 * DFF])
        kb.din("exp_w_down", [L, NE, DFF, D])
    kb.din("exp_b_down", [L, NE, D])
    for l in range(L):
        kb.din(f"cols{l}", [128, NCOLS])
        kb.din(f"lrubd{l}", [8, 128, 256])
        kb.din(f"bgu{l}", [128, NE * 16])
        kb.din(f"ln2bc{l}", [128, 2 * D])
        kb.din(f"rbbc{l}", [128, NE])
    kb.dscr("P", [NIN + 32, S])
    for nm in ("G", "VF", "SV", "RKV", "SR", "SA", "SB", "SK", "SBH", "SKH", "YS", "CC"):
        kb.dscr(nm, [C, S])
    kb.dscr("GL", [C, S // 128])
    for nm in ("YA", "YB", "YC"):
        kb.dscr(nm, [C, S], BF16)
    kb.dscr("X1T", [D, S])
    kb.dscr("X2T", [D, S])
    kb.dscr("GT", [S, NE])
    kb.dout("out", [S, D])


def make_inputs(inp, L, moe=True):
    m = {"konst": host_consts()}
    for nm in ("w_in", "rwkv_w2", "rwkv_a2", "rwkv_g2", "w_branch", "w_out", "router_w", "exp_b_down") + (
            ("exp_w_gu", "exp_w_down") if moe else ()):
        m[nm] = np.ascontiguousarray(inp[nm][:L], dtype=np.float32)
    for nm in ("w_in_vres", "rwkv_v2"):
        m[nm] = np.ascontiguousarray(inp[nm][:max(L - 1, 1)], dtype=np.float32)
    for l in range(L):
        for k_, v in host_layout(inp, l).items():
            m[f"{k_}{l}"] = np.ascontiguousarray(v, dtype=np.float32)
    return m


def stage_lru(kb, l):
    S = kb.S
    st = kb.stage(f"E{l}")
    cols = load_cols(st, kb, l)
    TB = min(1024, S)
    P, Pb = kb.d["P"], kb.bufs["P"]
    bd = st.sb("bd", [128, 8, 256])
    st.dma("sp", bd[:, :, :], kb.d[f"lrubd{l}"].rearrange("c p n -> p c n"), bd.b, r=[kb.bufs[f"lrubd{l}"]], w=[bd.b])
    csp = st.sb("csp", [128, 8])
    o, n = COLT["l_lam"]
    act(st, csp[:, :], cols[:, o:o + 8], AF.Exp, r=[cols.b], w=[csp.b], scale=-1.0)
    act(st, csp[:, :], csp[:, :], AF.Ln, r=[csp.b], w=[csp.b], bias=1.0)
    ts(st, "dve", csp[:, :], csp[:, :], -8.0, ALU.mult, r=[csp.b], w=[csp.b])
    lxr = st.ring("lx", 2, [128, TB + 3])
    lgr = st.ring("lg", 2, [128, TB])
    names = ["xc", "rg", "ig", "a", "t1", "b", "h", "ge"]
    tl = {n_: st.ring(n_, 1, [128, TB]) for n_ in names}
    outr = st.ring("out", 2, [128, TB], BF16)
    hc = st.sb("hcarry", [128, 8])
    st.op("pool", lambda e: e.memset(hc[:, :], 0.0), r=[], w=[hc.b])
    psr = st.ring("ps", 4, [128, 512], psum=True)
    for tb in range(S // TB):
        t0 = tb * TB
        for c in range(8):
            rs = slice(c * 128, (c + 1) * 128)
            lx, lg = lxr.next(), lgr.next()
            load_halo(st, lx, P[O_LX + c * 128:O_LX + (c + 1) * 128, :], 128, t0, TB, 3, Pb)
            st.dma("sp", lg[:, :], P[O_LG + c * 128:O_LG + (c + 1) * 128, t0:t0 + TB], lg.b, r=[Pb], w=[lg.b])
            xc, rg, ig, a, t1, b, h, ge = (tl[n_].next() for n_ in names)
            wo = COLT["lconv_w"][0] + c * 4
            ts(st, "dve", xc[:, :], lx[:, 3:3 + TB], cols[:, wo + 3:wo + 4], ALU.mult, r=[lx.b, cols.b], w=[xc.b],
               s2=colap(cols, "lconv_b", c), op1=ALU.add)
            for j in range(1, 4):
                stt(st, "dve", xc[:, :], lx[:, 3 - j:3 - j + TB], cols[:, wo + 3 - j:wo + 4 - j], xc[:, :], ALU.mult,
                    ALU.add, r=[lx.b, cols.b, xc.b], w=[xc.b])
            for hb in range(TB // 512):
                hs_ = slice(hb * 512, (hb + 1) * 512)
                p1, p2 = psr.next(), psr.next()
                mm(st, p1[:, :], bd[:, c, 0:128], xc[:, hs_], True, True, r=[bd.b, xc.b], w=[p1.b])
                mm(st, p2[:, :], bd[:, c, 128:256], xc[:, hs_], True, True, r=[bd.b, xc.b], w=[p2.b])
                act(st, rg[:, hs_], p1[:, :], AF.Sigmoid, r=[p1.b, cols.b], w=[rg.b], bias=colap(cols, "l_ba", c))
                act(st, ig[:, hs_], p2[:, :], AF.Sigmoid, r=[p2.b, cols.b], w=[ig.b], bias=colap(cols, "l_bx", c))
            act(st, a[:, :], rg[:, :], AF.Exp, r=[rg.b, csp.b], w=[a.b], scale=csp[:, c:c + 1])
            tt(st, "pool", t1[:, :], a[:, :], a[:, :], ALU.mult, r=[a.b], w=[t1.b])
            ts(st, "dve", t1[:, :], t1[:, :], -1.0, ALU.mult, r=[t1.b], w=[t1.b], s2=1.0, op1=ALU.add)
            ts(st, "dve", t1[:, :], t1[:, :], 0.0, ALU.max, r=[t1.b], w=[t1.b])
            act(st, t1[:, :], t1[:, :], AF.Sqrt, r=[t1.b], w=[t1.b])
            tt(st, "pool", b[:, :], ig[:, :], xc[:, :], ALU.mult, r=[ig.b, xc.b], w=[b.b])
            tt(st, "pool", b[:, :], b[:, :], t1[:, :], ALU.mult, r=[b.b, t1.b], w=[b.b])
            st.op("dve", lambda e, h=h, a=a, b=b, c=c: e.tensor_tensor_scan(
                out=h[:, :], data0=a[:, :], data1=b[:, :], initial=hc[:, c:c + 1], op0=ALU.mult, op1=ALU.add),
                r=[a.b, b.b, hc.b], w=[h.b])
            cp(st, "dve", hc[:, c:c + 1], h[:, TB - 1:TB], r=[h.b], w=[hc.b])
            tt(st, "pool", ge[:, :], lg[:, :], lg[:, :], ALU.mult, r=[lg.b], w=[ge.b])
            ts(st, "dve", ge[:, :], ge[:, :], 0.044715, ALU.mult, r=[ge.b], w=[ge.b], s2=1.0, op1=ALU.add)
            tt(st, "pool", ge[:, :], ge[:, :], lg[:, :], ALU.mult, r=[ge.b, lg.b], w=[ge.b])
            act(st, ge[:, :], ge[:, :], AF.Sigmoid, r=[ge.b], w=[ge.b], scale=1.5957691216057308)
            tt(st, "pool", ge[:, :], ge[:, :], lg[:, :], ALU.mult, r=[ge.b, lg.b], w=[ge.b])
            o_ = outr.next()
            tt(st, "dve", o_[:, :], ge[:, :], h[:, :], ALU.mult, r=[ge.b, h.b], w=[o_.b])
            st.dma("act", kb.d["YB"][rs, t0:t0 + TB], o_[:, :], o_.b, r=[o_.b], w=[kb.bufs["YB"]], disjoint=True)
    st.finish()


def stage_conf(kb, l):
    S = kb.S
    st = kb.stage(f"F{l}")
    k = load_consts(st, kb)
    cols = load_cols(st, kb, l)
    ones = k[:, K_ONES:K_ONES + 128]
    P, Pb = kb.d["P"], kb.bufs["P"]
    TB = min(1024, S)
    HL = 30
    vr = st.ring("val", 2, [128, TB + HL])
    gr = st.ring("gate", 2, [128, TB + HL])
    accr = st.ring("acc", 2, [128, TB])
    acc2r = st.ring("acc2", 2, [128, TB])
    tmpr = st.ring("tmp", 3, [128, TB])
    for tb in range(S // TB):
        t0 = tb * TB
        for c in range(8):
            rs = slice(c * 128, (c + 1) * 128)
            v, g = vr.next(), gr.next()
            load_halo(st, v, P[O_CU + c * 128:O_CU + (c + 1) * 128, :], 128, t0, TB, HL, Pb)
            load_halo(st, g, P[O_CU + C + c * 128:O_CU + C + (c + 1) * 128, :], 128, t0, TB, HL, Pb)
            act(st, g[:, :], g[:, :], AF.Sigmoid, r=[g.b], w=[g.b])
            tt(st, "pool", v[:, :], v[:, :], g[:, :], ALU.mult, r=[v.b, g.b], w=[v.b])
            acc, acc2 = accr.next(), acc2r.next()
            wo = COLT["cconv_w"][0] + c * 31
            ts(st, "dve", acc[:, :], v[:, HL:HL + TB], cols[:, wo + 30:wo + 31], ALU.mult, r=[v.b, cols.b], w=[acc.b],
               s2=colap(cols, "cconv_b", c), op1=ALU.add)
            for kk_ in range(29, 14, -1):
                sh = 30 - kk_
                stt(st, "dve", acc[:, :], v[:, HL - sh:HL - sh + TB], cols[:, wo + kk_:wo + kk_ + 1], acc[:, :],
                    ALU.mult, ALU.add, r=[v.b, cols.b, acc.b], w=[acc.b])
            act(st, acc2[:, :], v[:, 0:TB], AF.Copy, r=[v.b, cols.b], w=[acc2.b], scale=cols[:, wo:wo + 1])
            for kk_ in range(1, 15):
                sh = 30 - kk_
                tmp = tmpr.next()
                act(st, tmp[:, :], v[:, HL - sh:HL - sh + TB], AF.Copy, r=[v.b, cols.b], w=[tmp.b],
                    scale=cols[:, wo + kk_:wo + kk_ + 1])
                tt(st, "pool", acc2[:, :], acc2[:, :], tmp[:, :], ALU.add, r=[acc2.b, tmp.b], w=[acc2.b])
            tt(st, "dve", acc[:, :], acc[:, :], acc2[:, :], ALU.add, r=[acc.b, acc2.b], w=[acc.b])
            st.dma("act", kb.d["CC"][rs, t0:t0 + TB], acc[:, :], acc.b, r=[acc.b], w=[kb.bufs["CC"]], disjoint=True)
    st.finish()
    st = kb.stage(f"F2{l}")
    k = load_consts(st, kb)
    cols = load_cols(st, kb, l)
    ones = k[:, K_ONES:K_ONES + 128]
    ccr = st.ring("cc", 2, [128, 8, 512])
    sqr = st.ring("sq", 2, [128, 512])
    mean, var, tmp = st.sb("mean", [128, 512]), st.sb("var", [128, 512]), st.sb("tmp", [128, 512])
    ycr = st.ring("yc", 2, [128, 512])
    sgr = st.ring("sg", 2, [128, 512])
    outr = st.ring("out", 2, [128, 8, 512], BF16)
    ps1, ps2 = st.ring("ps1", 2, [128, 512], psum=True), st.ring("ps2", 2, [128, 512], psum=True)
    for tb in range(S // 512):
        t0 = tb * 512
        cc = ccr.next()
        st.dma("sp", cc[:, :, :], kb.d["CC"][:, t0:t0 + 512].rearrange("(c p) t -> p c t", p=128), cc.b,
               r=[kb.bufs["CC"]], w=[cc.b])
        p1, p2 = ps1.next(), ps2.next()
        for c in range(8):
            sq = sqr.next()
            tt(st, "pool", sq[:, :], cc[:, c, :], cc[:, c, :], ALU.mult, r=[cc.b], w=[sq.b])
            mm(st, p1[:, :], ones, cc[:, c, :], c == 0, c == 7, r=[k.b, cc.b], w=[p1.b])
            mm(st, p2[:, :], ones, sq[:, :], c == 0, c == 7, r=[k.b, sq.b], w=[p2.b])
        st.op("act", lambda e, i=p1: e.mul(out=mean[:, :], in_=i[:, :], mul=1.0 / C), r=[p1.b], w=[mean.b])
        tt(st, "pool", tmp[:, :], mean[:, :], mean[:, :], ALU.mult, r=[mean.b], w=[tmp.b])
        stt(st, "dve", var[:, :], p2[:, :], 1.0 / C, tmp[:, :], ALU.mult, ALU.subtract, r=[p2.b, tmp.b], w=[var.b])
        ts(st, "dve", var[:, :], var[:, :], 1e-5, ALU.add, r=[var.b], w=[var.b])
        act(st, var[:, :], var[:, :], AF.Sqrt, r=[var.b], w=[var.b])
        st.op("dve", lambda e: e.reciprocal(out=var[:, :], in_=var[:, :]), r=[var.b], w=[var.b])
        o_ = outr.next()
        for c in range(8):
            yc, sg = ycr.next(), sgr.next()
            tt(st, "pool", yc[:, :], cc[:, c, :], mean[:, :], ALU.subtract, r=[cc.b, mean.b], w=[yc.b])
            tt(st, "dve", yc[:, :], yc[:, :], var[:, :], ALU.mult, r=[yc.b, var.b], w=[yc.b])
            ts(st, "dve", yc[:, :], yc[:, :], colap(cols, "cln_g", c), ALU.mult, r=[yc.b, cols.b], w=[yc.b],
               s2=colap(cols, "cln_b", c), op1=ALU.add)
            act(st, sg[:, :], yc[:, :], AF.Sigmoid, r=[yc.b], w=[sg.b])
            tt(st, "pool", o_[:, c, :], yc[:, :], sg[:, :], ALU.mult, r=[yc.b, sg.b], w=[o_.b])
        st.dma("act", kb.d["YC"][:, t0:t0 + 512].rearrange("(c p) t -> p c t", p=128), o_[:, :, :], o_.b, r=[o_.b],
               w=[kb.bufs["YC"]], disjoint=True)
    st.finish()


def stage_merge(kb, l, xsrc):
    S = kb.S
    st = kb.stage(f"G{l}")
    k = load_consts(st, kb)
    cols = load_cols(st, kb, l)
    ones = k[:, K_ONES:K_ONES + 128]
    P, Pb = kb.d["P"], kb.bufs["P"]
    ybf = [st.ring(f"y{b}", 1, [128, 8, 512], BF16) for b in range(3)]
    wstB = st.ring("wstB", 2, [128, 8, 256])
    wbfB = st.ring("wbfB", 4, [128, 8, 256], BF16)
    wstO = st.ring("wstO", 2, [128, 16, 128])
    wbfO = st.ring("wbfO", 2, [128, 16, 128], BF16)
    mixed = st.sb("mixed", [128, 16, 512], BF16, nsub=16)
    res = st.sb("res", [128, 16, 512], F32, nsub=16)
    gts = st.ring("gt", 3, [128, 512])
    mts = st.ring("mt", 3, [128, 512])
    xts = st.ring("xt", 2, [128, 512])
    sqr = st.ring("sq", 2, [128, 512])
    mean, var, tmp = st.sb("mean", [128, 512]), st.sb("var", [128, 512]), st.sb("tmp", [128, 512])
    outr = st.ring("out", 3, [128, 512])
    psr = st.ring("ps", 6, [128, 512], psum=True)
    ps1, ps2 = st.ps("pstatA", [128, 512]), st.ps("pstatB", [128, 512])
    ynames = ("YA", "YB", "YC")
    xs, xsb = kb.d[xsrc], kb.bufs[xsrc]
    for tb in range(S // 512):
        t0 = tb * 512
        ys = []
        for b in range(3):
            y = ybf[b].next()
            st.dma("sp", y[:, :, :], kb.d[ynames[b]][:, t0:t0 + 512].rearrange("(c p) t -> p c t", p=128), y.b,
                   r=[kb.bufs[ynames[b]]], w=[y.b])
            ys.append(y)
        for g in range(8):
            wbs = []
            for b in range(3):
                ws = wstB.next()
                st.dma("sp", ws[:, :, :], kb.d["w_branch"][l, b][:, g * 256:(g + 1) * 256].rearrange(
                    "(kc p) c -> p kc c", p=128), ws.b, r=[kb.bufs["w_branch"]], w=[ws.b])
                wb = wbfB.next()
                cp(st, "act", wb[:, :, :], ws[:, :, :], r=[ws.b], w=[wb.b])
                wbs.append(wb)
            for dc in range(2):
                dch = g * 2 + dc
                ms = []
                for b in range(3):
                    gt = gts.next()
                    r0 = O_MG + b * D + dch * 128
                    st.dma("sp", gt[:, :], P[r0:r0 + 128, t0:t0 + 512], gt.b, r=[Pb], w=[gt.b])
                    act(st, gt[:, :], gt[:, :], AF.Sigmoid, r=[gt.b], w=[gt.b])
                    ps = psr.next()
                    for kc in range(8):
                        mm(st, ps[:, :], wbs[b][:, kc, dc * 128:(dc + 1) * 128], ys[b][:, kc, :], kc == 0, kc == 7,
                           r=[wbs[b].b, ys[b].b], w=[ps.b])
                    mt = mts.next()
                    tt(st, "dve", mt[:, :], ps[:, :], gt[:, :], ALU.mult, r=[ps.b, gt.b], w=[mt.b])
                    ms.append(mt)
                tt(st, "pool", ms[0][:, :], ms[0][:, :], ms[1][:, :], ALU.add, r=[ms[0].b, ms[1].b], w=[ms[0].b])
                tt(st, "pool", mixed[:, dch, :], ms[0][:, :], ms[2][:, :], ALU.add, r=[ms[0].b, ms[2].b],
                   w=[mixed.sub[dch]])
        for dch in range(16):
            ws = wstO.next()
            st.dma("sp", ws[:, :, :], kb.d["w_out"][l][:, dch * 128:(dch + 1) * 128].rearrange("(kc p) c -> p kc c", p=128),
                   ws.b, r=[kb.bufs["w_out"]], w=[ws.b])
            wb = wbfO.next()
            cp(st, "act", wb[:, :, :], ws[:, :, :], r=[ws.b], w=[wb.b])
            xt = xts.next()
            st.dma("sp", xt[:, :], xs[dch * 128:(dch + 1) * 128, t0:t0 + 512], xt.b, r=[xsb], w=[xt.b])
            ps = psr.next()
            for kc in range(16):
                mm(st, ps[:, :], wb[:, kc, :], mixed[:, kc, :], kc == 0, kc == 15, r=[wb.b, mixed.sub[kc]], w=[ps.b])
            stt(st, "dve", res[:, dch, :], xt[:, :], ALPHA, ps[:, :], ALU.mult, ALU.add, r=[xt.b, ps.b],
                w=[res.sub[dch]])
        for dch in range(16):
            sq = sqr.next()
            tt(st, "pool", sq[:, :], res[:, dch, :], res[:, dch, :], ALU.mult, r=[res.sub[dch]], w=[sq.b])
            mm(st, ps1[:, :], ones, res[:, dch, :], dch == 0, dch == 15, r=[k.b, res.sub[dch]], w=[ps1.b])
            mm(st, ps2[:, :], ones, sq[:, :], dch == 0, dch == 15, r=[k.b, sq.b], w=[ps2.b])
        st.op("act", lambda e: e.mul(out=mean[:, :], in_=ps1[:, :], mul=1.0 / D), r=[ps1.b], w=[mean.b])
        tt(st, "pool", tmp[:, :], mean[:, :], mean[:, :], ALU.mult, r=[mean.b], w=[tmp.b])
        stt(st, "dve", var[:, :], ps2[:, :], 1.0 / D, tmp[:, :], ALU.mult, ALU.subtract, r=[ps2.b, tmp.b], w=[var.b])
        ts(st, "dve", var[:, :], var[:, :], 1e-5, ALU.add, r=[var.b], w=[var.b])
        act(st, var[:, :], var[:, :], AF.Sqrt, r=[var.b], w=[var.b])
        st.op("dve", lambda e: e.reciprocal(out=var[:, :], in_=var[:, :]), r=[var.b], w=[var.b])
        for dch in range(16):
            o_ = outr.next()
            tt(st, "pool", o_[:, :], res[:, dch, :], mean[:, :], ALU.subtract, r=[res.sub[dch], mean.b], w=[o_.b])
            tt(st, "dve", o_[:, :], o_[:, :], var[:, :], ALU.mult, r=[o_.b, var.b], w=[o_.b])
            ts(st, "dve", o_[:, :], o_[:, :], colap(cols, "ln1_g", dch), ALU.mult, r=[o_.b, cols.b], w=[o_.b],
               s2=colap(cols, "ln1_b", dch), op1=ALU.add)
            st.dma("act", kb.d["X1T"][dch * 128:(dch + 1) * 128, t0:t0 + 512], o_[:, :], o_.b, r=[o_.b],
                   w=[kb.bufs["X1T"]], disjoint=True)
    st.finish()


def stage_router(kb, l):
    S = kb.S
    st = kb.stage(f"H1{l}")
    rw = st.sb("rw", [128, 16, NE])
    st.dma("sp", rw[:, :, :], kb.d["router_w"][l].rearrange("(kc p) e -> p kc e", p=128), rw.b,
           r=[kb.bufs["router_w"]], w=[rw.b])
    rb = st.sb("rb", [128, NE])
    st.dma("sp", rb[:, :], kb.d[f"rbbc{l}"][:, :], rb.b, r=[kb.bufs[f"rbbc{l}"]], w=[rb.b])
    xr = st.ring("x", 3, [128, 16, 128])
    lgr, er, mkr, gr_ = (st.ring(n_, 2, [128, NE]) for n_ in ("lg", "e", "mk", "g"))
    m8r = st.ring("m8", 2, [128, 8])
    smr = st.ring("sm", 2, [128, 2])
    psr = st.ring("ps", 4, [128, 512], psum=True)
    for ti in range(S // 128):
        t0 = ti * 128
        x = xr.next()
        st.dma("sp", x[:, :, :], kb.d["X1T"][:, t0:t0 + 128].rearrange("(c p) t -> p c t", p=128), x.b,
               r=[kb.bufs["X1T"]], w=[x.b])
        ps = psr.next()
        for kc in range(16):
            mm(st, ps[:, 0:NE], x[:, kc, :], rw[:, kc, :], kc == 0, kc == 15, r=[x.b, rw.b], w=[ps.b])
        lg, e_, mk, g, m8, sm = lgr.next(), er.next(), mkr.next(), gr_.next(), m8r.next(), smr.next()
        tt(st, "dve", lg[:, :], ps[:, 0:NE], rb[:, :], ALU.add, r=[ps.b, rb.b], w=[lg.b])
        st.op("dve", lambda e, m8=m8, lg=lg: e.max(out=m8[:, :], in_=lg[:, :]), r=[lg.b], w=[m8.b])
        ts(st, "dve", sm[:, 0:1], m8[:, 0:1], -1.0, ALU.mult, r=[m8.b], w=[sm.b])
        act(st, e_[:, :], lg[:, :], AF.Exp, r=[lg.b, sm.b], w=[e_.b], bias=sm[:, 0:1])
        ts(st, "dve", mk[:, :], lg[:, :], m8[:, 3:4], ALU.is_ge, r=[lg.b, m8.b], w=[mk.b])
        tt(st, "dve", e_[:, :], e_[:, :], mk[:, :], ALU.mult, r=[e_.b, mk.b], w=[e_.b])
        st.op("dve", lambda e, sm=sm, e_=e_: e.reduce_sum(out=sm[:, 1:2], in_=e_[:, :], axis=AX.X), r=[e_.b], w=[sm.b])
        st.op("dve", lambda e, sm=sm: e.reciprocal(out=sm[:, 1:2], in_=sm[:, 1:2]), r=[sm.b], w=[sm.b])
        ts(st, "dve", g[:, :], e_[:, :], sm[:, 1:2], ALU.mult, r=[e_.b, sm.b], w=[g.b])
        st.dma("act", kb.d["GT"][t0:t0 + 128, :], g[:, :], g.b, r=[g.b], w=[kb.bufs["GT"]], disjoint=True)
    st.finish()


def stage_moe(kb, l, last):
    S = kb.S
    st = kb.stage(f"H2{l}")
    k = load_consts(st, kb)
    ident = k[:, K_ID:K_ID + 128]
    TS = min(1024, S)
    NTL = TS // 128
    NTB = TS // 512
    x1b = st.sb("x1b", [128, 16, TS], BF16, nsub=16)
    acc = st.sb("acc", [128, NTL, D], F32, nsub=NTL)
    hbf = st.sb("h", [128, 8, TS], BF16, nsub=8)
    wst = st.ring("wst", 4, [128, 2048])
    wbf = st.ring("wbf", 2, [128, 8192], BF16)
    gates = st.sb("gates", [128, NTL, NE])
    gT = st.sb("gT", [32, TS])
    bgu = st.sb("bgu", [128, NE * 16])
    st.dma("sp", bgu[:, :], kb.d[f"bgu{l}"][:, :], bgu.b, r=[kb.bufs[f"bgu{l}"]], w=[bgu.b])
    gpr, sgr, upr = (st.ring(n_, 1, [128, 512]) for n_ in ("gp", "sg", "up"))
    stat = st.sb("stat", [128, 4, 6])
    mv = st.sb("mv", [128, 2])
    stg = None if last else st.ring("stg", 1, [128, 16, 128])
    psg = st.ring("psg", 2, [128, 512], psum=True)
    psu = st.ring("psu", 2, [128, 512], psum=True)
    psd = st.ring("psd", 2, [128, 512], psum=True)
    pst = st.ring("pst", 2, [128, 512], psum=True)
    Wgu, Wgub = kb.d["exp_w_gu"], kb.bufs["exp_w_gu"]
    Wd, Wdb = kb.d["exp_w_down"], kb.bufs["exp_w_down"]
    for sbi in range(S // TS):
        t0 = sbi * TS
        for kc4 in range(4):
            xts = []
            for q in range(4):
                kc = kc4 * 4 + q
                xt = wst.next()
                st.dma("sp", xt[:, 0:TS], kb.d["X1T"][kc * 128:(kc + 1) * 128, t0:t0 + TS], xt.b, r=[kb.bufs["X1T"]],
                       w=[xt.b])
                cp(st, "act" if kc % 2 else "dve", x1b[:, kc, :], xt[:, 0:TS], r=[xt.b], w=[x1b.sub[kc]])
                for tl_ in range(NTL):
                    pt = pst.next()
                    tr(st, pt[:, 0:128], xt[:, tl_ * 128:(tl_ + 1) * 128], ident, r=[xt.b, k.b], w=[pt.b])
                    st.op("act", lambda e, pt=pt, tl_=tl_, kc=kc: e.mul(out=acc[:, tl_, kc * 128:(kc + 1) * 128],
                                                                       in_=pt[:, 0:128], mul=ALPHA),
                          r=[pt.b], w=[acc.sub[tl_]])
        st.dma("sp", gates[:, :, :], kb.d["GT"][t0:t0 + TS, :].rearrange("(a p) e -> p a e", p=128), gates.b,
               r=[kb.bufs["GT"]], w=[gates.b])
        for tl_ in range(NTL):
            pt = pst.next()
            tr(st, pt[0:32, 0:128], gates[:, tl_, :], ident, r=[gates.b, k.b], w=[pt.b])
            cp(st, "act", gT[:, tl_ * 128:(tl_ + 1) * 128], pt[0:32, 0:128], r=[pt.b], w=[gT.b])
        bd_ = wst.next()
        st.dma("sp", bd_[0:32, :], kb.d["exp_b_down"][l], bd_.b, r=[kb.bufs["exp_b_down"]], w=[bd_.b])
        for tl_ in range(NTL):
            for blk in range(4):
                ps = psd.next()
                mm(st, ps[:, :], gT[:, tl_ * 128:(tl_ + 1) * 128], bd_[0:32, blk * 512:(blk + 1) * 512], True, True,
                   r=[gT.b, bd_.b], w=[ps.b])
                tt(st, "dve", acc[:, tl_, blk * 512:(blk + 1) * 512], ps[:, :], acc[:, tl_, blk * 512:(blk + 1) * 512],
                   ALU.add, r=[ps.b, acc.sub[tl_]], w=[acc.sub[tl_]])
        for e_i in range(NE):
            bcol = e_i * 16
            for g4 in range(4):
                wb = wbf.next()
                wbv = wb[:, :].rearrange("p (a b) -> p a b", b=512)
                for q in range(4):
                    ws = wst.next()
                    wsv = ws[:, :].rearrange("p (a b) -> p a b", b=512)
                    rows = slice(q * 512, (q + 1) * 512)
                    st.dma("sp", wsv[:, :, 0:256],
                           Wgu[l, e_i][rows, g4 * 256:(g4 + 1) * 256].rearrange("(kc p) c -> p kc c", p=128), ws.b,
                           r=[Wgub], w=[ws.b])
                    st.dma("sp", wsv[:, :, 256:512],
                           Wgu[l, e_i][rows, DFF + g4 * 256:DFF + (g4 + 1) * 256].rearrange("(kc p) c -> p kc c", p=128),
                           ws.b, r=[Wgub], w=[ws.b], group=True)
                    cp(st, "act", wb[:, q * 2048:(q + 1) * 2048], ws[:, :], r=[ws.b], w=[wb.b])
                for jj in range(2):
                    j = g4 * 2 + jj
                    for tb in range(NTB):
                        tsl = slice(tb * 512, (tb + 1) * 512)
                        pg, pu = psg.next(), psu.next()
                        for kc in range(16):
                            mm(st, pg[:, :], wbv[:, kc, jj * 128:(jj + 1) * 128], x1b[:, kc, tsl], kc == 0, kc == 15,
                               r=[wb.b, x1b.sub[kc]], w=[pg.b])
                        for kc in range(16):
                            mm(st, pu[:, :], wbv[:, kc, 256 + jj * 128:256 + (jj + 1) * 128], x1b[:, kc, tsl], kc == 0,
                               kc == 15, r=[wb.b, x1b.sub[kc]], w=[pu.b])
                        gp, sg, up = gpr.next(), sgr.next(), upr.next()
                        ts(st, "dve", gp[:, :], pg[:, :], bgu[:, bcol + j:bcol + j + 1], ALU.add, r=[pg.b, bgu.b],
                           w=[gp.b], s2=7.0, op1=ALU.min)
                        act(st, sg[:, :], gp[:, :], AF.Sigmoid, r=[gp.b], w=[sg.b], scale=1.702)
                        ts(st, "dve", up[:, :], pu[:, :], bgu[:, bcol + 8 + j:bcol + 9 + j], ALU.add, r=[pu.b, bgu.b],
                           w=[up.b], s2=7.0, op1=ALU.min)
                        ts(st, "dve", up[:, :], up[:, :], -7.0, ALU.max, r=[up.b], w=[up.b], s2=1.0, op1=ALU.add)
                        tt(st, "pool", gp[:, :], gp[:, :], sg[:, :], ALU.mult, r=[gp.b, sg.b], w=[gp.b])
                        tt(st, "pool", hbf[:, j, tsl], up[:, :], gp[:, :], ALU.mult, r=[up.b, gp.b], w=[hbf.sub[j]])
            for dg in range(2):
                wb = wbf.next()
                wbv = wb[:, :].rearrange("p (a b) -> p a b", b=1024)
                for q in range(4):
                    ws = wst.next()
                    st.dma("sp", ws[:, :].rearrange("p (a b) -> p a b", b=1024),
                           Wd[l, e_i][q * 256:(q + 1) * 256, dg * 1024:(dg + 1) * 1024].rearrange(
                               "(kc p) c -> p kc c", p=128), ws.b, r=[Wdb], w=[ws.b])
                    cp(st, "act", wb[:, q * 2048:(q + 1) * 2048], ws[:, :], r=[ws.b], w=[wb.b])
                for tl_ in range(NTL):
                    for hh in range(2):
                        c0 = dg * 1024 + hh * 512
                        ps = psd.next()
                        for kc in range(8):
                            mm(st, ps[:, :], hbf[:, kc, tl_ * 128:(tl_ + 1) * 128], wbv[:, kc, hh * 512:(hh + 1) * 512],
                               kc == 0, kc == 7, r=[hbf.sub[kc], wb.b], w=[ps.b])
                        stt(st, "dve", acc[:, tl_, c0:c0 + 512], ps[:, :], gates[:, tl_, e_i:e_i + 1],
                            acc[:, tl_, c0:c0 + 512], ALU.mult, ALU.add, r=[ps.b, gates.b, acc.sub[tl_]],
                            w=[acc.sub[tl_]])
        lg_ = wst.next()
        lb_ = wst.next()
        st.dma("sp", lg_[:, :], kb.d[f"ln2bc{l}"][:, 0:D], lg_.b, r=[kb.bufs[f"ln2bc{l}"]], w=[lg_.b])
        st.dma("sp", lb_[:, :], kb.d[f"ln2bc{l}"][:, D:2 * D], lb_.b, r=[kb.bufs[f"ln2bc{l}"]], w=[lb_.b])
        for tl_ in range(NTL):
            a_ = acc[:, tl_, :]
            ab = acc.sub[tl_]
            for i in range(4):
                st.op("dve", lambda e, i=i, tl_=tl_: e.bn_stats(out=stat[:, i, :], in_=acc[:, tl_, i * 512:(i + 1) * 512]),
                      r=[ab], w=[stat.b])
            st.op("dve", lambda e: e.bn_aggr(out=mv[:, :], in_=stat[:, :, :].rearrange("p a b -> p (a b)")),
                  r=[stat.b], w=[mv.b])
            ts(st, "dve", mv[:, 1:2], mv[:, 1:2], 1e-5, ALU.add, r=[mv.b], w=[mv.b])
            act(st, mv[:, 1:2], mv[:, 1:2], AF.Sqrt, r=[mv.b], w=[mv.b])
            st.op("dve", lambda e: e.reciprocal(out=mv[:, 1:2], in_=mv[:, 1:2]), r=[mv.b], w=[mv.b])
            ts(st, "dve", a_, a_, mv[:, 0:1], ALU.subtract, r=[ab, mv.b], w=[ab], s2=mv[:, 1:2], op1=ALU.mult)
            tt(st, "pool", a_, a_, lg_[:, :], ALU.mult, r=[ab, lg_.b], w=[ab])
            tt(st, "pool", a_, a_, lb_[:, :], ALU.add, r=[ab, lb_.b], w=[ab])
            if last:
                st.dma("act", kb.d["out"][t0 + tl_ * 128:t0 + (tl_ + 1) * 128, :], a_, ab, r=[ab],
                       w=[kb.bufs["out"]], disjoint=True)
            else:
                sg_ = stg.next()
                for kc4 in range(4):
                    pt = pst.next()
                    for q in range(4):
                        kc = kc4 * 4 + q
                        tr(st, pt[:, q * 128:(q + 1) * 128], acc[:, tl_, kc * 128:(kc + 1) * 128], ident, r=[ab, k.b],
                           w=[pt.b])
                    cp(st, "act", sg_[:, kc4 * 4:(kc4 + 1) * 4, :], pt[:, :].rearrange("p (a b) -> p a b", b=128),
                       r=[pt.b], w=[sg_.b])
                tsl = slice(t0 + tl_ * 128, t0 + (tl_ + 1) * 128)
                st.dma("act", kb.d["X2T"][:, tsl].rearrange("(c p) t -> p c t", p=128), sg_[:, :, :], sg_.b, r=[sg_.b],
                       w=[kb.bufs["X2T"]], disjoint=True)
    st.finish()


def build_program(S, L, debug=False, moe=True):
    kb = KB(S, L, debug=debug)
    declare(kb, moe=moe)
    src = "xT"
    for l in range(L):
        stage_inproj(kb, l, src)
        stage_rwkv_prep(kb, l)
        stage_rwkv_scan(kb, l)
        stage_rwkv_post(kb, l)
        stage_lru(kb, l)
        stage_conf(kb, l)
        stage_merge(kb, l, src)
        stage_router(kb, l)
        if moe:
            stage_moe(kb, l, last=(l == L - 1))
        src = "X2T"
    return kb


def kernel(**inputs):
    x = np.asarray(inputs["x"], np.float32)
    B, S, _ = x.shape
    L = inputs["w_in"].shape[0]
    kb = build_program(S, L)
    shared = {"d_" + k_: v for k_, v in make_inputs(inputs, L).items()}
    in_maps = []
    for b in range(B):
        m = dict(shared)
        m["d_xT"] = np.ascontiguousarray(x[b].T)
        in_maps.append(m)
    res = run_bass_kernel_spmd(kb.nc, in_maps, core_ids=list(range(B)))
    return np.stack([np.asarray(r["d_out"], np.float32) for r in res.results], axis=0)
```

```python
import contextlib
import math
import numpy as np
import concourse.bass as bass
import concourse.mybir as mybir
from concourse.bass_utils import run_bass_kernel_spmd

F32 = mybir.dt.float32
BF16 = mybir.dt.bfloat16
AF = mybir.ActivationFunctionType
ALU = mybir.AluOpType
AX = mybir.AxisListType
ENG = ("pe", "act", "dve", "pool", "sp")

D = 2048
C = 1024
NIN = 13600
NE = 32
DFF = 1024
KAPPA = math.exp(-0.5)
ALPHA = 4.0 ** 0.25
O_R, O_K, O_V, O_WL, O_AL, O_GL, O_LG, O_LX, O_CU, O_MG, O_VR = 0, 1024, 2048, 3072, 3136, 3200, 3360, 4384, 5408, 7456, 13600


class Buf:
    __slots__ = ("name", "we", "wd", "re", "rd", "sem", "cnt", "last")

    def __init__(self, name):
        self.name = name
        self.reset()

    def reset(self):
        self.we = {}
        self.wd = {}
        self.re = {}
        self.rd = {}
        self.sem = None
        self.cnt = 0
        self.last = None


class T:
    def __init__(self, t, name, nsub=0):
        self.t = t
        self.b = Buf(name)
        self.sub = [Buf(f"{name}.{i}") for i in range(nsub)]

    def __getitem__(self, k):
        return self.t[k]


class Ring:
    def __init__(self, items):
        self.items = items
        self.i = 0

    def next(self):
        r = self.items[self.i % len(self.items)]
        self.i += 1
        return r


class Stage:
    def __init__(self, kb, name):
        self.kb = kb
        self.nc = kb.nc
        self.name = name
        self.es = contextlib.ExitStack()
        self.streams = {e: [] for e in ENG}
        self.count = {e: 0 for e in ENG}
        self.waited = {e: {} for e in ENG}
        self.sems = []
        self.touched = []
        self.esem = {e: self._newsem(e) for e in ENG if e != "sp"}
        self.alt = 0

    def _newsem(self, nm):
        s = self.es.enter_context(self.nc.semaphore(f"{self.name}_{nm}_{len(self.sems)}"))
        self.sems.append(s)
        return len(self.sems) - 1

    def sb(self, name, shape, dt=F32, nsub=0):
        t = self.es.enter_context(self.nc.sbuf_tensor(f"{self.name}_{name}", list(shape), dt))
        return T(t, name, nsub)

    def ps(self, name, shape, dt=F32):
        t = self.es.enter_context(self.nc.psum_tensor(f"{self.name}_{name}", list(shape), dt))
        return T(t, name)

    def ring(self, name, n, shape, dt=F32, psum=False):
        mk = self.ps if psum else self.sb
        return Ring([mk(f"{name}{i}", shape, dt) for i in range(n)])

    def _touch(self, b):
        self.touched.append(b)

    def _deps(self, eng, reads, writes, disjoint=False):
        need = {}

        def add_e(d):
            for e2, n in d.items():
                if e2 == eng and eng == "pe":
                    continue
                s = self.esem[e2]
                if need.get(s, 0) < n:
                    need[s] = n

        def add_d(d):
            for s, v in d.items():
                if need.get(s, 0) < v:
                    need[s] = v

        for b in reads:
            add_e(b.we)
            add_d(b.wd)
        for b in writes:
            if not disjoint:
                add_e(b.we)
                add_d(b.wd)
            add_e(b.re)
            add_d(b.rd)
        out = []
        wt = self.waited[eng]
        for s, v in need.items():
            if wt.get(s, 0) >= v:
                continue
            wt[s] = v
            out.append((s, v))
        return out

    def op(self, eng, fn, r=(), w=()):
        waits = self._deps(eng, r, w)
        self.count[eng] += 1
        n = self.count[eng]
        si = self.esem[eng]
        sems = self.sems

        def run(e, fn=fn, waits=waits, si=si):
            for s, v in waits:
                e.wait_ge(sems[s], v)
            fn(e).then_inc(sems[si], 1)

        self.streams[eng].append(run)
        for b in r:
            b.re[eng] = n
            self._touch(b)
        for b in w:
            b.we = {eng: n}
            b.wd = {}
            b.re = {}
            b.rd = {}
            self._touch(b)

    def dma(self, q, out, in_, owner, r=(), w=(), disjoint=False, kw=None, group=False):
        disjoint = disjoint or group
        waits = self._deps(q, r, w, disjoint=disjoint)
        if owner.sem is None:
            owner.sem = self._newsem("d")
            owner.cnt = 0
            self._touch(owner)
        if owner.last is not None and not group:
            s, v = owner.last
            if self.waited[q].get(s, 0) < v:
                self.waited[q][s] = v
                waits.append((s, v))
        owner.cnt += 16
        ev = (owner.sem, owner.cnt)
        owner.last = ev
        sems = self.sems
        kw = kw or {}

        def run(e, waits=waits, ev=ev, out=out, in_=in_):
            for s, v in waits:
                e.wait_ge(sems[s], v)
            e.dma_start(out=out, in_=in_, **kw).then_inc(sems[ev[0]], 16)

        self.streams[q].append(run)
        for b in r:
            b.rd[ev[0]] = ev[1]
            self._touch(b)
        for b in w:
            if disjoint:
                b.wd[ev[0]] = ev[1]
            else:
                b.we = {}
                b.wd = {ev[0]: ev[1]}
                b.re = {}
                b.rd = {}
            self._touch(b)

    def aeng(self, choices=("dve", "pool")):
        self.alt += 1
        return choices[self.alt % len(choices)]

    def finish(self):
        nc = self.nc
        sems = self.sems
        finals = []
        seen = set()
        for b in self.touched:
            if b.sem is not None and b.sem not in seen:
                seen.add(b.sem)
                finals.append((b.sem, b.cnt))
        for e in ENG:
            if e != "sp" and self.count[e] > 0:
                finals.append((self.esem[e], self.count[e]))

        def fin(e):
            for s, v in finals:
                e.wait_ge(sems[s], v)

        self.streams["sp"].append(fin)
        streams = self.streams
        with nc.Block(self.name) as block:
            @block.sync
            def _(e):
                for f in streams["sp"]:
                    f(e)

            @block.tensor
            def _(e):
                for f in streams["pe"]:
                    f(e)

            @block.scalar
            def _(e):
                for f in streams["act"]:
                    f(e)

            @block.vector
            def _(e):
                for f in streams["dve"]:
                    f(e)

            @block.gpsimd
            def _(e):
                for f in streams["pool"]:
                    f(e)
        with nc.Block(self.name + "_clr") as blk:
            @blk.gpsimd
            def _(e):
                for s_ in sems:
                    e.sem_clear(s_)
        for b in self.touched:
            b.reset()
        self.es.close()


def tt(st, eng, out, in0, in1, op, r, w):
    st.op(eng, lambda e: e.tensor_tensor(out=out, in0=in0, in1=in1, op=op), r=r, w=w)


def ts(st, eng, out, in0, s1, op0, r, w, s2=None, op1=None):
    if op1 is None:
        st.op(eng, lambda e: e.tensor_scalar(out=out, in0=in0, scalar1=s1, scalar2=None, op0=op0), r=r, w=w)
    else:
        st.op(eng, lambda e: e.tensor_scalar(out=out, in0=in0, scalar1=s1, scalar2=s2, op0=op0, op1=op1), r=r, w=w)


def stt(st, eng, out, in0, scalar, in1, op0, op1, r, w):
    st.op(eng, lambda e: e.scalar_tensor_tensor(out=out, in0=in0, scalar=scalar, in1=in1, op0=op0, op1=op1), r=r, w=w)


def act(st, out, in_, func, r, w, bias=None, scale=None):
    kw = {}
    if bias is not None:
        kw["bias"] = bias
    if scale is not None:
        kw["scale"] = scale
    st.op("act", lambda e: e.activation(out=out, in_=in_, func=func, **kw), r=r, w=w)


def cp(st, eng, out, in_, r, w):
    if eng == "act":
        st.op("act", lambda e: e.copy(out=out, in_=in_), r=r, w=w)
    else:
        st.op(eng, lambda e: e.tensor_copy(out=out, in_=in_), r=r, w=w)


def mm(st, out, lhsT, rhs, start, stop, r, w):
    st.op("pe", lambda e: e.matmul(out, lhsT, rhs, start=start, stop=stop), r=r, w=w)


F32R = mybir.dt.float32r


def mmr(st, out, lhsT, rhs, start, stop, r, w):
    st.op("pe", lambda e: e.matmul(out, lhsT.bitcast(F32R), rhs.bitcast(F32R), start=start, stop=stop), r=r, w=w)


def tr(st, out, in_, ident, r, w):
    st.op("pe", lambda e: e.transpose(out, in_, ident), r=r, w=w)


def col_table():
    tab = {}
    off = 0
    for nm, n in [("mu_r", 8), ("mu_k", 8), ("mu_v", 8), ("w0", 8), ("a0", 8), ("v0", 8), ("k_k", 8), ("k_a", 8),
                  ("r_k", 8), ("gn_g", 8), ("gn_b", 8), ("lconv_b", 8), ("l_ba", 8), ("l_bx", 8), ("l_lam", 8),
                  ("lconv_w", 32), ("cconv_w", 248), ("cconv_b", 8), ("cln_g", 8), ("cln_b", 8),
                  ("ln1_g", 16), ("ln1_b", 16), ("mu_wl", 1), ("mu_al", 1), ("mu_gl", 2), ("mu_vr", 1)]:
        tab[nm] = (off, n)
        off += n
    return tab, off


COLT, NCOLS = col_table()


def chunkcols(v):
    v = np.asarray(v, np.float32).reshape(-1)
    n = v.shape[0]
    m = (n + 127) // 128
    buf = np.zeros((m * 128,), np.float32)
    buf[:n] = v
    return buf.reshape(m, 128).T


def host_layout(inp, l):
    cols = np.zeros((128, NCOLS), np.float32)

    def put(nm, arr):
        o, n = COLT[nm]
        cols[:, o:o + n] = arr

    mu = inp["shift_mu"][l]
    put("mu_r", chunkcols(mu[0:1024]))
    put("mu_k", chunkcols(mu[1024:2048]))
    put("mu_v", chunkcols(mu[2048:3072]))
    put("mu_wl", chunkcols(mu[3072:3136]))
    put("mu_al", chunkcols(mu[3136:3200]))
    put("mu_gl", chunkcols(mu[3200:3360]))
    put("w0", chunkcols(inp["rwkv_w0"][l]))
    put("a0", chunkcols(inp["rwkv_a0"][l]))
    if l > 0:
        put("v0", chunkcols(inp["rwkv_v0"][l - 1]))
        put("mu_vr", chunkcols(inp["shift_mu_vres"][l - 1]))
    put("k_k", chunkcols(inp["rwkv_k_k"][l]))
    put("k_a", chunkcols(inp["rwkv_k_a"][l]))
    put("r_k", chunkcols(inp["rwkv_r_k"][l].reshape(-1)))
    put("gn_g", chunkcols(inp["rwkv_gn_g"][l]))
    put("gn_b", chunkcols(inp["rwkv_gn_b"][l]))
    put("lconv_b", chunkcols(inp["lru_conv_b"][l]))
    put("l_ba", chunkcols(inp["lru_ba"][l]))
    put("l_bx", chunkcols(inp["lru_bx"][l]))
    put("l_lam", chunkcols(inp["lru_lambda"][l]))
    lw = inp["lru_conv_w"][l]
    put("lconv_w", lw.reshape(4, 8, 128).transpose(2, 1, 0).reshape(128, 32))
    cw = inp["conf_conv_w"][l]
    put("cconv_w", cw.reshape(31, 8, 128).transpose(2, 1, 0).reshape(128, 248))
    put("cconv_b", chunkcols(inp["conf_conv_b"][l]))
    put("cln_g", chunkcols(inp["conf_ln_g"][l]))
    put("cln_b", chunkcols(inp["conf_ln_b"][l]))
    put("ln1_g", chunkcols(inp["ln1_g"][l]))
    put("ln1_b", chunkcols(inp["ln1_b"][l]))
    bd = np.zeros((8, 128, 256), np.float32)
    for c in range(8):
        for hh in range(2):
            bd[c, hh * 64:(hh + 1) * 64, hh * 64:(hh + 1) * 64] = inp["lru_wa"][l][2 * c + hh]
            bd[c, hh * 64:(hh + 1) * 64, 128 + hh * 64:128 + (hh + 1) * 64] = inp["lru_wx"][l][2 * c + hh]
    bgu = np.ascontiguousarray(inp["exp_b_gu"][l].reshape(NE, 16, 128).transpose(2, 0, 1)).reshape(128, NE * 16)
    ln2 = np.ascontiguousarray(np.broadcast_to(
        np.concatenate([inp["ln2_g"][l], inp["ln2_b"][l]])[None, :], (128, 2 * D))).astype(np.float32)
    rb = np.ascontiguousarray(np.broadcast_to(inp["router_b"][l][None, :], (128, NE))).astype(np.float32)
    return {"cols": cols, "lrubd": bd, "bgu": bgu, "ln2bc": ln2, "rbbc": rb}


def host_consts():
    ident = np.eye(128, dtype=np.float32)
    ones = np.ones((128, 128), np.float32)
    bones = np.zeros((128, 128), np.float32)
    bones[:64, :64] = 1
    bones[64:, 64:] = 1
    s = np.arange(128)[:, None]
    t = np.arange(128)[None, :]
    m_su = (s < t).astype(np.float32)
    m_ui = (s <= t).astype(np.float32)
    m_sl = (s > t).astype(np.float32)
    cmask = np.ones((128, 512), np.float32)
    cmask[:, ::128] = 0
    return np.concatenate([ident, ones, bones, m_su, m_ui, m_sl, cmask], axis=1)


K_ID, K_ONES, K_BONES, K_SU, K_UI, K_SL, K_CM = 0, 128, 256, 384, 512, 640, 768
NCONST = 768 + 512


class KB:
    def __init__(self, S, L, debug=False):
        self.S = S
        self.L = L
        self.debug = debug
        self.nc = bass.Bass("TRN2", target_bir_lowering=False)
        self.d = {}
        self.bufs = {}

    def din(self, name, shape, dt=F32):
        self.d[name] = self.nc.dram_tensor("d_" + name, list(shape), dt, kind="ExternalInput").ap()
        self.bufs[name] = Buf(name)

    def dout(self, name, shape, dt=F32):
        self.d[name] = self.nc.dram_tensor("d_" + name, list(shape), dt, kind="ExternalOutput").ap()
        self.bufs[name] = Buf(name)

    def dscr(self, name, shape, dt=F32):
        kind = "ExternalOutput" if (self.debug and name in self.debug) else "Internal"
        self.d[name] = self.nc.dram_tensor("d_" + name, list(shape), dt, kind=kind).ap()
        self.bufs[name] = Buf(name)

    def stage(self, name):
        return Stage(self, name)


def load_consts(st, kb):
    k = st.sb("konst", [128, NCONST], F32)
    st.dma("sp", k[:, :], kb.d["konst"][:, :], k.b, r=[kb.bufs["konst"]], w=[k.b])
    return k


def load_cols(st, kb, l):
    c = st.sb("cols", [128, NCOLS], F32)
    st.dma("sp", c[:, :], kb.d[f"cols{l}"][:, :], c.b, r=[kb.bufs[f"cols{l}"]], w=[c.b])
    return c


def colap(cols, nm, j=0, rows=128):
    o, n = COLT[nm]
    return cols[0:rows, o + j:o + j + 1]


def stage_inproj(kb, l, xsrc):
    S = kb.S
    st = kb.stage(f"A{l}")
    TS = min(2048, S)
    nsb = S // TS
    ntb = TS // 512
    xbf = st.sb("xbf", [128, 16, TS], BF16, nsub=16)
    xst = st.ring("xst", 2, [128, TS])
    wst = st.ring("wst", 2, [128, 16, 256])
    wbf = st.ring("wbf", 2, [128, 16, 256], BF16)
    ost = st.ring("ost", 4, [128, 512])
    pss = st.ring("ps", 4, [128, 512], psum=True)
    P = kb.d["P"]
    Pb = kb.bufs["P"]
    groups = [(kb.d["w_in"][l], kb.bufs["w_in"], c0, min(256, NIN - c0), c0) for c0 in range(0, NIN, 256)]
    if l > 0:
        groups.append((kb.d["w_in_vres"][l - 1], kb.bufs["w_in_vres"], 0, 32, O_VR))
    xs, xsb = kb.d[xsrc], kb.bufs[xsrc]

    def load_w(g):
        W, Wb, c0, gc, prow = g
        ws = wst.next()
        st.dma("sp", ws[:, :, 0:gc], W[:, c0:c0 + gc].rearrange("(kc p) c -> p kc c", p=128), ws.b, r=[Wb], w=[ws.b])
        wb = wbf.next()
        cp(st, "pool", wb[:, :, 0:gc], ws[:, :, 0:gc], r=[ws.b], w=[wb.b])
        return wb

    for sbi in range(nsb):
        t0 = sbi * TS
        for kc in range(16):
            xt = xst.next()
            st.dma("sp", xt[:, :], xs[kc * 128:(kc + 1) * 128, t0:t0 + TS], xt.b, r=[xsb], w=[xt.b])
            cp(st, "act" if kc % 2 else "dve", xbf[:, kc, :], xt[:, :], r=[xt.b], w=[xbf.sub[kc]])
        nxt = load_w(groups[0])
        for gi, g in enumerate(groups):
            wb = nxt
            if gi + 1 < len(groups):
                nxt = load_w(groups[gi + 1])
            _, _, c0, gc, prow = g
            for cc in range(0, gc, 128):
                ncol = min(128, gc - cc)
                for tb in range(ntb):
                    ps = pss.next()
                    for kc in range(16):
                        mm(st, ps[0:ncol, :], wb[:, kc, cc:cc + ncol], xbf[:, kc, tb * 512:(tb + 1) * 512],
                           kc == 0, kc == 15, r=[wb.b, xbf.sub[kc]], w=[ps.b])
                    o = ost.next()
                    cp(st, "dve", o[0:ncol, :], ps[0:ncol, :], r=[ps.b], w=[o.b])
                    st.dma("act", P[prow + cc:prow + cc + ncol, t0 + tb * 512:t0 + (tb + 1) * 512], o[0:ncol, :], o.b,
                           r=[o.b], w=[Pb], disjoint=True)
    st.finish()


def load_halo(st, dst, src, rows, t0, n, h, srcbuf, q="sp"):
    if t0 == 0:
        st.op("pool", lambda e: e.memset(dst[0:rows, 0:h], 0.0), r=[], w=[dst.b])
        st.dma(q, dst[0:rows, h:h + n], src[:, 0:n], dst.b, r=[srcbuf], w=[dst.b])
    else:
        st.dma(q, dst[0:rows, 0:h + n], src[:, t0 - h:t0 + n], dst.b, r=[srcbuf], w=[dst.b])


def stage_rwkv_prep(kb, l):
    S = kb.S
    st = kb.stage(f"B{l}")
    k = load_consts(st, kb)
    cols = load_cols(st, kb, l)
    P, Pb = kb.d["P"], kb.bufs["P"]
    ntb = S // 512
    bones = k[:, K_BONES:K_BONES + 128]
    cmask = k[:, K_CM:K_CM + 512]
    w2 = st.sb("w2", [64, C])
    a2 = st.sb("a2", [64, C])
    g2a = st.sb("g2a", [128, C])
    g2b = st.sb("g2b", [32, C])
    st.dma("sp", w2[:, :], kb.d["rwkv_w2"][l], w2.b, r=[kb.bufs["rwkv_w2"]], w=[w2.b])
    st.dma("sp", a2[:, :], kb.d["rwkv_a2"][l], a2.b, r=[kb.bufs["rwkv_a2"]], w=[a2.b])
    st.dma("sp", g2a[:, :], kb.d["rwkv_g2"][l][0:128, :], g2a.b, r=[kb.bufs["rwkv_g2"]], w=[g2a.b])
    st.dma("sp", g2b[:, :], kb.d["rwkv_g2"][l][128:160, :], g2b.b, r=[kb.bufs["rwkv_g2"]], w=[g2b.b])
    if l > 0:
        v2 = st.sb("v2", [32, C])
        st.dma("sp", v2[:, :], kb.d["rwkv_v2"][l - 1], v2.b, r=[kb.bufs["rwkv_v2"]], w=[v2.b])

    def R(name, n=2, shape=(128, 512)):
        return st.ring(name, n, list(shape))

    lraw = R("lraw", 2, (128, 513))
    ld = R("ld", 1)
    LW, LA, LG1, LG2, LV = R("LW", 2), R("LA", 2), R("LG1", 2), R("LG2", 2), R("LV", 2)
    raw = {n: R("raw" + n, 2, (128, 513)) for n in "rkv"}
    names = ["dm", "rp", "kp", "vp", "sg", "ag", "kk", "kk2", "nrm", "kkn", "u", "kmod", "rkp", "bvec", "cs", "d3",
             "d4", "E1", "E2", "E3", "E4", "o_r", "o_a", "o_b", "o_k", "o_bh", "o_kh", "o_g", "o_rkv", "vf", "sv"]
    tl = {n: R(n, 2 if n.startswith("o_") else 1) for n in names}
    psr = st.ring("ps", 6, [128, 512], psum=True)

    def mix(dst, src, mucol, rows):
        d = ld.next()
        tt(st, "pool", d[0:rows, :], src[0:rows, 0:512], src[0:rows, 1:513], ALU.subtract, r=[src.b], w=[d.b])
        stt(st, "dve", dst[0:rows, :], d[0:rows, :], mucol, src[0:rows, 1:513], ALU.mult, ALU.add,
            r=[d.b, src.b, cols.b], w=[dst.b])

    def store(dname, row0, t0, tile_, rows=128, n=512):
        st.dma("act", kb.d[dname][row0:row0 + rows, t0:t0 + n], tile_[0:rows, 0:n], tile_.b, r=[tile_.b],
               w=[kb.bufs[dname]], disjoint=True)

    for tb in range(ntb):
        t0 = tb * 512
        x = lraw.next()
        load_halo(st, x, P[O_WL:O_WL + 64, :], 64, t0, 512, 1, Pb)
        lw = LW.next()
        mix(lw, x, colap(cols, "mu_wl", 0, 64), 64)
        act(st, lw[0:64, :], lw[0:64, :], AF.Tanh, r=[lw.b], w=[lw.b])
        x = lraw.next()
        load_halo(st, x, P[O_AL:O_AL + 64, :], 64, t0, 512, 1, Pb)
        la = LA.next()
        mix(la, x, colap(cols, "mu_al", 0, 64), 64)
        x = lraw.next()
        load_halo(st, x, P[O_GL:O_GL + 128, :], 128, t0, 512, 1, Pb)
        lg1 = LG1.next()
        mix(lg1, x, colap(cols, "mu_gl", 0, 128), 128)
        act(st, lg1[:, :], lg1[:, :], AF.Sigmoid, r=[lg1.b], w=[lg1.b])
        x = lraw.next()
        load_halo(st, x, P[O_GL + 128:O_GL + 160, :], 32, t0, 512, 1, Pb)
        lg2 = LG2.next()
        mix(lg2, x, colap(cols, "mu_gl", 1, 32), 32)
        act(st, lg2[0:32, :], lg2[0:32, :], AF.Sigmoid, r=[lg2.b], w=[lg2.b])
        if l > 0:
            x = lraw.next()
            load_halo(st, x, P[O_VR:O_VR + 32, :], 32, t0, 512, 1, Pb)
            lv = LV.next()
            mix(lv, x, colap(cols, "mu_vr", 0, 32), 32)
        for c in range(8):
            cs_ = slice(c * 128, (c + 1) * 128)
            rw = {}
            for n_, off in (("r", O_R), ("k", O_K), ("v", O_V)):
                rw[n_] = raw[n_].next()
                load_halo(st, rw[n_], P[off + c * 128:off + (c + 1) * 128, :], 128, t0, 512, 1, Pb)
            rp, kp, vp = tl["rp"].next(), tl["kp"].next(), tl["vp"].next()
            mix(rp, rw["r"], colap(cols, "mu_r", c), 128)
            mix(kp, rw["k"], colap(cols, "mu_k", c), 128)
            mix(vp, rw["v"], colap(cols, "mu_v", c), 128)
            ps = psr.next()
            mm(st, ps[:, :], w2[0:64, cs_], lw[0:64, :], True, True, r=[w2.b, lw.b], w=[ps.b])
            sg = tl["sg"].next()
            act(st, sg[:, :], ps[:, :], AF.Sigmoid, r=[ps.b, cols.b], w=[sg.b], bias=colap(cols, "w0", c))
            ps = psr.next()
            mm(st, ps[:, :], a2[0:64, cs_], la[0:64, :], True, True, r=[a2.b, la.b], w=[ps.b])
            ag = tl["ag"].next()
            act(st, ag[:, :], ps[:, :], AF.Sigmoid, r=[ps.b, cols.b], w=[ag.b], bias=colap(cols, "a0", c))
            ps = psr.next()
            mm(st, ps[:, :], g2a[:, cs_], lg1[:, :], True, False, r=[g2a.b, lg1.b], w=[ps.b])
            mm(st, ps[:, :], g2b[0:32, cs_], lg2[0:32, :], False, True, r=[g2b.b, lg2.b], w=[ps.b])
            og = tl["o_g"].next()
            cp(st, "act", og[:, :], ps[:, :], r=[ps.b], w=[og.b])
            store("G", c * 128, t0, og)
            if l > 0:
                ps = psr.next()
                mm(st, ps[:, :], v2[0:32, cs_], lv[0:32, :], True, True, r=[v2.b, lv.b], w=[ps.b])
                sv = tl["sv"].next()
                act(st, sv[:, :], ps[:, :], AF.Sigmoid, r=[ps.b, cols.b], w=[sv.b], bias=colap(cols, "v0", c))
                vf = tl["vf"].next()
                st.dma("sp", vf[:, :], kb.d["VF"][cs_, t0:t0 + 512], vf.b, r=[kb.bufs["VF"]], w=[vf.b])
                tt(st, "pool", vf[:, :], vf[:, :], vp[:, :], ALU.subtract, r=[vf.b, vp.b], w=[vf.b])
                tt(st, "dve", vf[:, :], vf[:, :], sv[:, :], ALU.mult, r=[vf.b, sv.b], w=[vf.b])
                tt(st, "pool", vp[:, :], vp[:, :], vf[:, :], ALU.add, r=[vp.b, vf.b], w=[vp.b])
            else:
                store("VF", c * 128, t0, vp)
            store("SV", c * 128, t0, vp)
            kk, kk2 = tl["kk"].next(), tl["kk2"].next()
            ts(st, "dve", kk[:, :], kp[:, :], colap(cols, "k_k", c), ALU.mult, r=[kp.b, cols.b], w=[kk.b])
            tt(st, "pool", kk2[:, :], kk[:, :], kk[:, :], ALU.mult, r=[kk.b], w=[kk2.b])
            ps = psr.next()
            mm(st, ps[:, :], bones, kk2[:, :], True, True, r=[k.b, kk2.b], w=[ps.b])
            nrm = tl["nrm"].next()
            act(st, nrm[:, :], ps[:, :], AF.Sqrt, r=[ps.b], w=[nrm.b])
            ts(st, "dve", nrm[:, :], nrm[:, :], 1e-12, ALU.max, r=[nrm.b], w=[nrm.b])
            st.op("dve", lambda e, o=nrm: e.reciprocal(out=o[:, :], in_=o[:, :]), r=[nrm.b], w=[nrm.b])
            kkn = tl["kkn"].next()
            tt(st, "pool", kkn[:, :], kk[:, :], nrm[:, :], ALU.mult, r=[kk.b, nrm.b], w=[kkn.b])
            u, kmod = tl["u"].next(), tl["kmod"].next()
            ts(st, "dve", u[:, :], ag[:, :], -1.0, ALU.add, r=[ag.b, cols.b], w=[u.b], s2=colap(cols, "k_a", c),
               op1=ALU.mult)
            stt(st, "dve", kmod[:, :], u[:, :], 1.0, kp[:, :], ALU.add, ALU.mult, r=[u.b, kp.b], w=[kmod.b])
            rkp = tl["rkp"].next()
            stt(st, "dve", rkp[:, :], rp[:, :], colap(cols, "r_k", c), kmod[:, :], ALU.mult, ALU.mult,
                r=[rp.b, kmod.b, cols.b], w=[rkp.b])
            ps = psr.next()
            mm(st, ps[:, :], bones, rkp[:, :], True, True, r=[k.b, rkp.b], w=[ps.b])
            orkv = tl["o_rkv"].next()
            tt(st, "dve", orkv[:, :], ps[:, :], vp[:, :], ALU.mult, r=[ps.b, vp.b], w=[orkv.b])
            store("RKV", c * 128, t0, orkv)
            bvec = tl["bvec"].next()
            tt(st, "pool", bvec[:, :], kkn[:, :], ag[:, :], ALU.mult, r=[kkn.b, ag.b], w=[bvec.b])
            cs = tl["cs"].next()
            st.op("dve", lambda e, o=cs, s=sg: e.tensor_tensor_scan(out=o[:, :], data0=cmask, data1=s[:, :],
                                                                    initial=0.0, op0=ALU.mult, op1=ALU.add),
                  r=[k.b, sg.b], w=[cs.b])
            E1, E2, E3, E4 = tl["E1"].next(), tl["E2"].next(), tl["E3"].next(), tl["E4"].next()
            act(st, E1[:, :], cs[:, :], AF.Exp, r=[cs.b], w=[E1.b], scale=-KAPPA)
            act(st, E2[:, :], cs[:, :], AF.Exp, r=[cs.b], w=[E2.b], scale=KAPPA)
            d3, d4 = tl["d3"].next(), tl["d4"].next()
            tt(st, "pool", d3[:, :], cs[:, :], sg[:, :], ALU.subtract, r=[cs.b, sg.b], w=[d3.b])
            act(st, E3[:, :], d3[:, :], AF.Exp, r=[d3.b], w=[E3.b], scale=-KAPPA)
            for ci in range(4):
                ts(st, "dve", d4[:, ci * 128:(ci + 1) * 128], cs[:, ci * 128:(ci + 1) * 128],
                   cs[:, ci * 128 + 127:ci * 128 + 128], ALU.subtract, r=[cs.b], w=[d4.b])
            act(st, E4[:, :], d4[:, :], AF.Exp, r=[d4.b], w=[E4.b], scale=KAPPA)
            o = tl["o_r"].next()
            tt(st, "pool", o[:, :], rp[:, :], E1[:, :], ALU.mult, r=[rp.b, E1.b], w=[o.b])
            store("SR", c * 128, t0, o)
            o = tl["o_a"].next()
            stt(st, "dve", o[:, :], kkn[:, :], -1.0, E3[:, :], ALU.mult, ALU.mult, r=[kkn.b, E3.b], w=[o.b])
            store("SA", c * 128, t0, o)
            o = tl["o_b"].next()
            tt(st, "pool", o[:, :], bvec[:, :], E2[:, :], ALU.mult, r=[bvec.b, E2.b], w=[o.b])
            store("SB", c * 128, t0, o)
            o = tl["o_k"].next()
            tt(st, "dve", o[:, :], kmod[:, :], E2[:, :], ALU.mult, r=[kmod.b, E2.b], w=[o.b])
            store("SK", c * 128, t0, o)
            o = tl["o_bh"].next()
            tt(st, "pool", o[:, :], bvec[:, :], E4[:, :], ALU.mult, r=[bvec.b, E4.b], w=[o.b])
            store("SBH", c * 128, t0, o)
            o = tl["o_kh"].next()
            tt(st, "dve", o[:, :], kmod[:, :], E4[:, :], ALU.mult, r=[kmod.b, E4.b], w=[o.b])
            store("SKH", c * 128, t0, o)
            st.dma("act", kb.d["GL"][cs_, tb * 4:tb * 4 + 4], E1[:, 127:512:128], E1.b, r=[E1.b], w=[kb.bufs["GL"]],
                   disjoint=True, kw={"allow_slow_non_contiguous": True})
    st.finish()


def stage_rwkv_scan(kb, l, G=2, upto=99):
    S = kb.S
    st = kb.stage(f"C{l}")
    k = load_consts(st, kb)
    ident64 = k[0:64, K_ID:K_ID + 64]
    NCH = S // 128
    NB = S // 256
    slots = []
    for g in range(G):
        d = {}
        d["AR"] = st.ring(f"AR{g}", 2, [64, 2, 256])
        d["BK"] = st.ring(f"BK{g}", 2, [64, 2, 256])
        d["HV"] = st.ring(f"HV{g}", 2, [64, 3, 256])
        d["YS"] = st.ring(f"YS{g}", 2, [64, 256])
        d["GL"] = st.sb(f"GL{g}", [64, NCH])
        d["N0RB"] = st.sb(f"N0RB{g}", [128, 256])
        d["AKRK"] = st.sb(f"AKRK{g}", [128, 256])
        d["N"] = [st.sb(f"N{g}_{i}", [128, 128], F32R) for i in range(0, 7)]
        d["M"] = [st.sb(f"M{g}_{i}", [128, 128], F32R) for i in range(2)]
        d["VBK"] = st.sb(f"VBK{g}", [128, 192])
        d["U"] = [st.sb(f"U{g}_{i}", [128, 64]) for i in range(2)]
        d["H"] = [st.sb(f"H{g}_{i}", [64, 64]) for i in range(2)]
        pa = st.ps(f"pa{g}", [128, 512])
        pb = st.ps(f"pb{g}", [128, 512])
        pc = st.ps(f"pc{g}", [128, 512])
        pd = st.ps(f"pd{g}", [128, 512])
        d["p_s1"] = (pa, slice(0, 256), pa.b)
        d["p_s2"] = (pa, slice(256, 512), pa.b)
        d["p_m"] = (pb, slice(0, 128), pb.b)
        d["p_t"] = (pb, slice(128, 320), pb.b)
        d["p_w"] = (pb, slice(320, 384), pb.b)
        d["p_n"] = (pc, slice(0, 128), pc.b)
        d["p_mm"] = (pc, slice(128, 256), pc.b)
        d["p_u"] = (pd, slice(0, 64), pd.b)
        d["p_h"] = (pd, slice(64, 128), pd.b)
        d["p_y"] = (pd, slice(128, 256), pd.b)
        slots.append(d)
    m_a = k[:, K_SU:K_SU + 256]
    m_sl = k[:, K_SL:K_SL + 128]

    def P_(d, nm, rows=128):
        t_, sl, b = d[nm]
        return t_[0:rows, sl], b

    for h0 in range(0, 16, G):
        heads = list(range(h0, min(16, h0 + G)))
        hs = {}
        for gi, h in enumerate(heads):
            d = slots[gi]
            r0 = h * 64
            st.dma("sp", d["GL"][:, :], kb.d["GL"][r0:r0 + 64, :], d["GL"].b, r=[kb.bufs["GL"]], w=[d["GL"].b])
            st.op("pool", lambda e, d=d: e.memset(d["H"][0][:, :], 0.0), r=[], w=[d["H"][0].b])
            hs[h] = {"hi": 0}
        for bi in range(NB):
            t0 = bi * 256
            cur = {}
            for gi, h in enumerate(heads):
                d = slots[gi]
                r0 = h * 64
                AR, BK, HV, YS = d["AR"].next(), d["BK"].next(), d["HV"].next(), d["YS"].next()

                def ld(dst, nm):
                    st.dma("sp", dst, kb.d[nm][r0:r0 + 64, t0:t0 + 256], dstb, r=[kb.bufs[nm]], w=[dstb])

                dstb = AR.b
                st.dma("sp", AR[:, :, 0:128], kb.d["SA"][r0:r0 + 64, t0:t0 + 256].rearrange("p (c t) -> p c t", t=128),
                       AR.b, r=[kb.bufs["SA"]], w=[AR.b])
                st.dma("sp", AR[:, :, 128:256], kb.d["SR"][r0:r0 + 64, t0:t0 + 256].rearrange("p (c t) -> p c t", t=128),
                       AR.b, r=[kb.bufs["SR"]], w=[AR.b], group=True)
                st.dma("sp", BK[:, 0, :], kb.d["SB"][r0:r0 + 64, t0:t0 + 256], BK.b, r=[kb.bufs["SB"]], w=[BK.b])
                st.dma("sp", BK[:, 1, :], kb.d["SK"][r0:r0 + 64, t0:t0 + 256], BK.b, r=[kb.bufs["SK"]], w=[BK.b], group=True)
                st.dma("sp", HV[:, 0, :], kb.d["SBH"][r0:r0 + 64, t0:t0 + 256], HV.b, r=[kb.bufs["SBH"]], w=[HV.b])
                st.dma("sp", HV[:, 1, :], kb.d["SKH"][r0:r0 + 64, t0:t0 + 256], HV.b, r=[kb.bufs["SKH"]], w=[HV.b], group=True)
                st.dma("sp", HV[:, 2, :], kb.d["SV"][r0:r0 + 64, t0:t0 + 256], HV.b, r=[kb.bufs["SV"]], w=[HV.b], group=True)
                cur[h] = (AR, BK, HV, YS)
            for ci in range(2):
                ch = bi * 2 + ci
                cs_ = slice(ci * 128, (ci + 1) * 128)
                for gi, h in enumerate(heads):
                    d = slots[gi]
                    AR, BK, HV, YS = cur[h]
                    o, b = P_(d, "p_s1")
                    mm(st, o, BK[:, 0, cs_], AR[:, ci, :], True, True, r=[BK.b, AR.b], w=[b])
                    tt(st, "dve", d["N0RB"][:, :], o, m_a, ALU.mult, r=[b, k.b], w=[d["N0RB"].b])
                    cp(st, "act", d["N"][0][:, :], d["N0RB"][:, 0:128], r=[d["N0RB"].b], w=[d["N"][0].b])
                    o, b = P_(d, "p_s2")
                    mm(st, o, BK[:, 1, cs_], AR[:, ci, :], True, True, r=[BK.b, AR.b], w=[b])
                    tt(st, "dve", d["AKRK"][:, :], o, m_a, ALU.mult, r=[b, k.b], w=[d["AKRK"].b])
                    o, b = P_(d, "p_m")
                    mm(st, o, AR[:, ci, 0:128], BK[:, 0, cs_], True, True, r=[BK.b, AR.b], w=[b])
                    tt(st, "dve", d["M"][0][:, :], o, m_sl, ALU.mult, r=[b, k.b], w=[d["M"][0].b])
                    t_, sl, b = d["p_t"]
                    base = sl.start
                    tr(st, t_[:, base:base + 64], HV[:, 2, cs_], ident64, r=[HV.b, k.b], w=[b])
                    tr(st, t_[:, base + 64:base + 128], HV[:, 0, cs_], ident64, r=[HV.b, k.b], w=[b])
                    tr(st, t_[:, base + 128:base + 192], HV[:, 1, cs_], ident64, r=[HV.b, k.b], w=[b])
                    cp(st, "act", d["VBK"][:, :], t_[:, sl], r=[b], w=[d["VBK"].b])
                if upto < 2:
                    continue
                for kk_ in range(6):
                    for gi, h in enumerate(heads):
                        d = slots[gi]
                        Nk = d["N"][kk_]
                        Mk, Mn = d["M"][kk_ % 2], d["M"][(kk_ + 1) % 2]
                        o, b = P_(d, "p_n")
                        mm(st, o, Mk[:, :], Nk[:, :], True, True, r=[Mk.b, Nk.b], w=[b])
                        cp(st, "act", d["N"][kk_ + 1][:, :], o, r=[b], w=[d["N"][kk_ + 1].b])
                        if kk_ < 5:
                            o2, b2 = P_(d, "p_mm")
                            mm(st, o2, Nk[:, :], Mk[:, :], True, True, r=[Mk.b, Nk.b], w=[b2])
                            cp(st, "dve", Mn[:, :], o2, r=[b2], w=[Mn.b])
                if upto < 3:
                    continue
                for gi, h in enumerate(heads):
                    d = slots[gi]
                    AR, BK, HV, YS = cur[h]
                    H = d["H"][hs[h]["hi"]]
                    o, b = P_(d, "p_w")
                    mm(st, o, AR[:, ci, 0:128], H[:, :], True, False, r=[AR.b, H.b], w=[b])
                    mm(st, o, d["AKRK"][:, 0:128], d["VBK"][:, 0:64], False, True, r=[d["AKRK"].b, d["VBK"].b], w=[b])
                    cp(st, "act", d["U"][0][:, :], o, r=[b], w=[d["U"][0].b])
                for kk_ in range(7):
                    for gi, h in enumerate(heads):
                        d = slots[gi]
                        Nk = d["N0RB"][:, 0:128] if kk_ == 0 else d["N"][kk_][:, :].bitcast(F32)
                        Nkb = d["N0RB"].b if kk_ == 0 else d["N"][kk_].b
                        Uk, Un = d["U"][kk_ % 2], d["U"][(kk_ + 1) % 2]
                        o, b = P_(d, "p_u")
                        mm(st, o, Nk, Uk[:, :], True, True, r=[Nkb, Uk.b], w=[b])
                        tt(st, "dve", Un[:, :], o, Uk[:, :], ALU.add, r=[b, Uk.b], w=[Un.b])
                if upto < 4:
                    continue
                for gi, h in enumerate(heads):
                    d = slots[gi]
                    AR, BK, HV, YS = cur[h]
                    H = d["H"][hs[h]["hi"]]
                    Hn = d["H"][1 - hs[h]["hi"]]
                    Uf = d["U"][1]
                    o, b = P_(d, "p_y", 64)
                    mm(st, o, H[:, :], AR[:, ci, 128:256], True, False, r=[H.b, AR.b], w=[b])
                    mm(st, o, Uf[:, :], d["N0RB"][:, 128:256], False, False, r=[Uf.b, d["N0RB"].b], w=[b])
                    mm(st, o, d["VBK"][:, 0:64], d["AKRK"][:, 128:256], False, True, r=[d["VBK"].b, d["AKRK"].b], w=[b])
                    cp(st, "act", YS[:, cs_], o, r=[b], w=[YS.b])
                    o, b = P_(d, "p_h", 64)
                    mm(st, o, d["VBK"][:, 64:128], Uf[:, :], True, False, r=[d["VBK"].b, Uf.b], w=[b])
                    mm(st, o, d["VBK"][:, 128:192], d["VBK"][:, 0:64], False, True, r=[d["VBK"].b], w=[b])
                    stt(st, "dve", Hn[:, :], H[:, :], d["GL"][:, ch:ch + 1], o, ALU.mult, ALU.add,
                        r=[H.b, d["GL"].b, b], w=[Hn.b])
                    hs[h]["hi"] = 1 - hs[h]["hi"]
            for gi, h in enumerate(heads):
                AR, BK, HV, YS = cur[h]
                st.dma("act", kb.d["YS"][h * 64:h * 64 + 64, t0:t0 + 256], YS[:, :], YS.b, r=[YS.b], w=[kb.bufs["YS"]],
                       disjoint=True)
    st.finish()


def stage_rwkv_post(kb, l):
    S = kb.S
    st = kb.stage(f"D{l}")
    k = load_consts(st, kb)
    cols = load_cols(st, kb, l)
    bones = k[:, K_BONES:K_BONES + 128]
    yr, rr, gr = st.ring("y", 2, [128, 512]), st.ring("rkv", 2, [128, 512]), st.ring("g", 2, [128, 512])
    y2r, mr, vr, ycr = (st.ring(n, 1, [128, 512]) for n in ("y2", "mean", "var", "yc"))
    outr = st.ring("out", 2, [128, 512], BF16)
    psr = st.ring("ps", 4, [128, 512], psum=True)
    for tb in range(S // 512):
        t0 = tb * 512
        for c in range(8):
            rs = slice(c * 128, (c + 1) * 128)
            y, rk, g = yr.next(), rr.next(), gr.next()
            st.dma("sp", y[:, :], kb.d["YS"][rs, t0:t0 + 512], y.b, r=[kb.bufs["YS"]], w=[y.b])
            st.dma("sp", rk[:, :], kb.d["RKV"][rs, t0:t0 + 512], rk.b, r=[kb.bufs["RKV"]], w=[rk.b])
            st.dma("sp", g[:, :], kb.d["G"][rs, t0:t0 + 512], g.b, r=[kb.bufs["G"]], w=[g.b])
            y2, mean, var, yc = y2r.next(), mr.next(), vr.next(), ycr.next()
            tt(st, "pool", y2[:, :], y[:, :], y[:, :], ALU.mult, r=[y.b], w=[y2.b])
            p1, p2 = psr.next(), psr.next()
            mm(st, p1[:, :], bones, y[:, :], True, True, r=[k.b, y.b], w=[p1.b])
            mm(st, p2[:, :], bones, y2[:, :], True, True, r=[k.b, y2.b], w=[p2.b])
            st.op("act", lambda e, o=mean, i=p1: e.mul(out=o[:, :], in_=i[:, :], mul=1.0 / 64), r=[p1.b], w=[mean.b])
            tt(st, "pool", y2[:, :], mean[:, :], mean[:, :], ALU.mult, r=[mean.b], w=[y2.b])
            stt(st, "dve", var[:, :], p2[:, :], 1.0 / 64, y2[:, :], ALU.mult, ALU.subtract, r=[p2.b, y2.b], w=[var.b])
            ts(st, "dve", var[:, :], var[:, :], 64e-5, ALU.add, r=[var.b], w=[var.b])
            act(st, var[:, :], var[:, :], AF.Sqrt, r=[var.b], w=[var.b])
            st.op("dve", lambda e, o=var: e.reciprocal(out=o[:, :], in_=o[:, :]), r=[var.b], w=[var.b])
            tt(st, "pool", yc[:, :], y[:, :], mean[:, :], ALU.subtract, r=[y.b, mean.b], w=[yc.b])
            tt(st, "dve", yc[:, :], yc[:, :], var[:, :], ALU.mult, r=[yc.b, var.b], w=[yc.b])
            ts(st, "dve", yc[:, :], yc[:, :], colap(cols, "gn_g", c), ALU.mult, r=[yc.b, cols.b], w=[yc.b],
               s2=colap(cols, "gn_b", c), op1=ALU.add)
            tt(st, "pool", yc[:, :], yc[:, :], rk[:, :], ALU.add, r=[yc.b, rk.b], w=[yc.b])
            o = outr.next()
            tt(st, "dve", o[:, :], yc[:, :], g[:, :], ALU.mult, r=[yc.b, g.b], w=[o.b])
            st.dma("act", kb.d["YA"][rs, t0:t0 + 512], o[:, :], o.b, r=[o.b], w=[kb.bufs["YA"]], disjoint=True)
    st.finish()


def declare(kb, moe=True):
    S, L = kb.S, kb.L
    kb.din("xT", [D, S])
    kb.din("konst", [128, NCONST])
    kb.din("w_in", [L, D, NIN])
    kb.din("w_in_vres", [max(L - 1, 1), D, 32])
    kb.din("rwkv_w2", [L, 64, C])
    kb.din("rwkv_a2", [L, 64, C])
    kb.din("rwkv_v2", [max(L - 1, 1), 32, C])
    kb.din("rwkv_g2", [L, 160, C])
    kb.din("w_branch", [L, 3, C, D])
    kb.din("w_out", [L, D, D])
    kb.din("router_w", [L, D, NE])
    if moe:
        kb.din("exp_w_gu", [L, NE, D, 2 * DFF])
        kb.din("exp_w_down", [L, NE, DFF, D])
    kb.din("exp_b_down", [L, NE, D])
    for l in range(L):
        kb.din(f"cols{l}", [128, NCOLS])
        kb.din(f"lrubd{l}", [8, 128, 256])
        kb.din(f"bgu{l}", [128, NE * 16])
        kb.din(f"ln2bc{l}", [128, 2 * D])
        kb.din(f"rbbc{l}", [128, NE])
    kb.dscr("P", [NIN + 32, S])
    for nm in ("G", "VF", "SV", "RKV", "SR", "SA", "SB", "SK", "SBH", "SKH", "YS", "CC"):
        kb.dscr(nm, [C, S])
    kb.dscr("GL", [C, S // 128])
    for nm in ("YA", "YB", "YC"):
        kb.dscr(nm, [C, S], BF16)
    kb.dscr("X1T", [D, S])
    kb.dscr("X2T", [D, S])
    kb.dscr("GT", [S, NE])
    kb.dout("out", [S, D])


def make_inputs(inp, L, moe=True):
    m = {"konst": host_consts()}
    for nm in ("w_in", "rwkv_w2", "rwkv_a2", "rwkv_g2", "w_branch", "w_out", "router_w", "exp_b_down") + (
            ("exp_w_gu", "exp_w_down") if moe else ()):
        m[nm] = np.ascontiguousarray(inp[nm][:L], dtype=np.float32)
    for nm in ("w_in_vres", "rwkv_v2"):
        m[nm] = np.ascontiguousarray(inp[nm][:max(L - 1, 1)], dtype=np.float32)
    for l in range(L):
        for k_, v in host_layout(inp, l).items():
            m[f"{k_}{l}"] = np.ascontiguousarray(v, dtype=np.float32)
    return m


def stage_lru(kb, l):
    S = kb.S
    st = kb.stage(f"E{l}")
    cols = load_cols(st, kb, l)
    TB = min(1024, S)
    P, Pb = kb.d["P"], kb.bufs["P"]
    bd = st.sb("bd", [128, 8, 256])
    st.dma("sp", bd[:, :, :], kb.d[f"lrubd{l}"].rearrange("c p n -> p c n"), bd.b, r=[kb.bufs[f"lrubd{l}"]], w=[bd.b])
    csp = st.sb("csp", [128, 8])
    o, n = COLT["l_lam"]
    act(st, csp[:, :], cols[:, o:o + 8], AF.Exp, r=[cols.b], w=[csp.b], scale=-1.0)
    act(st, csp[:, :], csp[:, :], AF.Ln, r=[csp.b], w=[csp.b], bias=1.0)
    ts(st, "dve", csp[:, :], csp[:, :], -8.0, ALU.mult, r=[csp.b], w=[csp.b])
    lxr = st.ring("lx", 2, [128, TB + 3])
    lgr = st.ring("lg", 2, [128, TB])
    names = ["xc", "rg", "ig", "a", "t1", "b", "h", "ge"]
    tl = {n_: st.ring(n_, 1, [128, TB]) for n_ in names}
    outr = st.ring("out", 2, [128, TB], BF16)
    hc = st.sb("hcarry", [128, 8])
    st.op("pool", lambda e: e.memset(hc[:, :], 0.0), r=[], w=[hc.b])
    psr = st.ring("ps", 4, [128, 512], psum=True)
    for tb in range(S // TB):
        t0 = tb * TB
        for c in range(8):
            rs = slice(c * 128, (c + 1) * 128)
            lx, lg = lxr.next(), lgr.next()
            load_halo(st, lx, P[O_LX + c * 128:O_LX + (c + 1) * 128, :], 128, t0, TB, 3, Pb)
            st.dma("sp", lg[:, :], P[O_LG + c * 128:O_LG + (c + 1) * 128, t0:t0 + TB], lg.b, r=[Pb], w=[lg.b])
            xc, rg, ig, a, t1, b, h, ge = (tl[n_].next() for n_ in names)
            wo = COLT["lconv_w"][0] + c * 4
            ts(st, "dve", xc[:, :], lx[:, 3:3 + TB], cols[:, wo + 3:wo + 4], ALU.mult, r=[lx.b, cols.b], w=[xc.b],
               s2=colap(cols, "lconv_b", c), op1=ALU.add)
            for j in range(1, 4):
                stt(st, "dve", xc[:, :], lx[:, 3 - j:3 - j + TB], cols[:, wo + 3 - j:wo + 4 - j], xc[:, :], ALU.mult,
                    ALU.add, r=[lx.b, cols.b, xc.b], w=[xc.b])
            for hb in range(TB // 512):
                hs_ = slice(hb * 512, (hb + 1) * 512)
                p1, p2 = psr.next(), psr.next()
                mm(st, p1[:, :], bd[:, c, 0:128], xc[:, hs_], True, True, r=[bd.b, xc.b], w=[p1.b])
                mm(st, p2[:, :], bd[:, c, 128:256], xc[:, hs_], True, True, r=[bd.b, xc.b], w=[p2.b])
                act(st, rg[:, hs_], p1[:, :], AF.Sigmoid, r=[p1.b, cols.b], w=[rg.b], bias=colap(cols, "l_ba", c))
                act(st, ig[:, hs_], p2[:, :], AF.Sigmoid, r=[p2.b, cols.b], w=[ig.b], bias=colap(cols, "l_bx", c))
            act(st, a[:, :], rg[:, :], AF.Exp, r=[rg.b, csp.b], w=[a.b], scale=csp[:, c:c + 1])
            tt(st, "pool", t1[:, :], a[:, :], a[:, :], ALU.mult, r=[a.b], w=[t1.b])
            ts(st, "dve", t1[:, :], t1[:, :], -1.0, ALU.mult, r=[t1.b], w=[t1.b], s2=1.0, op1=ALU.add)
            ts(st, "dve", t1[:, :], t1[:, :], 0.0, ALU.max, r=[t1.b], w=[t1.b])
            act(st, t1[:, :], t1[:, :], AF.Sqrt, r=[t1.b], w=[t1.b])
            tt(st, "pool", b[:, :], ig[:, :], xc[:, :], ALU.mult, r=[ig.b, xc.b], w=[b.b])
            tt(st, "pool", b[:, :], b[:, :], t1[:, :], ALU.mult, r=[b.b, t1.b], w=[b.b])
            st.op("dve", lambda e, h=h, a=a, b=b, c=c: e.tensor_tensor_scan(
                out=h[:, :], data0=a[:, :], data1=b[:, :], initial=hc[:, c:c + 1], op0=ALU.mult, op1=ALU.add),
                r=[a.b, b.b, hc.b], w=[h.b])
            cp(st, "dve", hc[:, c:c + 1], h[:, TB - 1:TB], r=[h.b], w=[hc.b])
            tt(st, "pool", ge[:, :], lg[:, :], lg[:, :], ALU.mult, r=[lg.b], w=[ge.b])
            ts(st, "dve", ge[:, :], ge[:, :], 0.044715, ALU.mult, r=[ge.b], w=[ge.b], s2=1.0, op1=ALU.add)
            tt(st, "pool", ge[:, :], ge[:, :], lg[:, :], ALU.mult, r=[ge.b, lg.b], w=[ge.b])
            act(st, ge[:, :], ge[:, :], AF.Sigmoid, r=[ge.b], w=[ge.b], scale=1.5957691216057308)
            tt(st, "pool", ge[:, :], ge[:, :], lg[:, :], ALU.mult, r=[ge.b, lg.b], w=[ge.b])
            o_ = outr.next()
            tt(st, "dve", o_[:, :], ge[:, :], h[:, :], ALU.mult, r=[ge.b, h.b], w=[o_.b])
            st.dma("act", kb.d["YB"][rs, t0:t0 + TB], o_[:, :], o_.b, r=[o_.b], w=[kb.bufs["YB"]], disjoint=True)
    st.finish()


def stage_conf(kb, l):
    S = kb.S
    st = kb.stage(f"F{l}")
    k = load_consts(st, kb)
    cols = load_cols(st, kb, l)
    ones = k[:, K_ONES:K_ONES + 128]
    P, Pb = kb.d["P"], kb.bufs["P"]
    TB = min(1024, S)
    HL = 30
    vr = st.ring("val", 2, [128, TB + HL])
    gr = st.ring("gate", 2, [128, TB + HL])
    accr = st.ring("acc", 2, [128, TB])
    acc2r = st.ring("acc2", 2, [128, TB])
    tmpr = st.ring("tmp", 3, [128, TB])
    for tb in range(S // TB):
        t0 = tb * TB
        for c in range(8):
            rs = slice(c * 128, (c + 1) * 128)
            v, g = vr.next(), gr.next()
            load_halo(st, v, P[O_CU + c * 128:O_CU + (c + 1) * 128, :], 128, t0, TB, HL, Pb)
            load_halo(st, g, P[O_CU + C + c * 128:O_CU + C + (c + 1) * 128, :], 128, t0, TB, HL, Pb)
            act(st, g[:, :], g[:, :], AF.Sigmoid, r=[g.b], w=[g.b])
            tt(st, "pool", v[:, :], v[:, :], g[:, :], ALU.mult, r=[v.b, g.b], w=[v.b])
            acc, acc2 = accr.next(), acc2r.next()
            wo = COLT["cconv_w"][0] + c * 31
            ts(st, "dve", acc[:, :], v[:, HL:HL + TB], cols[:, wo + 30:wo + 31], ALU.mult, r=[v.b, cols.b], w=[acc.b],
               s2=colap(cols, "cconv_b", c), op1=ALU.add)
            for kk_ in range(29, 14, -1):
                sh = 30 - kk_
                stt(st, "dve", acc[:, :], v[:, HL - sh:HL - sh + TB], cols[:, wo + kk_:wo + kk_ + 1], acc[:, :],
                    ALU.mult, ALU.add, r=[v.b, cols.b, acc.b], w=[acc.b])
            act(st, acc2[:, :], v[:, 0:TB], AF.Copy, r=[v.b, cols.b], w=[acc2.b], scale=cols[:, wo:wo + 1])
            for kk_ in range(1, 15):
                sh = 30 - kk_
                tmp = tmpr.next()
                act(st, tmp[:, :], v[:, HL - sh:HL - sh + TB], AF.Copy, r=[v.b, cols.b], w=[tmp.b],
                    scale=cols[:, wo + kk_:wo + kk_ + 1])
                tt(st, "pool", acc2[:, :], acc2[:, :], tmp[:, :], ALU.add, r=[acc2.b, tmp.b], w=[acc2.b])
            tt(st, "dve", acc[:, :], acc[:, :], acc2[:, :], ALU.add, r=[acc.b, acc2.b], w=[acc.b])
            st.dma("act", kb.d["CC"][rs, t0:t0 + TB], acc[:, :], acc.b, r=[acc.b], w=[kb.bufs["CC"]], disjoint=True)
    st.finish()
    st = kb.stage(f"F2{l}")
    k = load_consts(st, kb)
    cols = load_cols(st, kb, l)
    ones = k[:, K_ONES:K_ONES + 128]
    ccr = st.ring("cc", 2, [128, 8, 512])
    sqr = st.ring("sq", 2, [128, 512])
    mean, var, tmp = st.sb("mean", [128, 512]), st.sb("var", [128, 512]), st.sb("tmp", [128, 512])
    ycr = st.ring("yc", 2, [128, 512])
    sgr = st.ring("sg", 2, [128, 512])
    outr = st.ring("out", 2, [128, 8, 512], BF16)
    ps1, ps2 = st.ring("ps1", 2, [128, 512], psum=True), st.ring("ps2", 2, [128, 512], psum=True)
    for tb in range(S // 512):
        t0 = tb * 512
        cc = ccr.next()
        st.dma("sp", cc[:, :, :], kb.d["CC"][:, t0:t0 + 512].rearrange("(c p) t -> p c t", p=128), cc.b,
               r=[kb.bufs["CC"]], w=[cc.b])
        p1, p2 = ps1.next(), ps2.next()
        for c in range(8):
            sq = sqr.next()
            tt(st, "pool", sq[:, :], cc[:, c, :], cc[:, c, :], ALU.mult, r=[cc.b], w=[sq.b])
            mm(st, p1[:, :], ones, cc[:, c, :], c == 0, c == 7, r=[k.b, cc.b], w=[p1.b])
            mm(st, p2[:, :], ones, sq[:, :], c == 0, c == 7, r=[k.b, sq.b], w=[p2.b])
        st.op("act", lambda e, i=p1: e.mul(out=mean[:, :], in_=i[:, :], mul=1.0 / C), r=[p1.b], w=[mean.b])
        tt(st, "pool", tmp[:, :], mean[:, :], mean[:, :], ALU.mult, r=[mean.b], w=[tmp.b])
        stt(st, "dve", var[:, :], p2[:, :], 1.0 / C, tmp[:, :], ALU.mult, ALU.subtract, r=[p2.b, tmp.b], w=[var.b])
        ts(st, "dve", var[:, :], var[:, :], 1e-5, ALU.add, r=[var.b], w=[var.b])
        act(st, var[:, :], var[:, :], AF.Sqrt, r=[var.b], w=[var.b])
        st.op("dve", lambda e: e.reciprocal(out=var[:, :], in_=var[:, :]), r=[var.b], w=[var.b])
        o_ = outr.next()
        for c in range(8):
            yc, sg = ycr.next(), sgr.next()
            tt(st, "pool", yc[:, :], cc[:, c, :], mean[:, :], ALU.subtract, r=[cc.b, mean.b], w=[yc.b])
            tt(st, "dve", yc[:, :], yc[:, :], var[:, :], ALU.mult, r=[yc.b, var.b], w=[yc.b])
            ts(st, "dve", yc[:, :], yc[:, :], colap(cols, "cln_g", c), ALU.mult, r=[yc.b, cols.b], w=[yc.b],
               s2=colap(cols, "cln_b", c), op1=ALU.add)
            act(st, sg[:, :], yc[:, :], AF.Sigmoid, r=[yc.b], w=[sg.b])
            tt(st, "pool", o_[:, c, :], yc[:, :], sg[:, :], ALU.mult, r=[yc.b, sg.b], w=[o_.b])
        st.dma("act", kb.d["YC"][:, t0:t0 + 512].rearrange("(c p) t -> p c t", p=128), o_[:, :, :], o_.b, r=[o_.b],
               w=[kb.bufs["YC"]], disjoint=True)
    st.finish()


def stage_merge(kb, l, xsrc):
    S = kb.S
    st = kb.stage(f"G{l}")
    k = load_consts(st, kb)
    cols = load_cols(st, kb, l)
    ones = k[:, K_ONES:K_ONES + 128]
    P, Pb = kb.d["P"], kb.bufs["P"]
    ybf = [st.ring(f"y{b}", 1, [128, 8, 512], BF16) for b in range(3)]
    wstB = st.ring("wstB", 2, [128, 8, 256])
    wbfB = st.ring("wbfB", 4, [128, 8, 256], BF16)
    wstO = st.ring("wstO", 2, [128, 16, 128])
    wbfO = st.ring("wbfO", 2, [128, 16, 128], BF16)
    mixed = st.sb("mixed", [128, 16, 512], BF16, nsub=16)
    res = st.sb("res", [128, 16, 512], F32, nsub=16)
    gts = st.ring("gt", 3, [128, 512])
    mts = st.ring("mt", 3, [128, 512])
    xts = st.ring("xt", 2, [128, 512])
    sqr = st.ring("sq", 2, [128, 512])
    mean, var, tmp = st.sb("mean", [128, 512]), st.sb("var", [128, 512]), st.sb("tmp", [128, 512])
    outr = st.ring("out", 3, [128, 512])
    psr = st.ring("ps", 6, [128, 512], psum=True)
    ps1, ps2 = st.ps("pstatA", [128, 512]), st.ps("pstatB", [128, 512])
    ynames = ("YA", "YB", "YC")
    xs, xsb = kb.d[xsrc], kb.bufs[xsrc]
    for tb in range(S // 512):
        t0 = tb * 512
        ys = []
        for b in range(3):
            y = ybf[b].next()
            st.dma("sp", y[:, :, :], kb.d[ynames[b]][:, t0:t0 + 512].rearrange("(c p) t -> p c t", p=128), y.b,
                   r=[kb.bufs[ynames[b]]], w=[y.b])
            ys.append(y)
        for g in range(8):
            wbs = []
            for b in range(3):
                ws = wstB.next()
                st.dma("sp", ws[:, :, :], kb.d["w_branch"][l, b][:, g * 256:(g + 1) * 256].rearrange(
                    "(kc p) c -> p kc c", p=128), ws.b, r=[kb.bufs["w_branch"]], w=[ws.b])
                wb = wbfB.next()
                cp(st, "act", wb[:, :, :], ws[:, :, :], r=[ws.b], w=[wb.b])
                wbs.append(wb)
            for dc in range(2):
                dch = g * 2 + dc
                ms = []
                for b in range(3):
                    gt = gts.next()
                    r0 = O_MG + b * D + dch * 128
                    st.dma("sp", gt[:, :], P[r0:r0 + 128, t0:t0 + 512], gt.b, r=[Pb], w=[gt.b])
                    act(st, gt[:, :], gt[:, :], AF.Sigmoid, r=[gt.b], w=[gt.b])
                    ps = psr.next()
                    for kc in range(8):
                        mm(st, ps[:, :], wbs[b][:, kc, dc * 128:(dc + 1) * 128], ys[b][:, kc, :], kc == 0, kc == 7,
                           r=[wbs[b].b, ys[b].b], w=[ps.b])
                    mt = mts.next()
                    tt(st, "dve", mt[:, :], ps[:, :], gt[:, :], ALU.mult, r=[ps.b, gt.b], w=[mt.b])
                    ms.append(mt)
                tt(st, "pool", ms[0][:, :], ms[0][:, :], ms[1][:, :], ALU.add, r=[ms[0].b, ms[1].b], w=[ms[0].b])
                tt(st, "pool", mixed[:, dch, :], ms[0][:, :], ms[2][:, :], ALU.add, r=[ms[0].b, ms[2].b],
                   w=[mixed.sub[dch]])
        for dch in range(16):
            ws = wstO.next()
            st.dma("sp", ws[:, :, :], kb.d["w_out"][l][:, dch * 128:(dch + 1) * 128].rearrange("(kc p) c -> p kc c", p=128),
                   ws.b, r=[kb.bufs["w_out"]], w=[ws.b])
            wb = wbfO.next()
            cp(st, "act", wb[:, :, :], ws[:, :, :], r=[ws.b], w=[wb.b])
            xt = xts.next()
            st.dma("sp", xt[:, :], xs[dch * 128:(dch + 1) * 128, t0:t0 + 512], xt.b, r=[xsb], w=[xt.b])
            ps = psr.next()
            for kc in range(16):
                mm(st, ps[:, :], wb[:, kc, :], mixed[:, kc, :], kc == 0, kc == 15, r=[wb.b, mixed.sub[kc]], w=[ps.b])
            stt(st, "dve", res[:, dch, :], xt[:, :], ALPHA, ps[:, :], ALU.mult, ALU.add, r=[xt.b, ps.b],
                w=[res.sub[dch]])
        for dch in range(16):
            sq = sqr.next()
            tt(st, "pool", sq[:, :], res[:, dch, :], res[:, dch, :], ALU.mult, r=[res.sub[dch]], w=[sq.b])
            mm(st, ps1[:, :], ones, res[:, dch, :], dch == 0, dch == 15, r=[k.b, res.sub[dch]], w=[ps1.b])
            mm(st, ps2[:, :], ones, sq[:, :], dch == 0, dch == 15, r=[k.b, sq.b], w=[ps2.b])
        st.op("act", lambda e: e.mul(out=mean[:, :], in_=ps1[:, :], mul=1.0 / D), r=[ps1.b], w=[mean.b])
        tt(st, "pool", tmp[:, :], mean[:, :], mean[:, :], ALU.mult, r=[mean.b], w=[tmp.b])
        stt(st, "dve", var[:, :], ps2[:, :], 1.0 / D, tmp[:, :], ALU.mult, ALU.subtract, r=[ps2.b, tmp.b], w=[var.b])
        ts(st, "dve", var[:, :], var[:, :], 1e-5, ALU.add, r=[var.b], w=[var.b])
        act(st, var[:, :], var[:, :], AF.Sqrt, r=[var.b], w=[var.b])
        st.op("dve", lambda e: e.reciprocal(out=var[:, :], in_=var[:, :]), r=[var.b], w=[var.b])
        for dch in range(16):
            o_ = outr.next()
            tt(st, "pool", o_[:, :], res[:, dch, :], mean[:, :], ALU.subtract, r=[res.sub[dch], mean.b], w=[o_.b])
            tt(st, "dve", o_[:, :], o_[:, :], var[:, :], ALU.mult, r=[o_.b, var.b], w=[o_.b])
            ts(st, "dve", o_[:, :], o_[:, :], colap(cols, "ln1_g", dch), ALU.mult, r=[o_.b, cols.b], w=[o_.b],
               s2=colap(cols, "ln1_b", dch), op1=ALU.add)
            st.dma("act", kb.d["X1T"][dch * 128:(dch + 1) * 128, t0:t0 + 512], o_[:, :], o_.b, r=[o_.b],
                   w=[kb.bufs["X1T"]], disjoint=True)
    st.finish()


def stage_router(kb, l):
    S = kb.S
    st = kb.stage(f"H1{l}")
    rw = st.sb("rw", [128, 16, NE])
    st.dma("sp", rw[:, :, :], kb.d["router_w"][l].rearrange("(kc p) e -> p kc e", p=128), rw.b,
           r=[kb.bufs["router_w"]], w=[rw.b])
    rb = st.sb("rb", [128, NE])
    st.dma("sp", rb[:, :], kb.d[f"rbbc{l}"][:, :], rb.b, r=[kb.bufs[f"rbbc{l}"]], w=[rb.b])
    xr = st.ring("x", 3, [128, 16, 128])
    lgr, er, mkr, gr_ = (st.ring(n_, 2, [128, NE]) for n_ in ("lg", "e", "mk", "g"))
    m8r = st.ring("m8", 2, [128, 8])
    smr = st.ring("sm", 2, [128, 2])
    psr = st.ring("ps", 4, [128, 512], psum=True)
    for ti in range(S // 128):
        t0 = ti * 128
        x = xr.next()
        st.dma("sp", x[:, :, :], kb.d["X1T"][:, t0:t0 + 128].rearrange("(c p) t -> p c t", p=128), x.b,
               r=[kb.bufs["X1T"]], w=[x.b])
        ps = psr.next()
        for kc in range(16):
            mm(st, ps[:, 0:NE], x[:, kc, :], rw[:, kc, :], kc == 0, kc == 15, r=[x.b, rw.b], w=[ps.b])
        lg, e_, mk, g, m8, sm = lgr.next(), er.next(), mkr.next(), gr_.next(), m8r.next(), smr.next()
        tt(st, "dve", lg[:, :], ps[:, 0:NE], rb[:, :], ALU.add, r=[ps.b, rb.b], w=[lg.b])
        st.op("dve", lambda e, m8=m8, lg=lg: e.max(out=m8[:, :], in_=lg[:, :]), r=[lg.b], w=[m8.b])
        ts(st, "dve", sm[:, 0:1], m8[:, 0:1], -1.0, ALU.mult, r=[m8.b], w=[sm.b])
        act(st, e_[:, :], lg[:, :], AF.Exp, r=[lg.b, sm.b], w=[e_.b], bias=sm[:, 0:1])
        ts(st, "dve", mk[:, :], lg[:, :], m8[:, 3:4], ALU.is_ge, r=[lg.b, m8.b], w=[mk.b])
        tt(st, "dve", e_[:, :], e_[:, :], mk[:, :], ALU.mult, r=[e_.b, mk.b], w=[e_.b])
        st.op("dve", lambda e, sm=sm, e_=e_: e.reduce_sum(out=sm[:, 1:2], in_=e_[:, :], axis=AX.X), r=[e_.b], w=[sm.b])
        st.op("dve", lambda e, sm=sm: e.reciprocal(out=sm[:, 1:2], in_=sm[:, 1:2]), r=[sm.b], w=[sm.b])
        ts(st, "dve", g[:, :], e_[:, :], sm[:, 1:2], ALU.mult, r=[e_.b, sm.b], w=[g.b])
        st.dma("act", kb.d["GT"][t0:t0 + 128, :], g[:, :], g.b, r=[g.b], w=[kb.bufs["GT"]], disjoint=True)
    st.finish()


def stage_moe(kb, l, last):
    S = kb.S
    st = kb.stage(f"H2{l}")
    k = load_consts(st, kb)
    ident = k[:, K_ID:K_ID + 128]
    TS = min(1024, S)
    NTL = TS // 128
    NTB = TS // 512
    x1b = st.sb("x1b", [128, 16, TS], BF16, nsub=16)
    acc = st.sb("acc", [128, NTL, D], F32, nsub=NTL)
    hbf = st.sb("h", [128, 8, TS], BF16, nsub=8)
    wst = st.ring("wst", 4, [128, 2048])
    wbf = st.ring("wbf", 2, [128, 8192], BF16)
    gates = st.sb("gates", [128, NTL, NE])
    gT = st.sb("gT", [32, TS])
    bgu = st.sb("bgu", [128, NE * 16])
    st.dma("sp", bgu[:, :], kb.d[f"bgu{l}"][:, :], bgu.b, r=[kb.bufs[f"bgu{l}"]], w=[bgu.b])
    gpr, sgr, upr = (st.ring(n_, 1, [128, 512]) for n_ in ("gp", "sg", "up"))
    stat = st.sb("stat", [128, 4, 6])
    mv = st.sb("mv", [128, 2])
    stg = None if last else st.ring("stg", 1, [128, 16, 128])
    psg = st.ring("psg", 2, [128, 512], psum=True)
    psu = st.ring("psu", 2, [128, 512], psum=True)
    psd = st.ring("psd", 2, [128, 512], psum=True)
    pst = st.ring("pst", 2, [128, 512], psum=True)
    Wgu, Wgub = kb.d["exp_w_gu"], kb.bufs["exp_w_gu"]
    Wd, Wdb = kb.d["exp_w_down"], kb.bufs["exp_w_down"]
    for sbi in range(S // TS):
        t0 = sbi * TS
        for kc4 in range(4):
            xts = []
            for q in range(4):
                kc = kc4 * 4 + q
                xt = wst.next()
                st.dma("sp", xt[:, 0:TS], kb.d["X1T"][kc * 128:(kc + 1) * 128, t0:t0 + TS], xt.b, r=[kb.bufs["X1T"]],
                       w=[xt.b])
                cp(st, "act" if kc % 2 else "dve", x1b[:, kc, :], xt[:, 0:TS], r=[xt.b], w=[x1b.sub[kc]])
                for tl_ in range(NTL):
                    pt = pst.next()
                    tr(st, pt[:, 0:128], xt[:, tl_ * 128:(tl_ + 1) * 128], ident, r=[xt.b, k.b], w=[pt.b])
                    st.op("act", lambda e, pt=pt, tl_=tl_, kc=kc: e.mul(out=acc[:, tl_, kc * 128:(kc + 1) * 128],
                                                                       in_=pt[:, 0:128], mul=ALPHA),
                          r=[pt.b], w=[acc.sub[tl_]])
        st.dma("sp", gates[:, :, :], kb.d["GT"][t0:t0 + TS, :].rearrange("(a p) e -> p a e", p=128), gates.b,
               r=[kb.bufs["GT"]], w=[gates.b])
        for tl_ in range(NTL):
            pt = pst.next()
            tr(st, pt[0:32, 0:128], gates[:, tl_, :], ident, r=[gates.b, k.b], w=[pt.b])
            cp(st, "act", gT[:, tl_ * 128:(tl_ + 1) * 128], pt[0:32, 0:128], r=[pt.b], w=[gT.b])
        bd_ = wst.next()
        st.dma("sp", bd_[0:32, :], kb.d["exp_b_down"][l], bd_.b, r=[kb.bufs["exp_b_down"]], w=[bd_.b])
        for tl_ in range(NTL):
            for blk in range(4):
                ps = psd.next()
                mm(st, ps[:, :], gT[:, tl_ * 128:(tl_ + 1) * 128], bd_[0:32, blk * 512:(blk + 1) * 512], True, True,
                   r=[gT.b, bd_.b], w=[ps.b])
                tt(st, "dve", acc[:, tl_, blk * 512:(blk + 1) * 512], ps[:, :], acc[:, tl_, blk * 512:(blk + 1) * 512],
                   ALU.add, r=[ps.b, acc.sub[tl_]], w=[acc.sub[tl_]])
        for e_i in range(NE):
            bcol = e_i * 16
            for g4 in range(4):
                wb = wbf.next()
                wbv = wb[:, :].rearrange("p (a b) -> p a b", b=512)
                for q in range(4):
                    ws = wst.next()
                    wsv = ws[:, :].rearrange("p (a b) -> p a b", b=512)
                    rows = slice(q * 512, (q + 1) * 512)
                    st.dma("sp", wsv[:, :, 0:256],
                           Wgu[l, e_i][rows, g4 * 256:(g4 + 1) * 256].rearrange("(kc p) c -> p kc c", p=128), ws.b,
                           r=[Wgub], w=[ws.b])
                    st.dma("sp", wsv[:, :, 256:512],
                           Wgu[l, e_i][rows, DFF + g4 * 256:DFF + (g4 + 1) * 256].rearrange("(kc p) c -> p kc c", p=128),
                           ws.b, r=[Wgub], w=[ws.b], group=True)
                    cp(st, "act", wb[:, q * 2048:(q + 1) * 2048], ws[:, :], r=[ws.b], w=[wb.b])
                for jj in range(2):
                    j = g4 * 2 + jj
                    for tb in range(NTB):
                        tsl = slice(tb * 512, (tb + 1) * 512)
                        pg, pu = psg.next(), psu.next()
                        for kc in range(16):
                            mm(st, pg[:, :], wbv[:, kc, jj * 128:(jj + 1) * 128], x1b[:, kc, tsl], kc == 0, kc == 15,
                               r=[wb.b, x1b.sub[kc]], w=[pg.b])
                        for kc in range(16):
                            mm(st, pu[:, :], wbv[:, kc, 256 + jj * 128:256 + (jj + 1) * 128], x1b[:, kc, tsl], kc == 0,
                               kc == 15, r=[wb.b, x1b.sub[kc]], w=[pu.b])
                        gp, sg, up = gpr.next(), sgr.next(), upr.next()
                        ts(st, "dve", gp[:, :], pg[:, :], bgu[:, bcol + j:bcol + j + 1], ALU.add, r=[pg.b, bgu.b],
                           w=[gp.b], s2=7.0, op1=ALU.min)
                        act(st, sg[:, :], gp[:, :], AF.Sigmoid, r=[gp.b], w=[sg.b], scale=1.702)
                        ts(st, "dve", up[:, :], pu[:, :], bgu[:, bcol + 8 + j:bcol + 9 + j], ALU.add, r=[pu.b, bgu.b],
                           w=[up.b], s2=7.0, op1=ALU.min)
                        ts(st, "dve", up[:, :], up[:, :], -7.0, ALU.max, r=[up.b], w=[up.b], s2=1.0, op1=ALU.add)
                        tt(st, "pool", gp[:, :], gp[:, :], sg[:, :], ALU.mult, r=[gp.b, sg.b], w=[gp.b])
                        tt(st, "pool", hbf[:, j, tsl], up[:, :], gp[:, :], ALU.mult, r=[up.b, gp.b], w=[hbf.sub[j]])
            for dg in range(2):
                wb = wbf.next()
                wbv = wb[:, :].rearrange("p (a b) -> p a b", b=1024)
                for q in range(4):
                    ws = wst.next()
                    st.dma("sp", ws[:, :].rearrange("p (a b) -> p a b", b=1024),
                           Wd[l, e_i][q * 256:(q + 1) * 256, dg * 1024:(dg + 1) * 1024].rearrange(
                               "(kc p) c -> p kc c", p=128), ws.b, r=[Wdb], w=[ws.b])
                    cp(st, "act", wb[:, q * 2048:(q + 1) * 2048], ws[:, :], r=[ws.b], w=[wb.b])
                for tl_ in range(NTL):
                    for hh in range(2):
                        c0 = dg * 1024 + hh * 512
                        ps = psd.next()
                        for kc in range(8):
                            mm(st, ps[:, :], hbf[:, kc, tl_ * 128:(tl_ + 1) * 128], wbv[:, kc, hh * 512:(hh + 1) * 512],
                               kc == 0, kc == 7, r=[hbf.sub[kc], wb.b], w=[ps.b])
                        stt(st, "dve", acc[:, tl_, c0:c0 + 512], ps[:, :], gates[:, tl_, e_i:e_i + 1],
                            acc[:, tl_, c0:c0 + 512], ALU.mult, ALU.add, r=[ps.b, gates.b, acc.sub[tl_]],
                            w=[acc.sub[tl_]])
        lg_ = wst.next()
        lb_ = wst.next()
        st.dma("sp", lg_[:, :], kb.d[f"ln2bc{l}"][:, 0:D], lg_.b, r=[kb.bufs[f"ln2bc{l}"]], w=[lg_.b])
        st.dma("sp", lb_[:, :], kb.d[f"ln2bc{l}"][:, D:2 * D], lb_.b, r=[kb.bufs[f"ln2bc{l}"]], w=[lb_.b])
        for tl_ in range(NTL):
            a_ = acc[:, tl_, :]
            ab = acc.sub[tl_]
            for i in range(4):
                st.op("dve", lambda e, i=i, tl_=tl_: e.bn_stats(out=stat[:, i, :], in_=acc[:, tl_, i * 512:(i + 1) * 512]),
                      r=[ab], w=[stat.b])
            st.op("dve", lambda e: e.bn_aggr(out=mv[:, :], in_=stat[:, :, :].rearrange("p a b -> p (a b)")),
                  r=[stat.b], w=[mv.b])
            ts(st, "dve", mv[:, 1:2], mv[:, 1:2], 1e-5, ALU.add, r=[mv.b], w=[mv.b])
            act(st, mv[:, 1:2], mv[:, 1:2], AF.Sqrt, r=[mv.b], w=[mv.b])
            st.op("dve", lambda e: e.reciprocal(out=mv[:, 1:2], in_=mv[:, 1:2]), r=[mv.b], w=[mv.b])
            ts(st, "dve", a_, a_, mv[:, 0:1], ALU.subtract, r=[ab, mv.b], w=[ab], s2=mv[:, 1:2], op1=ALU.mult)
            tt(st, "pool", a_, a_, lg_[:, :], ALU.mult, r=[ab, lg_.b], w=[ab])
            tt(st, "pool", a_, a_, lb_[:, :], ALU.add, r=[ab, lb_.b], w=[ab])
            if last:
                st.dma("act", kb.d["out"][t0 + tl_ * 128:t0 + (tl_ + 1) * 128, :], a_, ab, r=[ab],
                       w=[kb.bufs["out"]], disjoint=True)
            else:
                sg_ = stg.next()
                for kc4 in range(4):
                    pt = pst.next()
                    for q in range(4):
                        kc = kc4 * 4 + q
                        tr(st, pt[:, q * 128:(q + 1) * 128], acc[:, tl_, kc * 128:(kc + 1) * 128], ident, r=[ab, k.b],
                           w=[pt.b])
                    cp(st, "act", sg_[:, kc4 * 4:(kc4 + 1) * 4, :], pt[:, :].rearrange("p (a b) -> p a b", b=128),
                       r=[pt.b], w=[sg_.b])
                tsl = slice(t0 + tl_ * 128, t0 + (tl_ + 1) * 128)
                st.dma("act", kb.d["X2T"][:, tsl].rearrange("(c p) t -> p c t", p=128), sg_[:, :, :], sg_.b, r=[sg_.b],
                       w=[kb.bufs["X2T"]], disjoint=True)
    st.finish()


def build_program(S, L, debug=False, moe=True):
    kb = KB(S, L, debug=debug)
    declare(kb, moe=moe)
    src = "xT"
    for l in range(L):
        stage_inproj(kb, l, src)
        stage_rwkv_prep(kb, l)
        stage_rwkv_scan(kb, l)
        stage_rwkv_post(kb, l)
        stage_lru(kb, l)
        stage_conf(kb, l)
        stage_merge(kb, l, src)
        stage_router(kb, l)
        if moe:
            stage_moe(kb, l, last=(l == L - 1))
        src = "X2T"
    return kb


def kernel(**inputs):
    x = np.asarray(inputs["x"], np.float32)
    B, S, _ = x.shape
    L = inputs["w_in"].shape[0]
    kb = build_program(S, L)
    shared = {"d_" + k_: v for k_, v in make_inputs(inputs, L).items()}
    in_maps = []
    for b in range(B):
        m = dict(shared)
        m["d_xT"] = np.ascontiguousarray(x[b].T)
        in_maps.append(m)
    res = run_bass_kernel_spmd(kb.nc, in_maps, core_ids=list(range(B)))
    return np.stack([np.asarray(r["d_out"], np.float32) for r in res.results], axis=0)
```
